# Optimizing a Trainium2 kernel written in Bass

```python
import math
import jax, jax.numpy as jnp
from jax import lax
import numpy as np

D_MODEL = 1024
BATCH = 16
SEQ = 4096
DEPTH = 4

N_MIXERS = 3
GRID_W = 64
DEEPNORM_ALPHA = (2 * DEPTH) ** 0.25
DEEPNORM_BETA = (8 * DEPTH) ** -0.25
LN_EPS = 1e-5

GLA_HEADS = 4
GLA_KEY_DIM = D_MODEL // 2
GLA_VAL_DIM = D_MODEL
GLA_HEAD_K = GLA_KEY_DIM // GLA_HEADS
GLA_HEAD_V = GLA_VAL_DIM // GLA_HEADS
GLA_GATE_RANK = 16
GLA_TAU = 16.0
GLA_CHUNK = 64
GLA_NORM_EPS = 1e-6
GLA_IN_WIDTH = 2 * GLA_KEY_DIM + 2 * GLA_VAL_DIM + 2 * GLA_GATE_RANK

HYENA_ORDER = 2
HYENA_POS_EMB = 33
HYENA_FILTER_HIDDEN = 64
HYENA_DECAY_TARGET = 1e-2
HYENA_FAST_DECAY = 0.3
HYENA_SLOW_DECAY = 1.5
HYENA_WINDOW_SHIFT = 0.05
HYENA_FILTER_INIT = 0.05

NA_HEAD_DIM = 32
NA_HEADS = D_MODEL // NA_HEAD_DIM
NA_WIN_ROWS = 8
NA_WIN_COLS = 16
NA_QUERY_COLS = 16
NA_BAND_COLS = NA_WIN_COLS + NA_QUERY_COLS

N_EXPERTS = 32
TOP_K = 4
MOE_D_FF = D_MODEL
SWIGLU_ALPHA = 1.702
SWIGLU_LIMIT = 7.0

N_GLA_LAYERS = len(range(0, DEPTH, N_MIXERS))
N_HYENA_LAYERS = len(range(1, DEPTH, N_MIXERS))
N_NA_LAYERS = len(range(2, DEPTH, N_MIXERS))

kernel_name = "hybrid_gla_hyena_natten_moe_encoder"


def _layer_norm(x, g, b):
    xf = x.astype(jnp.float32)
    mu = jnp.mean(xf, axis=-1, keepdims=True)
    var = jnp.mean(jnp.square(xf - mu), axis=-1, keepdims=True)
    return ((xf - mu) * lax.rsqrt(var + LN_EPS)).astype(x.dtype) * g + b


def _gla_scan(q, k, v, log_a, strict):
    B, L, H, dk = q.shape
    dv = v.shape[-1]
    C = GLA_CHUNK
    N = L // C

    def chunks(t):
        return t.astype(jnp.float32).reshape(B, N, C, H, t.shape[-1]).transpose(1, 0, 3, 2, 4)

    qc, kc, vc = chunks(q), chunks(k), chunks(v)
    bc = jnp.cumsum(chunks(log_a), axis=3)
    mask = jnp.tril(jnp.ones((C, C), dtype=bool), k=-1 if strict else 0)

    def step(state, inp):
        q_, k_, v_, b_ = inp
        diff = b_[:, :, :, None, :] - b_[:, :, None, :, :]
        decay = jnp.exp(jnp.where(mask[:, :, None], diff, -jnp.inf))
        scores = jnp.einsum('bhijd,bhjd->bhij', q_[:, :, :, None, :] * decay, k_)
        b_last = b_[:, :, -1:, :]
        out = (jnp.einsum('bhij,bhje->bhie', scores, v_)
               + jnp.einsum('bhid,bhde->bhie', q_ * jnp.exp(b_), state))
        state = (jnp.exp(b_last[:, :, 0, :])[..., None] * state
                 + jnp.einsum('bhjd,bhje->bhde', k_ * jnp.exp(b_last - b_), v_))
        return state, out

    s0 = jnp.zeros((B, H, dk, dv), jnp.float32)
    _, o = lax.scan(step, s0, (qc, kc, vc, bc))
    return o.transpose(1, 0, 3, 2, 4).reshape(B, L, H, dv)


def _gla_mixer(x, w_in, gate_w2, gate_b, norm_g, w_out):
    B, L, _ = x.shape
    H, dk, dv, r = GLA_HEADS, GLA_HEAD_K, GLA_HEAD_V, GLA_GATE_RANK
    proj = x @ w_in
    q, k, v, og, glow = jnp.split(
        proj, [GLA_KEY_DIM, 2 * GLA_KEY_DIM, 2 * GLA_KEY_DIM + GLA_VAL_DIM,
               2 * GLA_KEY_DIM + 2 * GLA_VAL_DIM], axis=-1)
    glow = glow.reshape(B, L, 2, r)
    logits = jnp.einsum('blnr,nrk->blnk', glow, gate_w2) + gate_b
    log_a = jax.nn.log_sigmoid(logits.astype(jnp.float32)) / GLA_TAU
    q = q.reshape(B, L, H, dk) * dk ** -0.5
    k = k.reshape(B, L, H, dk)
    v = v.reshape(B, L, H, dv)
    la_f = log_a[:, :, 0].reshape(B, L, H, dk)
    la_b = log_a[:, :, 1].reshape(B, L, H, dk)
    o_f = _gla_scan(q, k, v, la_f, strict=False)
    flip = lambda t: jnp.flip(t, axis=1)
    o_b = flip(_gla_scan(flip(q), flip(k), flip(v), flip(la_b), strict=True))
    o = o_f + o_b
    o = o * lax.rsqrt(jnp.mean(jnp.square(o), axis=-1, keepdims=True) + GLA_NORM_EPS)
    o = (o * norm_g.astype(jnp.float32)).astype(x.dtype).reshape(B, L, GLA_VAL_DIM)
    return (o * jax.nn.silu(og)) @ w_out


def _short_conv3(u, w, b):
    up = jnp.pad(u, ((0, 0), (1, 1), (0, 0)))
    return up[:, :-2] * w[0] + up[:, 1:-1] * w[1] + up[:, 2:] * w[2] + b


def _hyena_filters(L, w1, b1, sin_freq, w2, b2, w3):
    f32 = jnp.float32
    w1, b1, sin_freq, w2, b2, w3 = (a.astype(f32) for a in (w1, b1, sin_freq, w2, b2, w3))
    d = w3.shape[-1] // (2 * HYENA_ORDER)
    t = jnp.arange(L, dtype=f32)
    t_norm = t / max(L - 1, 1)
    bands = (HYENA_POS_EMB - 1) // 2
    freqs = jnp.linspace(1e-4, bands - 1, bands, dtype=f32)
    ang = (2.0 * math.pi / L) * t[:, None] * freqs[None, :]
    pe = jnp.concatenate([t_norm[:, None], jnp.cos(ang), -jnp.sin(ang)], axis=-1)
    h = jnp.sin(sin_freq[0] * (pe @ w1 + b1))
    h = jnp.sin(sin_freq[1] * (h @ w2 + b2))
    h = (h @ w3).reshape(L, HYENA_ORDER, 2, d)
    min_decay = math.log(HYENA_DECAY_TARGET) / HYENA_SLOW_DECAY
    max_decay = math.log(HYENA_DECAY_TARGET) / HYENA_FAST_DECAY
    deltas = jnp.abs(jnp.linspace(min_decay, max_decay, d, dtype=f32))
    window = jnp.exp(-t_norm[:, None] * deltas[None, :]) + HYENA_WINDOW_SHIFT
    h = h * window[:, None, None, :]
    h_fwd, h_bwd = h[:, :, 0], h[:, :, 1]
    two_sided = jnp.concatenate([h_fwd, jnp.zeros_like(h_fwd[:1]), h_bwd[:0:-1]], axis=0)
    return jnp.fft.rfft(two_sided, axis=0)


def _fft_long_conv(z, filt_f, bias):
    L = z.shape[1]
    Z = jnp.fft.rfft(z, n=2 * L, axis=1)
    y = jnp.fft.irfft(Z * filt_f[None], n=2 * L, axis=1)[:, :L]
    return y + z * bias.astype(jnp.float32)


def _hyena_mixer(x, w_in, b_in, conv_w, conv_b, ffn_w1, ffn_b1, sin_freq, ffn_w2, ffn_b2,
                 ffn_w3, filt_bias, w_out, b_out):
    L = x.shape[1]
    u = _short_conv3(x @ w_in + b_in, conv_w, conv_b)
    v, x1, x2 = jnp.split(u.astype(jnp.float32), 3, axis=-1)
    filt = _hyena_filters(L, ffn_w1, ffn_b1, sin_freq, ffn_w2, ffn_b2, ffn_w3)
    z = x1 * _fft_long_conv(v, filt[:, 0], filt_bias[0])
    z = x2 * _fft_long_conv(z, filt[:, 1], filt_bias[1])
    return z.astype(x.dtype) @ w_out + b_out


def _na_mixer(x, w_in, b_in, rpb, w_out, b_out):
    B, L, D = x.shape
    H, dh = NA_HEADS, D // NA_HEADS
    R = L // GRID_W
    KH = min(NA_WIN_ROWS, R)
    KW, QW, BW = NA_WIN_COLS, NA_QUERY_COLS, NA_BAND_COLS
    NCB = GRID_W // QW
    q, k, v = jnp.split(x @ w_in + b_in, 3, axis=-1)
    q = (q * dh ** -0.5).reshape(B, R, GRID_W, H, dh)
    k = k.reshape(B, R, GRID_W, H, dh)
    v = v.reshape(B, R, GRID_W, H, dh)
    rows = jnp.arange(R)
    row_start = jnp.clip(rows - KH // 2, 0, R - KH)
    key_rows = row_start[:, None] + jnp.arange(KH)[None, :]
    dr_idx = key_rows - rows[:, None] + (NA_WIN_ROWS - 1)
    q_cols = jnp.arange(NCB)[:, None] * QW + jnp.arange(QW)[None, :]
    col_start = jnp.clip(q_cols - KW // 2, 0, GRID_W - KW)
    band_start = jnp.clip(jnp.arange(NCB) * QW - KW // 2, 0, GRID_W - BW)
    band_cols = band_start[:, None] + jnp.arange(BW)[None, :]
    in_win = ((band_cols[:, None, :] >= col_start[..., None])
              & (band_cols[:, None, :] < col_start[..., None] + KW))
    dc_idx = jnp.clip(band_cols[:, None, :] - q_cols[..., None] + (KW - 1), 0, 2 * KW - 2)
    rpb32 = rpb.astype(jnp.float32)

    def row_block(inp):
        q_r, rows_r, dr_r = inp
        q_r = q_r.reshape(B, NCB, QW, H, dh)
        k_r = jnp.take(jnp.take(k, rows_r, axis=1), band_cols, axis=2)
        v_r = jnp.take(jnp.take(v, rows_r, axis=1), band_cols, axis=2)
        s = jnp.einsum('bcqhd,bacjhd->bhcqaj', q_r, k_r).astype(jnp.float32)
        bias = rpb32[:, dr_r][:, :, dc_idx].transpose(0, 2, 3, 1, 4)
        s = jnp.where(in_win[None, None, :, :, None, :], s + bias[None], -jnp.inf)
        p = jax.nn.softmax(s.reshape(B, H, NCB, QW, KH * BW), axis=-1)
        p = p.reshape(s.shape).astype(v.dtype)
        o = jnp.einsum('bhcqaj,bacjhd->bcqhd', p, v_r)
        return o.reshape(B, GRID_W, D)

    o = lax.map(row_block, (q.transpose(1, 0, 2, 3, 4), key_rows, dr_idx))
    o = o.transpose(1, 0, 2, 3).reshape(B, L, D)
    return o @ w_out + b_out


def _clamped_swiglu(h):
    glu, lin = jnp.split(h, 2, axis=-1)
    glu = jnp.minimum(glu, SWIGLU_LIMIT)
    lin = jnp.clip(lin, -SWIGLU_LIMIT, SWIGLU_LIMIT)
    return glu * jax.nn.sigmoid(SWIGLU_ALPHA * glu) * (lin + 1.0)


def _moe(x, router_w, router_b, w_gu, b_gu, w_down, b_down):
    B, L, D = x.shape
    xt = x.reshape(B * L, D)
    logits = (xt @ router_w + router_b).astype(jnp.float32)
    top_v, top_i = lax.top_k(logits, TOP_K)
    gates = jax.nn.softmax(top_v, axis=-1)
    combine = jnp.sum(jax.nn.one_hot(top_i, N_EXPERTS, dtype=jnp.float32) * gates[..., None],
                      axis=1).astype(x.dtype)
    out = jnp.zeros_like(xt)
    for e in range(N_EXPERTS):
        y = _clamped_swiglu(xt @ w_gu[e] + b_gu[e]) @ w_down[e] + b_down[e]
        out = out + combine[:, e:e + 1] * y
    return out.reshape(B, L, D)


def setup_inputs(seed: int = 0) -> dict:
    key = jax.random.key(seed)
    ks = iter(jax.random.split(key, 40))
    D, F = D_MODEL, MOE_D_FF
    nA, nB, nC = N_GLA_LAYERS, N_HYENA_LAYERS, N_NA_LAYERS

    def nrm(shape, scale):
        return scale * jax.random.normal(next(ks), shape, jnp.float32)

    return {
        "x": nrm((BATCH, SEQ, D), 1.0),
        "ln1_g": 1.0 + nrm((DEPTH, D), 0.02),
        "ln1_b": nrm((DEPTH, D), 0.02),
        "ln2_g": 1.0 + nrm((DEPTH, D), 0.02),
        "ln2_b": nrm((DEPTH, D), 0.02),
        "gla_w_in": nrm((nA, D, GLA_IN_WIDTH), D ** -0.5),
        "gla_gate_w2": nrm((nA, 2, GLA_GATE_RANK, GLA_KEY_DIM), GLA_GATE_RANK ** -0.5),
        "gla_gate_b": nrm((nA, 2, GLA_KEY_DIM), 0.1),
        "gla_norm_g": 1.0 + nrm((nA, GLA_HEAD_V), 0.02),
        "gla_w_out": nrm((nA, GLA_VAL_DIM, D), GLA_VAL_DIM ** -0.5 * DEEPNORM_BETA),
        "hy_w_in": nrm((nB, D, 3 * D), D ** -0.5),
        "hy_b_in": nrm((nB, 3 * D), 0.02),
        "hy_conv_w": nrm((nB, 3, 3 * D), 3 ** -0.5),
        "hy_conv_b": nrm((nB, 3 * D), 0.02),
        "hy_ffn_w1": nrm((nB, HYENA_POS_EMB, HYENA_FILTER_HIDDEN), HYENA_POS_EMB ** -0.5),
        "hy_ffn_b1": nrm((nB, HYENA_FILTER_HIDDEN), 0.5),
        "hy_sin_freq": 1.0 + nrm((nB, 2, HYENA_FILTER_HIDDEN), 0.02),
        "hy_ffn_w2": nrm((nB, HYENA_FILTER_HIDDEN, HYENA_FILTER_HIDDEN), HYENA_FILTER_HIDDEN ** -0.5),
        "hy_ffn_b2": nrm((nB, HYENA_FILTER_HIDDEN), 0.5),
        "hy_ffn_w3": nrm((nB, HYENA_FILTER_HIDDEN, 2 * HYENA_ORDER * D),
                          HYENA_FILTER_HIDDEN ** -0.5 * HYENA_FILTER_INIT),
        "hy_filt_bias": nrm((nB, HYENA_ORDER, D), 0.5),
        "hy_w_out": nrm((nB, D, D), D ** -0.5 * DEEPNORM_BETA),
        "hy_b_out": nrm((nB, D), 0.02),
        "na_w_in": nrm((nC, D, 3 * D), D ** -0.5),
        "na_b_in": nrm((nC, 3 * D), 0.02),
        "na_rpb": nrm((nC, NA_HEADS, 2 * NA_WIN_ROWS - 1, 2 * NA_WIN_COLS - 1), 0.02),
        "na_w_out": nrm((nC, D, D), D ** -0.5 * DEEPNORM_BETA),
        "na_b_out": nrm((nC, D), 0.02),
        "moe_router_w": nrm((DEPTH, D, N_EXPERTS), D ** -0.5),
        "moe_router_b": nrm((DEPTH, N_EXPERTS), 0.01),
        "moe_w_gu": nrm((DEPTH, N_EXPERTS, D, 2 * F), D ** -0.5),
        "moe_b_gu": nrm((DEPTH, N_EXPERTS, 2 * F), 0.01),
        "moe_w_down": nrm((DEPTH, N_EXPERTS, F, D), F ** -0.5 * DEEPNORM_BETA),
        "moe_b_down": nrm((DEPTH, N_EXPERTS, D), 0.01),
    }


def reference(x, ln1_g, ln1_b, ln2_g, ln2_b,
              gla_w_in, gla_gate_w2, gla_gate_b, gla_norm_g, gla_w_out,
              hy_w_in, hy_b_in, hy_conv_w, hy_conv_b, hy_ffn_w1, hy_ffn_b1, hy_sin_freq,
              hy_ffn_w2, hy_ffn_b2, hy_ffn_w3, hy_filt_bias, hy_w_out, hy_b_out,
              na_w_in, na_b_in, na_rpb, na_w_out, na_b_out,
              moe_router_w, moe_router_b, moe_w_gu, moe_b_gu, moe_w_down, moe_b_down):
    for i in range(DEPTH):
        m, j = i % N_MIXERS, i // N_MIXERS
        if m == 0:
            h = _gla_mixer(x, gla_w_in[j], gla_gate_w2[j], gla_gate_b[j], gla_norm_g[j], gla_w_out[j])
        elif m == 1:
            h = _hyena_mixer(x, hy_w_in[j], hy_b_in[j], hy_conv_w[j], hy_conv_b[j], hy_ffn_w1[j],
                             hy_ffn_b1[j], hy_sin_freq[j], hy_ffn_w2[j], hy_ffn_b2[j], hy_ffn_w3[j],
                             hy_filt_bias[j], hy_w_out[j], hy_b_out[j])
        else:
            h = _na_mixer(x, na_w_in[j], na_b_in[j], na_rpb[j], na_w_out[j], na_b_out[j])
        x = _layer_norm(DEEPNORM_ALPHA * x + h, ln1_g[i], ln1_b[i])
        f = _moe(x, moe_router_w[i], moe_router_b[i], moe_w_gu[i], moe_b_gu[i],
                 moe_w_down[i], moe_b_down[i])
        x = _layer_norm(DEEPNORM_ALPHA * x + f, ln2_g[i], ln2_b[i])
    return x
```

```python
import math
from contextlib import ExitStack
import numpy as np
import concourse.bass as bass
import concourse.mybir as mybir
from concourse.bass_utils import run_bass_kernel_spmd

F32 = mybir.dt.float32
BF16 = mybir.dt.bfloat16
AF = mybir.ActivationFunctionType
ALU = mybir.AluOpType
AX = mybir.AxisListType

D = 1024
DEPTH = 4
ALPHA = (2 * DEPTH) ** 0.25
LN_EPS = 1e-5
NE = 32
TOPK = 4


FORCE_SYNC = False
MOE_NEXP = 32


class Builder:
    def __init__(self, nc, es):
        self.nc = nc
        self.es = es
        self.levels = []
        self.depth = 0
        self._level(0)
        self.g = 0
        self.d = 0
        self.stack = []
        self.dummy = es.enter_context(nc.sbuf_tensor("sb_dummy", [128, 8], F32))
        self.engs = [nc.tensor, nc.vector, nc.scalar, nc.gpsimd, nc.sync]
        self.waited = {}
        self.dma_rr = 0
        self.nloops = 0
        self.loop_eng = nc.sync
        self.inloop = False

    def _level(self, k):
        while len(self.levels) <= k:
            n = len(self.levels)
            gs = self.es.enter_context(self.nc.semaphore(f"gsem{n}"))
            ds = self.es.enter_context(self.nc.semaphore(f"dsem{n}"))
            it = self.es.enter_context(self.nc.semaphore(f"isem{n}"))
            self.levels.append((gs, ds, it))
        return self.levels[k]

    def sb(self, name, shape, dt=F32):
        return self.es.enter_context(self.nc.sbuf_tensor("sb_" + name, list(shape), dt))

    def ps(self, name, shape, dt=F32):
        return self.es.enter_context(self.nc.psum_tensor("ps_" + name, list(shape), dt))

    def _wait(self, eng):
        gs, ds, _ = self.levels[self.depth]
        key = id(eng)
        lg, ld = self.waited.get(key, (-1, -1))
        if self.g > lg:
            eng.wait_ge(gs, self.g)
        if self.d > ld:
            eng.wait_ge(ds, self.d)
        self.waited[key] = (max(lg, self.g), max(ld, self.d))

    def op(self, eng, fn):
        self._wait(eng)
        fn().then_inc(self.levels[self.depth][0], 1)
        self.g += 1

    def V(self, fn):
        self.op(self.nc.vector, fn)

    def A(self, fn):
        self.op(self.nc.scalar, fn)

    def G(self, fn):
        self.op(self.nc.gpsimd, fn)

    def mm(self, fns):
        self._wait(self.nc.tensor)
        for f in fns[:-1]:
            f()
        fns[-1]().then_inc(self.levels[self.depth][0], 1)
        self.g += 1

    def dma(self, out, in_, slow=False):
        if not self.inloop:
            self.dma_rr = (self.dma_rr + 1) % 3
            eng = [self.nc.sync, self.nc.scalar, self.nc.sync][self.dma_rr]
        else:
            eng = self.loop_eng
        if FORCE_SYNC == "act":
            eng = self.nc.scalar
        elif FORCE_SYNC:
            eng = self.nc.sync
        self._wait(eng)
        if slow:
            eng.dma_start(out=out, in_=in_, allow_slow_non_contiguous=True).then_inc(self.levels[self.depth][1], 16)
        else:
            eng.dma_start(out=out, in_=in_).then_inc(self.levels[self.depth][1], 16)
        self.d += 16

    def loop(self, n, body, eng=None):
        prev_eng = self.loop_eng
        if eng is not None:
            self.loop_eng = eng
        try:
            self._loop(n, body)
        finally:
            self.loop_eng = prev_eng

    def _loop(self, n, body):
        if n == 1:
            old_depth_marker = self.inloop
            self.inloop = True
            body(0)
            self.inloop = old_depth_marker
            return
        nc = self.nc
        for e in self.engs:
            self._wait(e)
        og, od, odepth, owaited = self.g, self.d, self.depth, self.waited
        self.depth += 1
        gs, ds, it = self._level(self.depth)
        self.nloops += 1
        tag = self.nloops
        old_inloop = self.inloop
        self.inloop = True
        engines = mybir.ALL_ENGINES
        loop_start = f"L{tag}_loop"
        loop_end = f"L{tag}_end"
        registers = nc.alloc_registers(f"L{tag}_i", engines=engines)
        nc.regs_mov(registers, 0)
        nc.br(loop_start, engines=engines)
        with nc.body(loop_start, valid_engines=engines):
            i = nc.snap(registers, min_val=0, max_val=n - 1)
            for e in self.engs:
                e.wait_ge(it, i)
            self.g = 0
            self.d = 0
            self.waited = {}
            body(i)
            p = nc.gpsimd
            p.wait_ge(gs, self.g)
            p.wait_ge(ds, self.d)
            p.sem_clear(gs)
            p.sem_clear(ds)
            p.memset(self.dummy[0:1, 0:1], 0.0).then_inc(it, 1)
            nc.regs_alu(registers, registers, 1, op=ALU.add)
            nc.br_lt(registers, n, on_true=loop_start, on_false=loop_end, engines=engines)
        nc.switch_bb(loop_end)
        for h in registers.handles:
            nc.free_register(h)
        for h in i.val.handles:
            nc.free_register(h)
        p = nc.gpsimd
        p.wait_ge(it, n)
        p.sem_clear(it)
        self.depth = odepth
        self.inloop = old_inloop
        self.g, self.d, self.waited = og, od, owaited
        p.memset(self.dummy[0:1, 1:2], 0.0).then_inc(self.levels[self.depth][0], 1)
        self.g += 1

    def finish(self):
        for e in self.engs:
            self._wait(e)


def mm_group(nc, ps, pairs):
    fns = []
    n = len(pairs)
    for i, (l, r) in enumerate(pairs):
        fns.append(lambda l=l, r=r, i=i: nc.tensor.matmul(ps, l, r, start=(i == 0), stop=(i == n - 1)))
    return fns


class Ctx:
    pass


def alloc_common(b, c):
    c.ident = b.sb("ident", [128, 128], F32)
    c.xt = b.sb("xt", [128, D], F32)
    c.yt = b.sb("yt", [128, D], F32)
    c.gbc = b.sb("gbc", [128, D], F32)
    c.bbc = b.sb("bbc", [128, D], F32)
    c.stats = b.sb("stats", [128, 2, 6], F32)
    c.mv = b.sb("mv", [128, 2], F32)
    c.rstd = b.sb("rstd", [128, 1], F32)
    c.psA = b.ps("psA", [128, 512], F32)
    c.psB = b.ps("psB", [128, 512], F32)
    c.psC = b.ps("psC", [128, 512], F32)
    c.psD = b.ps("psD", [128, 512], F32)
    c.psT = b.ps("psT", [128, 512], F32)


def layer_norm(b, c, src, dst):
    nc = b.nc
    b.V(lambda: nc.vector.bn_stats(out=c.stats[:, 0, :], in_=src[:, 0:512]))
    b.V(lambda: nc.vector.bn_stats(out=c.stats[:, 1, :], in_=src[:, 512:1024]))
    b.V(lambda: nc.vector.bn_aggr(out=c.mv[:], in_=c.stats[:].rearrange("p a s -> p (a s)")))
    b.V(lambda: nc.vector.tensor_scalar(out=c.rstd[:], in0=c.mv[:, 1:2], scalar1=LN_EPS, scalar2=None, op0=ALU.add))
    b.A(lambda: nc.scalar.activation(out=c.rstd[:], in_=c.rstd[:], func=AF.Sqrt))
    b.V(lambda: nc.vector.reciprocal(out=c.rstd[:], in_=c.rstd[:]))
    b.V(lambda: nc.vector.tensor_scalar(out=dst, in0=src, scalar1=c.mv[:, 0:1], scalar2=c.rstd[:, 0:1],
                                        op0=ALU.subtract, op1=ALU.mult))
    b.V(lambda: nc.vector.tensor_tensor(out=dst, in0=dst, in1=c.gbc[:], op=ALU.mult))
    b.V(lambda: nc.vector.tensor_tensor(out=dst, in0=dst, in1=c.bbc[:], op=ALU.add))


def load_ln(b, c, g_ap, b_ap):
    b.dma(c.gbc[:], g_ap.partition_broadcast(128))
    b.dma(c.bbc[:], b_ap.partition_broadcast(128))


def alloc_moe(b, c):
    c.xT32 = b.sb("xT32", [128, 8, 128], F32)
    c.xTb = b.sb("xTb", [128, 8, 128], BF16)
    c.wr = b.sb("wr", [128, 8, NE], F32)
    c.rb = b.sb("rb", [128, NE], F32)
    c.bd = b.sb("bd", [NE, D], F32)
    c.lg = b.sb("lg", [128, NE], F32)
    c.ex = b.sb("ex", [128, NE], F32)
    c.msk = b.sb("msk", [128, NE], F32)
    c.top8 = b.sb("top8", [128, 8], F32)
    c.nm = b.sb("nm", [128, 1], F32)
    c.ssum = b.sb("ssum", [128, 1], F32)
    c.combT = b.sb("combT", [NE, 128], F32)
    c.xblk = b.sb("xblk", [128, 8, 8, 128], BF16)
    c.acc = b.sb("acc", [128, 8, D], F32)
    c.cmball = b.sb("cmball", [128, 8, NE], F32)
    c.stg = b.sb("stg", [128, 4, 2048], F32)
    c.wgu = b.sb("wgu", [128, 8, 2048], BF16)
    c.wd = b.sb("wd", [128, 8, D], BF16)
    c.bguall = b.sb("bguall", [128, NE, 16], F32)
    c.bl1all = b.sb("bl1all", [128, NE, 8], F32)
    c.hg = b.sb("hg", [128, 512], F32)
    c.hs = b.sb("hs", [128, 512], F32)
    c.hl = b.sb("hl", [128, 512], F32)
    c.hT = b.sb("hT", [128, 8, 512], BF16)


def moe_layer(b, c, li, src_d, dst_d, w, ntok):
    nc = b.nc
    ntiles = ntok // 128
    nblk = ntok // 1024
    src_t = src_d.rearrange("(n p) d -> n p d", p=128)
    dst_t = dst_d.rearrange("(n p) d -> n p d", p=128)
    acc_t = c.acc_d.rearrange("(n p) d -> n p d", p=128)
    xT_t = c.xT_d
    comb_t = c.comb_d

    b.dma(c.wr[:], w["router_w"].rearrange("(k p) e -> p k e", p=128))
    b.dma(c.rb[:], w["router_b"].partition_broadcast(128))
    b.dma(c.bd[:], w["b_down"])

    def p1(t):
        b.dma(c.xt[:], src_t[t])
        for k in range(8):
            b.mm([lambda k=k: nc.tensor.transpose(c.psT[:, 0:128], c.xt[:, k * 128:(k + 1) * 128], c.ident[:])])
            b.V(lambda k=k: nc.vector.tensor_copy(c.xT32[:, k, :], c.psT[:, 0:128]))
            b.G(lambda k=k: nc.gpsimd.tensor_copy(c.xTb[:, k, :], c.xT32[:, k, :]))
        b.mm(mm_group(nc, c.psA[:, 0:NE], [(c.xT32[:, k, :], c.wr[:, k, :]) for k in range(8)]))
        b.V(lambda: nc.vector.tensor_tensor(out=c.lg[:], in0=c.psA[:, 0:NE], in1=c.rb[:], op=ALU.add))
        b.V(lambda: nc.vector.max(out=c.top8[:], in_=c.lg[:]))
        b.V(lambda: nc.vector.tensor_scalar(out=c.msk[:], in0=c.lg[:], scalar1=c.top8[:, 3:4], scalar2=None, op0=ALU.is_ge))
        b.V(lambda: nc.vector.tensor_scalar(out=c.nm[:], in0=c.top8[:, 0:1], scalar1=-1.0, scalar2=None, op0=ALU.mult))
        b.A(lambda: nc.scalar.activation(out=c.ex[:], in_=c.lg[:], func=AF.Exp, bias=c.nm[:, 0:1], scale=1.0))
        b.V(lambda: nc.vector.tensor_tensor(out=c.ex[:], in0=c.ex[:], in1=c.msk[:], op=ALU.mult))
        b.V(lambda: nc.vector.reduce_sum(out=c.ssum[:], in_=c.ex[:], axis=AX.X))
        b.V(lambda: nc.vector.reciprocal(out=c.ssum[:], in_=c.ssum[:]))
        b.V(lambda: nc.vector.tensor_scalar(out=c.ex[:], in0=c.ex[:], scalar1=c.ssum[:, 0:1], scalar2=None, op0=ALU.mult))
        b.dma(comb_t[t][:, 0:NE], c.ex[:])
        b.mm([lambda: nc.tensor.transpose(c.psT[0:NE, 0:128], c.ex[:], c.ident[:])])
        b.V(lambda: nc.vector.tensor_copy(c.combT[:], c.psT[0:NE, 0:128]))
        for h in range(2):
            ps = c.psA if h == 0 else c.psB
            b.mm([lambda ps=ps, h=h: nc.tensor.matmul(ps[:], c.combT[:], c.bd[:, h * 512:(h + 1) * 512], start=True, stop=True)])
            b.V(lambda ps=ps, h=h: nc.vector.scalar_tensor_tensor(out=c.yt[:, h * 512:(h + 1) * 512], in0=c.xt[:, h * 512:(h + 1) * 512],
                                                                 scalar=ALPHA, in1=ps[:], op0=ALU.mult, op1=ALU.add))
        b.dma(acc_t[t], c.yt[:])
        b.dma(xT_t[t], c.xTb[:])

    b.loop(ntiles, p1, eng=nc.sync)

    load_ln(b, c, w["ln2_g"], w["ln2_b"])
    dst_bb = dst_d.rearrange("(n t p) d -> n p t d", t=8, p=128)
    xT_b = xT_t.rearrange("(n t) p k j -> n p t k j", t=8)
    acc_b = c.acc_d.rearrange("(n t p) d -> n p t d", t=8, p=128)
    comb_b = c.comb_d.rearrange("(n t) p e -> n p t e", t=8)[:, :, :, 0:NE]
    bgu_v = w["b_gu"].rearrange("e (c p) -> p e c", p=128)
    for e0 in range(0, NE, 4):
        b.dma(c.bguall[:, e0:e0 + 4, :], bgu_v[:, e0:e0 + 4, :], slow=True)
    b.V(lambda: nc.vector.tensor_scalar(out=c.bl1all[:], in0=c.bguall[:, :, 8:16], scalar1=1.0, scalar2=None, op0=ALU.add))
    wgu = w["w_gu"].rearrange("e (k p) f -> e p k f", p=128)
    wdn = w["w_down"].rearrange("e (k p) f -> e p k f", p=128)

    def blk(bi):
        b.dma(c.xblk[:], xT_b[bi])
        b.dma(c.acc[:], acc_b[bi])
        b.dma(c.cmball[:], comb_b[bi])

        def expert(e):
            for hh in range(2):
                b.dma(c.stg[:], wgu[e][:, hh * 4:(hh + 1) * 4, :])
                b.V(lambda hh=hh: nc.vector.tensor_copy(c.wgu[:, hh * 4:hh * 4 + 2, :], c.stg[:, 0:2, :]))
                b.G(lambda hh=hh: nc.gpsimd.tensor_copy(c.wgu[:, hh * 4 + 2:hh * 4 + 4, :], c.stg[:, 2:4, :]))
            b.dma(c.stg[:].rearrange("p a (h f) -> p (a h) f", h=2), wdn[e])
            b.V(lambda: nc.vector.tensor_copy(c.wd[:, 0:4, :], c.stg[:, 0:2, :].rearrange("p a (h f) -> p (a h) f", h=2)))
            b.G(lambda: nc.gpsimd.tensor_copy(c.wd[:, 4:8, :], c.stg[:, 2:4, :].rearrange("p a (h f) -> p (a h) f", h=2)))
            for tt in range(2):
                for j in range(8):
                    b.mm(mm_group(nc, c.psA[:], [(c.wgu[:, k, j * 128:(j + 1) * 128],
                                                  c.xblk[:, tt * 4:(tt + 1) * 4, k, :]) for k in range(8)]))
                    b.mm(mm_group(nc, c.psB[:], [(c.wgu[:, k, 1024 + j * 128:1024 + (j + 1) * 128],
                                                  c.xblk[:, tt * 4:(tt + 1) * 4, k, :]) for k in range(8)]))
                    b.V(lambda j=j, e=e: nc.vector.tensor_scalar(out=c.hg[:], in0=c.psA[:], scalar1=c.bguall[:, e, j:j + 1], scalar2=7.0,
                                                            op0=ALU.add, op1=ALU.min))
                    b.A(lambda: nc.scalar.activation(out=c.hs[:], in_=c.hg[:], func=AF.Sigmoid, scale=1.702))
                    b.V(lambda j=j, e=e: nc.vector.tensor_scalar(out=c.hl[:], in0=c.psB[:], scalar1=c.bl1all[:, e, j:j + 1], scalar2=8.0,
                                                            op0=ALU.add, op1=ALU.min))
                    b.G(lambda: nc.gpsimd.tensor_tensor(out=c.hg[:], in0=c.hg[:], in1=c.hs[:], op=ALU.mult))
                    b.V(lambda j=j: nc.vector.scalar_tensor_tensor(out=c.hT[:, j, :], in0=c.hl[:], scalar=-6.0, in1=c.hg[:],
                                                                   op0=ALU.max, op1=ALU.mult))
                for ts in range(4):
                    ti = tt * 4 + ts
                    for h in range(2):
                        ps = c.psC if h == 0 else c.psD
                        b.mm(mm_group(nc, ps[:], [(c.hT[:, j, ts * 128:(ts + 1) * 128], c.wd[:, j, h * 512:(h + 1) * 512])
                                                  for j in range(8)]))
                        b.V(lambda ps=ps, ti=ti, h=h, e=e: nc.vector.scalar_tensor_tensor(
                            out=c.acc[:, ti, h * 512:(h + 1) * 512], in0=ps[:], scalar=c.cmball[:, ti, e:e + 1],
                            in1=c.acc[:, ti, h * 512:(h + 1) * 512], op0=ALU.mult, op1=ALU.add))

        for e in range(MOE_NEXP):
            expert(e)
        for ti in range(8):
            layer_norm(b, c, c.acc[:, ti, :], c.acc[:, ti, :])
        b.dma(dst_bb[bi], c.acc[:])

    b.loop(nblk, blk, eng=nc.scalar)


def load_cast_rows(b, c, dst_bf, src_d, ncols):
    nc = b.nc
    src = src_d.rearrange("(k p) f -> p k f", p=128)
    st = c.stg[:].rearrange("p a f -> p (a f)")
    for k in range(8):
        for c0 in range(0, ncols, 2048):
            w = min(2048, ncols - c0)
            b.dma(st[:, 0:w], src[:, k, c0:c0 + w])
            b.V(lambda k=k, c0=c0, w=w: nc.vector.tensor_copy(dst_bf[:, k, c0:c0 + w], st[:, 0:w]))


def x_to_xT(b, c):
    nc = b.nc
    for k in range(8):
        b.mm([lambda k=k: nc.tensor.transpose(c.psT[:, 0:128], c.xt[:, k * 128:(k + 1) * 128], c.ident[:])])
        b.V(lambda k=k: nc.vector.tensor_copy(c.xTb[:, k, :], c.psT[:, 0:128]))


def out_proj_ln(b, c, src, wout_bf, bias_bc, dst_ap):
    nc = b.nc
    for k in range(8):
        b.mm([lambda k=k: nc.tensor.transpose(c.psT[:, 0:128], src[:, k * 128:(k + 1) * 128], c.ident[:])])
        b.V(lambda k=k: nc.vector.tensor_copy(c.xTb[:, k, :], c.psT[:, 0:128]))
    for h in range(2):
        ps = c.psA if h == 0 else c.psB
        b.mm(mm_group(nc, ps[:], [(c.xTb[:, k, :], wout_bf[:, k, h * 512:(h + 1) * 512]) for k in range(8)]))
        b.V(lambda ps=ps, h=h: nc.vector.scalar_tensor_tensor(out=c.yt[:, h * 512:(h + 1) * 512], in0=c.xt[:, h * 512:(h + 1) * 512],
                                                             scalar=ALPHA, in1=ps[:], op0=ALU.mult, op1=ALU.add))
    if bias_bc is not None:
        b.V(lambda: nc.vector.tensor_tensor(out=c.yt[:], in0=c.yt[:], in1=bias_bc, op=ALU.add))
    layer_norm(b, c, c.yt[:], c.yt[:])
    b.dma(dst_ap, c.yt[:])


def alloc_mixer(b, c):
    c.wmB = b.sb("wmB", [128, 8, 1056], BF16)
    c.tri = b.sb("tri", [128, 3, 128], F32)
    sflat = c.stg[:].rearrange("p a f -> p (a f)")
    c.gw2 = sflat[0:32, 0:1024].rearrange("p (n f) -> p n f", n=2)
    c.gbb = sflat[:, 2048:2560]
    c.ngb = b.sb("ngb", [128, 256], F32)
    c.qT = b.sb("qT", [128, 4, 128], BF16)
    c.kT = b.sb("kT", [128, 4, 128], BF16)
    c.ktm = b.sb("ktm", [128, 512], BF16)
    c.vbf = b.sb("vbf", [128, D], BF16)
    c.scT = b.sb("scT", [128, 128], BF16)
    c.Sbf = b.sb("Sbf", [128, 4, 256], BF16)
    c.gcol = b.sb("gcol", [128, 4], F32)
    c.glT = b.sb("glT", [32, 128], F32)
    c.gl = b.sb("gl", [128, 32], F32)
    c.r4 = b.sb("r4", [128, 4], F32)
    c.hv = b.sb("hv", [128, 16], F32)


def gla_layer(b, c, src_d, dst_d, w, ntok, L):
    nc = b.nc
    nseq = ntok // L
    tps = L // 128
    src_t = src_d.rearrange("(s n p) d -> s n p d", p=128, n=tps)
    dst_t = dst_d.rearrange("(s n p) d -> s n p d", p=128, n=tps)
    of_t = c.acc_d.rearrange("(s n p) d -> s n p d", p=128, n=tps)
    load_cast_rows(b, c, c.wgu, w["w_in"][:, 0:2048], 2048)
    load_cast_rows(b, c, c.wmB, w["w_in"][:, 2048:3104], 1056)
    load_cast_rows(b, c, c.wd, w["w_out"], 1024)
    b.G(lambda: nc.gpsimd.memset(c.gw2, 0.0))
    b.dma(c.gw2[0:16, 0, :], w["gate_w2"][0])
    b.dma(c.gw2[16:32, 1, :], w["gate_w2"][1])
    b.dma(c.ngb[:], w["norm_g"].partition_broadcast(128))
    load_ln(b, c, w["ln1_g"], w["ln1_b"])
    og = c.acc[:, 0, :]
    q = c.acc[:, 1, 0:512]
    k_ = c.acc[:, 1, 512:1024]
    oacc = c.acc[:, 2, :]
    S = c.acc[:, 3, :].rearrange("p (h e) -> p h e", h=4)
    oft = c.acc[:, 4, :]
    sq = c.acc[:, 5, :]
    B = c.hg
    EB = c.hs
    EnB = c.hl

    def one_pass(direction):
        n = direction
        b.dma(c.gbb, w["gate_b"][n].partition_broadcast(128))

        def seq(si):
            b.V(lambda: nc.vector.memset(S, 0.0))
            b.V(lambda: nc.vector.memset(c.Sbf[:], 0.0))

            def tile(ti):
                t = ti if n == 0 else (tps - 1) - ti
                b.dma(c.xt[:], src_t[si][t])
                x_to_xT(b, c)
                def proj(ps, wbf, c0, wid):
                    b.mm(mm_group(nc, ps[:, 0:wid], [(c.xTb[:, kk, :], wbf[:, kk, c0:c0 + wid]) for kk in range(8)]))
                proj(c.psA, c.wgu, 0, 512)
                b.V(lambda: nc.vector.tensor_copy(q, c.psA[:]))
                proj(c.psA, c.wgu, 512, 512)
                b.V(lambda: nc.vector.tensor_copy(k_, c.psA[:]))
                proj(c.psA, c.wgu, 1024, 512)
                b.V(lambda: nc.vector.tensor_copy(c.vbf[:, 0:512], c.psA[:]))
                proj(c.psA, c.wgu, 1536, 512)
                b.V(lambda: nc.vector.tensor_copy(c.vbf[:, 512:1024], c.psA[:]))
                proj(c.psA, c.wmB, 1024, 32)
                b.V(lambda: nc.vector.tensor_copy(c.gl[:], c.psA[:, 0:32]))
                b.mm([lambda: nc.tensor.transpose(c.psT[0:32, 0:128], c.gl[:], c.ident[:])])
                b.V(lambda: nc.vector.tensor_copy(c.glT[:], c.psT[0:32, 0:128]))
                b.mm([lambda: nc.tensor.matmul(c.psA[:], c.glT[:], c.gw2[:, n, :], start=True, stop=True)])
                b.V(lambda: nc.vector.tensor_tensor(out=B[:], in0=c.psA[:], in1=c.gbb, op=ALU.add))
                b.A(lambda: nc.scalar.activation(out=B[:], in_=B[:], func=AF.Exp, scale=-1.0))
                b.A(lambda: nc.scalar.activation(out=B[:], in_=B[:], func=AF.Ln, bias=1.0, scale=1.0))
                b.mm([lambda: nc.tensor.matmul(c.psA[:], c.tri[:, n, :], B[:], start=True, stop=True)])
                b.A(lambda: nc.scalar.activation(out=EB[:], in_=c.psA[:], func=AF.Exp, scale=-1.0 / 16.0))
                b.A(lambda: nc.scalar.activation(out=EnB[:], in_=c.psA[:], func=AF.Exp, scale=1.0 / 16.0))
                b.V(lambda: nc.vector.scalar_tensor_tensor(out=q, in0=q, scalar=128.0 ** -0.5, in1=EB[:], op0=ALU.mult, op1=ALU.mult))
                b.V(lambda: nc.vector.tensor_tensor(out=k_, in0=k_, in1=EnB[:], op=ALU.mult))
                b.V(lambda: nc.vector.tensor_copy(c.ktm[:], k_))
                for h in range(4):
                    hs = slice(h * 128, (h + 1) * 128)
                    b.mm([lambda hs=hs: nc.tensor.transpose(c.psT[:, 0:128], q[:, hs], c.ident[:])])
                    b.V(lambda h=h: nc.vector.tensor_copy(c.qT[:, h, :], c.psT[:, 0:128]))
                    b.mm([lambda hs=hs: nc.tensor.transpose(c.psT[:, 0:128], k_[:, hs], c.ident[:])])
                    b.V(lambda h=h: nc.vector.tensor_copy(c.kT[:, h, :], c.psT[:, 0:128]))
                    b.mm([lambda hs=hs: nc.tensor.transpose(c.psT[:, 0:128], EB[:, hs], c.ident[:])])
                    col = 127 if n == 0 else 0
                    b.V(lambda h=h, col=col: nc.vector.tensor_copy(c.gcol[:, h:h + 1], c.psT[:, col:col + 1]))
                for h in range(4):
                    vs = slice(h * 256, (h + 1) * 256)
                    b.mm([lambda h=h: nc.tensor.matmul(c.psB[:, 0:128], c.kT[:, h, :], c.qT[:, h, :], start=True, stop=True)])
                    mk = 0 if n == 0 else 2
                    b.V(lambda mk=mk: nc.vector.tensor_tensor(out=c.scT[:], in0=c.psB[:, 0:128], in1=c.tri[:, mk, :], op=ALU.mult))
                    b.mm(mm_group(nc, c.psC[:, 0:256], [(c.scT[:], c.vbf[:, vs]), (c.qT[:, h, :], c.Sbf[:, h, :])]))
                    b.V(lambda vs=vs: nc.vector.tensor_copy(oacc[:, vs], c.psC[:, 0:256]))
                    b.mm([lambda h=h, vs=vs: nc.tensor.matmul(c.psD[:, 0:256], c.ktm[:, h * 128:(h + 1) * 128], c.vbf[:, vs], start=True, stop=True)])
                    b.V(lambda h=h: nc.vector.tensor_tensor(out=S[:, h, :], in0=c.psD[:, 0:256], in1=S[:, h, :], op=ALU.add))
                    b.V(lambda h=h: nc.vector.tensor_scalar(out=S[:, h, :], in0=S[:, h, :], scalar1=c.gcol[:, h:h + 1], scalar2=None, op0=ALU.mult))
                    b.V(lambda h=h: nc.vector.tensor_copy(c.Sbf[:, h, :], S[:, h, :]))
                if n == 0:
                    b.dma(of_t[si][t], oacc)
                else:
                    b.dma(oft, of_t[si][t])
                    b.V(lambda: nc.vector.tensor_tensor(out=oacc, in0=oacc, in1=oft, op=ALU.add))
                    b.V(lambda: nc.vector.tensor_tensor(out=sq, in0=oacc, in1=oacc, op=ALU.mult))
                    b.V(lambda: nc.vector.reduce_sum(out=c.r4[:], in_=sq.rearrange("p (h e) -> p h e", h=4), axis=AX.X))
                    b.V(lambda: nc.vector.tensor_scalar(out=c.r4[:], in0=c.r4[:], scalar1=1.0 / 256.0, scalar2=1e-6, op0=ALU.mult, op1=ALU.add))
                    b.A(lambda: nc.scalar.activation(out=c.r4[:], in_=c.r4[:], func=AF.Sqrt))
                    b.V(lambda: nc.vector.reciprocal(out=c.r4[:], in_=c.r4[:]))
                    for h in range(4):
                        vs = slice(h * 256, (h + 1) * 256)
                        b.V(lambda h=h, vs=vs: nc.vector.scalar_tensor_tensor(out=oacc[:, vs], in0=oacc[:, vs], scalar=c.r4[:, h:h + 1],
                                                                           in1=c.ngb[:], op0=ALU.mult, op1=ALU.mult))
                    for hh in range(2):
                        proj(c.psA, c.wmB, hh * 512, 512)
                        b.V(lambda hh=hh: nc.vector.tensor_copy(og[:, hh * 512:(hh + 1) * 512], c.psA[:]))
                    b.A(lambda: nc.scalar.activation(out=sq, in_=og, func=AF.Sigmoid))
                    b.V(lambda: nc.vector.tensor_tensor(out=sq, in0=sq, in1=og, op=ALU.mult))
                    b.V(lambda: nc.vector.tensor_tensor(out=oacc, in0=oacc, in1=sq, op=ALU.mult))
                    out_proj_ln(b, c, oacc, c.wd, None, dst_t[si][t])

            b.loop(tps, tile)

        b.loop(nseq, seq, eng=nc.sync)

    one_pass(0)
    one_pass(1)


def alloc_na(b, c):
    xflat = c.xblk[:].rearrange("p a k t -> p (a k t)")
    sflat = c.stg[:].rearrange("p a f -> p (a f)")
    c.nqT = xflat[0:32, 0:4096]
    c.nkT = xflat[0:32, 4096:8192]
    c.nv = c.hT[:].rearrange("p a t -> p (a t)")[0:64, 0:2048].rearrange("p (r e) -> p r e", e=32)
    c.no = sflat[0:64, 0:2048].rearrange("p (r e) -> p r e", e=32)
    c.nbias = sflat[0:64, 2048:3008].rearrange("p (a j) -> p a j", j=64)
    c.nsc = sflat[0:64, 4096:4608]
    c.nP = b.sb("nP", [64, 512], BF16)
    c.nPT = b.sb("nPT", [64, 8, 64], BF16)
    c.nmx = b.sb("nmx", [64, 1], F32)
    c.nsm = b.sb("nsm", [64, 1], F32)
    c.identb = b.sb("identb", [128, 128], BF16)
    c.qkb = c.hl[:].bitcast(BF16)
    c.psTb = b.ps("psTb", [128, 1024], BF16)


def na_layer(b, c, src_d, dst_d, w, ntok, L):
    nc = b.nc
    nseq = ntok // L
    tps = L // 128
    R = L // 64
    src_t = src_d.rearrange("(n p) d -> n p d", p=128)
    dst_t = dst_d.rearrange("(n p) d -> n p d", p=128)
    o_t = c.acc_d.rearrange("(n p) d -> n p d", p=128)
    o_h = c.acc_d.rearrange("(s r p) (h e) -> s h p r e", r=R, p=64, e=32)
    qT_d = c.qT_d.rearrange("g (a e) t -> (g a) e t", e=32)
    kT_d = c.kT_d.rearrange("g (a e) t -> (g a) e t", e=32)
    qT_w = c.qT_d.rearrange("g p (n t) -> n g p t", t=128)
    kT_w = c.kT_d.rearrange("g p (n t) -> n g p t", t=128)
    v_w = c.v_d.rearrange("(n p) d -> n p d", p=128)
    v_h = c.v_d.rearrange("(s r p) (h e) -> s h p r e", r=R, p=64, e=32)
    load_cast_rows(b, c, c.wgu, w["w_in"][:, 0:2048], 2048)
    load_cast_rows(b, c, c.wmB, w["w_in"][:, 2048:3072], 1024)
    load_cast_rows(b, c, c.wd, w["w_out"], 1024)
    load_ln(b, c, w["ln1_g"], w["ln1_b"])
    bin_bc = c.acc[:, 0:3, :].rearrange("p a d -> p (a d)")
    bout_bc = c.acc[:, 3, :]
    pq = c.acc[:, 4, :]
    b.dma(bin_bc, w["b_in"].partition_broadcast(128))
    b.dma(bout_bc, w["b_out"].partition_broadcast(128))
    b.V(lambda: nc.vector.tensor_copy(c.identb[:], c.ident[:]))

    def p1(t):
        b.dma(c.xt[:], src_t[t])
        x_to_xT(b, c)
        for part in range(3):
            for hh in range(2):
                c0 = part * 1024 + hh * 512
                wbf, wc0 = (c.wgu, c0) if c0 < 2048 else (c.wmB, c0 - 2048)
                b.mm(mm_group(nc, c.psA[:], [(c.xTb[:, kk, :], wbf[:, kk, wc0:wc0 + 512]) for kk in range(8)]))
                b.V(lambda hh=hh, c0=c0: nc.vector.tensor_tensor(out=pq[:, hh * 512:(hh + 1) * 512], in0=c.psA[:],
                                                               in1=bin_bc[:, c0:c0 + 512], op=ALU.add))
            if part == 0:
                b.V(lambda: nc.vector.tensor_scalar(out=c.qkb, in0=pq, scalar1=32.0 ** -0.5, scalar2=None, op0=ALU.mult))
            else:
                b.V(lambda: nc.vector.tensor_copy(c.qkb, pq))
            if part == 2:
                b.dma(v_w[t], c.qkb)
            else:
                for g in range(8):
                    b.mm([lambda g=g: nc.tensor.transpose(c.psTb[:, g * 128:(g + 1) * 128], c.qkb[:, g * 128:(g + 1) * 128], c.identb[:])])
                b.V(lambda: nc.vector.tensor_copy(c.vbf[:], c.psTb[:]))
                dstw = qT_w if part == 0 else kT_w
                b.dma(dstw[t].rearrange("g p t -> p g t"), c.vbf[:].rearrange("p (g t) -> p g t", g=8))

    b.loop(ntok // 128, p1, eng=nc.sync)

    def seq(si):
        def head(h):
            b.dma(c.nqT[:, 0:L], qT_d[h].rearrange("e (s t) -> s e t", t=L)[si])
            b.dma(c.nkT[:, 0:L], kT_d[h].rearrange("e (s t) -> s e t", t=L)[si])
            b.dma(c.nv[:, 0:R, :], v_h[si][h])
            b.dma(c.nbias, w["bias2"][h])
            for r in range(R):
                rs = min(max(r - 4, 0), R - 8)
                dr0 = rs - r + 7
                b.mm([lambda r=r, rs=rs: nc.tensor.matmul(c.psA[0:64, :], c.nqT[:, r * 64:(r + 1) * 64], c.nkT[:, rs * 64:rs * 64 + 512],
                                                          start=True, stop=True)])
                b.V(lambda dr0=dr0: nc.vector.tensor_tensor(out=c.nsc, in0=c.psA[0:64, :],
                                                            in1=c.nbias[:, dr0:dr0 + 8, :].rearrange("p a j -> p (a j)"), op=ALU.add))
                b.V(lambda: nc.vector.reduce_max(out=c.nmx[:], in_=c.nsc, axis=AX.X))
                b.V(lambda: nc.vector.tensor_scalar(out=c.nmx[:], in0=c.nmx[:], scalar1=-1.0, scalar2=None, op0=ALU.mult))
                b.A(lambda: nc.scalar.activation(out=c.nsc, in_=c.nsc, func=AF.Exp, bias=c.nmx[:, 0:1], scale=1.0))
                b.V(lambda: nc.vector.reduce_sum(out=c.nsm[:], in_=c.nsc, axis=AX.X))
                b.V(lambda: nc.vector.reciprocal(out=c.nsm[:], in_=c.nsm[:]))
                b.V(lambda: nc.vector.tensor_copy(c.nP[:], c.nsc))
                for a in range(8):
                    b.mm([lambda a=a: nc.tensor.transpose(c.psTb[0:64, a * 64:(a + 1) * 64], c.nP[:, a * 64:(a + 1) * 64], c.identb[0:64, 0:64])])
                b.V(lambda: nc.vector.tensor_copy(c.nPT[:].rearrange("p a q -> p (a q)"), c.psTb[0:64, 0:512]))
                b.mm(mm_group(nc, c.psB[0:64, 0:32], [(c.nPT[:, a, :], c.nv[:, rs + a, :]) for a in range(8)]))
                b.V(lambda r=r: nc.vector.tensor_scalar(out=c.no[:, r, :], in0=c.psB[0:64, 0:32], scalar1=c.nsm[:, 0:1], scalar2=None, op0=ALU.mult))
            b.dma(o_h[si][h], c.no[:, 0:R, :])

        b.loop(32, head)

    b.loop(nseq, seq, eng=nc.scalar)

    def p3(t):
        b.dma(c.xt[:], src_t[t])
        b.dma(pq, o_t[t])
        out_proj_ln(b, c, pq, c.wd, bout_bc, dst_t[t])

    b.loop(ntok // 128, p3, eng=nc.sync)


def hyena_consts(L):
    import ml_dtypes
    N = 2 * L
    nfc = L // 128 + 1
    NF = nfc * 128
    t = np.arange(L, dtype=np.int64)
    f = np.arange(NF, dtype=np.int64)
    ang = 2.0 * np.pi * ((t[:, None] * f[None, :]) % N).astype(np.float64) / N
    cs, sn = np.cos(ang), np.sin(ang)
    wf = np.where((f == 0) | (f == L), 1.0, 2.0) / N
    wf[f > L] = 0.0
    bf = ml_dtypes.bfloat16
    bf = ml_dtypes.bfloat16
    tps = L // 128

    def fwd_blk(m):
        return np.ascontiguousarray(m.reshape(tps, 128, nfc, 128).transpose(2, 1, 0, 3)).astype(np.float32).astype(bf)

    def inv_blk(m):
        return np.ascontiguousarray(m.reshape(nfc, 128, tps, 128).transpose(2, 1, 0, 3)).astype(np.float32).astype(bf)

    out = {"hy_dft_c": fwd_blk(cs), "hy_dft_s": fwd_blk(sn),
           "hy_idft_c": inv_blk((cs * wf[None, :]).T), "hy_idft_s": inv_blk((sn * wf[None, :]).T)}
    tf = np.arange(L, dtype=np.float32)
    t_norm = tf / np.float32(max(L - 1, 1))
    freqs = np.linspace(1e-4, 15, 16, dtype=np.float32)
    a2 = (np.float32(2.0 * math.pi / L) * tf[:, None] * freqs[None, :]).astype(np.float32)
    pe = np.concatenate([t_norm[:, None], np.cos(a2), -np.sin(a2)], axis=-1).astype(np.float32)
    out["hy_peT"] = np.ascontiguousarray(pe.T)
    min_decay = math.log(1e-2) / 1.5
    max_decay = math.log(1e-2) / 0.3
    deltas = np.abs(np.linspace(min_decay, max_decay, D, dtype=np.float32))
    out["hy_win"] = (np.exp(-t_norm[:, None] * deltas[None, :]) + np.float32(0.05)).astype(np.float32)
    return out


def hyena_layer(b, c, src_d, dst_d, w, ntok, L):
    nc = b.nc
    nseq = ntok // L
    tps = L // 128
    nfc = tps + 1
    CW = 256
    nct = D // CW
    src_t = src_d.rearrange("(s n p) d -> s n p d", p=128, n=tps)
    dst_t = dst_d.rearrange("(s n p) d -> s n p d", p=128, n=tps)
    accf = c.acc[:].rearrange("p a d -> p (a d)")
    stgf = c.stg[:].rearrange("p a f -> p (a f)")
    accb = accf.bitcast(BF16)
    stgb = stgf.bitcast(BF16)
    zt = c.xblk[:].rearrange("p a k t -> p (a k t)")[:, 0:tps * CW].rearrange("p (t c) -> p t c", c=CW)
    hv = c.hv

    w1 = c.xt[0:33, 0:64]
    w2 = c.xt[0:64, 64:128]
    w3 = accf[0:64, 0:4096]
    peT = accf[0:33, 4096:4096 + L]
    b.dma(w1, w["ffn_w1"])
    b.dma(w2, w["ffn_w2"])
    b.dma(w3, w["ffn_w3"])
    b.dma(peT, w["peT"])
    b.dma(hv[0:64, 0:1], w["ffn_b1"].rearrange("(p o) -> p o", o=1))
    b.dma(hv[0:64, 1:2], w["ffn_b2"].rearrange("(p o) -> p o", o=1))
    b.dma(hv[0:64, 2:3], w["sin_freq"][0].rearrange("(p o) -> p o", o=1))
    b.dma(hv[0:64, 3:4], w["sin_freq"][1].rearrange("(p o) -> p o", o=1))
    for i in range(2):
        b.V(lambda i=i: nc.vector.tensor_scalar(out=hv[0:64, 4 + i:5 + i], in0=hv[0:64, 2 + i:3 + i], scalar1=0.125, scalar2=None, op0=ALU.mult))
        b.V(lambda i=i: nc.vector.tensor_tensor(out=hv[0:64, 6 + i:7 + i], in0=hv[0:64, 4 + i:5 + i], in1=hv[0:64, i:i + 1], op=ALU.mult))
        b.V(lambda i=i: nc.vector.tensor_scalar(out=hv[0:64, 8 + i:9 + i], in0=hv[0:64, 6 + i:7 + i], scalar1=math.pi / 2, scalar2=None, op0=ALU.add))
    m0 = hv[:, 10:11]
    b.V(lambda: nc.vector.tensor_scalar(out=m0, in0=c.ident[:, 0:1], scalar1=-1.0, scalar2=1.0, op0=ALU.mult, op1=ALU.add))

    def sin_layer(ps, i, out):
        S, C, T = out, c.hl[0:64, :], c.yt[0:64, 0:512]
        b.A(lambda: nc.scalar.activation(out=S, in_=ps, func=AF.Sin, bias=hv[0:64, 6 + i:7 + i], scale=hv[0:64, 4 + i:5 + i]))
        b.A(lambda: nc.scalar.activation(out=C, in_=ps, func=AF.Sin, bias=hv[0:64, 8 + i:9 + i], scale=hv[0:64, 4 + i:5 + i]))
        for _ in range(3):
            b.V(lambda: nc.vector.tensor_tensor(out=T, in0=S, in1=S, op=ALU.mult))
            b.V(lambda: nc.vector.scalar_tensor_tensor(out=S, in0=S, scalar=2.0, in1=C, op0=ALU.mult, op1=ALU.mult))
            b.V(lambda: nc.vector.tensor_scalar(out=C, in0=T, scalar1=-2.0, scalar2=1.0, op0=ALU.mult, op1=ALU.add))

    h3w = stgf[:, 0:4096]
    wint = stgf[:, 4096:5120]
    for q in range(L // 512):
        b.mm([lambda q=q: nc.tensor.matmul(c.psA[0:64, :], w1, peT[:, q * 512:(q + 1) * 512], start=True, stop=True)])
        sin_layer(c.psA[0:64, :], 0, c.hg[0:64, :])
        b.mm([lambda: nc.tensor.matmul(c.psB[0:64, :], w2, c.hg[0:64, :], start=True, stop=True)])
        sin_layer(c.psB[0:64, :], 1, c.hs[0:64, :])
        for tt in range(4):
            t = q * 4 + tt
            b.dma(wint, w["win"][t * 128:(t + 1) * 128, :])
            for cb in range(8):
                b.mm([lambda tt=tt, cb=cb: nc.tensor.matmul(c.psC[:], c.hs[0:64, tt * 128:(tt + 1) * 128], w3[:, cb * 512:(cb + 1) * 512],
                                                         start=True, stop=True)])
                b.V(lambda cb=cb: nc.vector.tensor_tensor(out=h3w[:, cb * 512:(cb + 1) * 512], in0=c.psC[:],
                                                         in1=wint[:, (cb % 2) * 512:(cb % 2 + 1) * 512], op=ALU.mult))
            for o in range(2):
                hf = h3w[:, o * 2048:o * 2048 + 1024]
                hb = h3w[:, o * 2048 + 1024:(o + 1) * 2048]
                if t == 0:
                    b.V(lambda hb=hb: nc.vector.tensor_scalar(out=hb, in0=hb, scalar1=m0, scalar2=None, op0=ALU.mult))
                b.V(lambda hf=hf, hb=hb: nc.vector.tensor_tensor(out=c.vbf[:], in0=hf, in1=hb, op=ALU.add))
                b.dma(c.hk_d[o][0][t], c.vbf[:])
                b.V(lambda hf=hf, hb=hb: nc.vector.tensor_tensor(out=c.vbf[:], in0=hf, in1=hb, op=ALU.subtract))
                b.dma(c.hk_d[o][1][t], c.vbf[:])

    dcv, dsv, icv, isv = w["dft_c"], w["dft_s"], w["idft_c"], w["idft_s"]
    dcb = accb[:, 8448:8448 + tps * 128].rearrange("p (t f) -> p t f", f=128)
    dsb = stgb[:, 8448:8448 + tps * 128].rearrange("p (t f) -> p t f", f=128)
    icb = accb[:, 8448:8448 + nfc * 128].rearrange("p (a t) -> p a t", t=128)
    isb = stgb[:, 8448:8448 + nfc * 128].rearrange("p (a t) -> p a t", t=128)
    Yr = accb[:, 0:nfc * CW].rearrange("p (a c) -> p a c", c=CW)
    Ys = stgb[:, 0:nfc * CW].rearrange("p (a c) -> p a c", c=CW)
    zt2 = accb[:, 0:tps * CW].rearrange("p (t c) -> p t c", c=CW)
    for o in range(2):
        for ct in range(nct):
            cs_ = slice(ct * CW, (ct + 1) * CW)
            b.dma(zt, c.hk_d[o][0].rearrange("t p c -> p t c")[:, :, cs_])
            b.dma(zt2, c.hk_d[o][1].rearrange("t p c -> p t c")[:, :, cs_])
            for fc in range(nfc):
                b.dma(dcb, dcv[fc])
                b.dma(dsb, dsv[fc])
                b.mm(mm_group(nc, c.psA[:, 0:CW], [(dcb[:, tc, :], zt[:, tc, :]) for tc in range(tps)]))
                b.mm(mm_group(nc, c.psB[:, 0:CW], [(dsb[:, tc, :], zt2[:, tc, :]) for tc in range(tps)]))
                b.V(lambda: nc.vector.tensor_copy(c.hg[:, 0:CW], c.psA[:, 0:CW]))
                b.V(lambda: nc.vector.tensor_copy(c.hg[:, CW:2 * CW], c.psB[:, 0:CW]))
                b.dma(c.K_d[o][0][fc][:, cs_], c.hg[:, 0:CW])
                b.dma(c.K_d[o][1][fc][:, cs_], c.hg[:, CW:2 * CW])

    load_cast_rows(b, c, c.wgu, w["w_in"][:, 0:2048], 2048)
    load_cast_rows(b, c, c.wmB, w["w_in"][:, 2048:3072], 1024)
    load_cast_rows(b, c, c.wd, w["w_out"], 1024)
    b.G(lambda: nc.gpsimd.memset(c.yt[:], 0.0))
    for j3 in range(3):
        b.dma(c.p_d[0:1, j3 * 1024:(j3 + 1) * 1024], c.yt[0:1, :])
        b.dma(c.p_d[L + 1:L + 2, j3 * 1024:(j3 + 1) * 1024], c.yt[0:1, :])
    p_rows = c.p_d[1:L + 1, :].rearrange("(n p) c -> n p c", p=128)
    p_m = c.p_d[0:L, :].rearrange("(n p) c -> n p c", p=128)
    p_p = c.p_d[2:L + 2, :].rearrange("(n p) c -> n p c", p=128)

    def conv(o, in_d, gate_d, out_d, out_bf):
        b.dma(c.gbc[:], w["filt_bias"][o].partition_broadcast(128))
        in_v = in_d.rearrange("t p c -> p t c")
        for ct in range(nct):
            cs_ = slice(ct * CW, (ct + 1) * CW)
            b.dma(zt, in_v[:, :, cs_])
            for fc in range(nfc):
                b.dma(dcb, dcv[fc])
                b.dma(dsb, dsv[fc])
                b.dma(c.hg[:, 0:CW], c.K_d[o][0][fc][:, cs_])
                b.dma(c.hg[:, CW:2 * CW], c.K_d[o][1][fc][:, cs_])
                b.mm(mm_group(nc, c.psA[:, 0:CW], [(dcb[:, tc, :], zt[:, tc, :]) for tc in range(tps)]))
                b.mm(mm_group(nc, c.psB[:, 0:CW], [(dsb[:, tc, :], zt[:, tc, :]) for tc in range(tps)]))
                b.V(lambda: nc.vector.tensor_tensor(out=c.hs[:, 0:CW], in0=c.psA[:, 0:CW], in1=c.hg[:, 0:CW], op=ALU.mult))
                b.V(lambda: nc.vector.tensor_tensor(out=c.hs[:, CW:2 * CW], in0=c.psB[:, 0:CW], in1=c.hg[:, CW:2 * CW], op=ALU.mult))
                b.V(lambda fc=fc: nc.vector.tensor_tensor(out=Yr[:, fc, :], in0=c.hs[:, 0:CW], in1=c.hs[:, CW:2 * CW], op=ALU.subtract))
                b.V(lambda: nc.vector.tensor_tensor(out=c.hs[:, 0:CW], in0=c.psA[:, 0:CW], in1=c.hg[:, CW:2 * CW], op=ALU.mult))
                b.V(lambda: nc.vector.tensor_tensor(out=c.hs[:, CW:2 * CW], in0=c.psB[:, 0:CW], in1=c.hg[:, 0:CW], op=ALU.mult))
                b.V(lambda fc=fc: nc.vector.tensor_tensor(out=Ys[:, fc, :], in0=c.hs[:, 0:CW], in1=c.hs[:, CW:2 * CW], op=ALU.add))
            for tc in range(tps):
                b.dma(icb, icv[tc])
                b.dma(isb, isv[tc])
                b.dma(c.hl[:, 0:CW], gate_d[tc][:, cs_])
                pairs = []
                for fc in range(nfc):
                    pairs.append((icb[:, fc, :], Yr[:, fc, :]))
                    pairs.append((isb[:, fc, :], Ys[:, fc, :]))
                b.mm(mm_group(nc, c.psC[:, 0:CW], pairs))
                b.V(lambda tc=tc, cs_=cs_: nc.vector.tensor_tensor(out=c.hl[:, CW:2 * CW], in0=zt[:, tc, :], in1=c.gbc[:, cs_], op=ALU.mult))
                b.V(lambda: nc.vector.tensor_tensor(out=c.hl[:, CW:2 * CW], in0=c.psC[:, 0:CW], in1=c.hl[:, CW:2 * CW], op=ALU.add))
                if out_bf:
                    b.V(lambda: nc.vector.tensor_tensor(out=c.vbf[:, 0:CW], in0=c.hl[:, CW:2 * CW], in1=c.hl[:, 0:CW], op=ALU.mult))
                    b.dma(out_d[tc][:, cs_], c.vbf[:, 0:CW])
                else:
                    b.V(lambda: nc.vector.tensor_tensor(out=c.hs[:, 0:CW], in0=c.hl[:, CW:2 * CW], in1=c.hl[:, 0:CW], op=ALU.mult))
                    b.dma(out_d[tc][:, cs_], c.hs[:, 0:CW])

    def seq(si):
        bin_bc = accf[:, 0:3072]
        pt = accf[:, 3072:6144]
        b.dma(bin_bc, w["b_in"].partition_broadcast(128))

        def tA(ti):
            b.dma(c.xt[:], src_t[si][ti])
            x_to_xT(b, c)
            for j in range(6):
                c0 = j * 512
                wbf, wc0 = (c.wgu, c0) if c0 < 2048 else (c.wmB, c0 - 2048)
                b.mm(mm_group(nc, c.psA[:], [(c.xTb[:, kk, :], wbf[:, kk, wc0:wc0 + 512]) for kk in range(8)]))
                b.V(lambda c0=c0: nc.vector.tensor_tensor(out=pt[:, c0:c0 + 512], in0=c.psA[:], in1=bin_bc[:, c0:c0 + 512], op=ALU.add))
            b.dma(p_rows[ti], pt)

        b.loop(tps, tA, eng=nc.scalar)

        def tB(ti):
            pm = accf[:, 0:3072]
            p0 = accf[:, 3072:6144]
            pp = stgf[:, 0:3072]
            cw = stgf[:, 3072:5120].rearrange("p (a f) -> p a f", a=4)
            tmp = stgf[:, 5120:5632]
            b.dma(pm, p_m[ti])
            b.dma(p0, p_rows[ti])
            b.dma(pp, p_p[ti])
            for j in range(6):
                cs_ = slice(j * 512, (j + 1) * 512)
                for a in range(3):
                    b.dma(cw[:, a, :], w["conv_w"][a][cs_].partition_broadcast(128))
                b.dma(cw[:, 3, :], w["conv_b"][cs_].partition_broadcast(128))
                b.V(lambda cs_=cs_: nc.vector.tensor_tensor(out=tmp, in0=pm[:, cs_], in1=cw[:, 0, :], op=ALU.mult))
                b.V(lambda cs_=cs_: nc.vector.tensor_tensor(out=p0[:, cs_], in0=p0[:, cs_], in1=cw[:, 1, :], op=ALU.mult))
                b.V(lambda cs_=cs_: nc.vector.tensor_tensor(out=p0[:, cs_], in0=p0[:, cs_], in1=tmp, op=ALU.add))
                b.V(lambda cs_=cs_: nc.vector.tensor_tensor(out=tmp, in0=pp[:, cs_], in1=cw[:, 2, :], op=ALU.mult))
                b.V(lambda cs_=cs_: nc.vector.tensor_tensor(out=p0[:, cs_], in0=p0[:, cs_], in1=tmp, op=ALU.add))
                b.V(lambda cs_=cs_: nc.vector.tensor_tensor(out=p0[:, cs_], in0=p0[:, cs_], in1=cw[:, 3, :], op=ALU.add))
            b.V(lambda: nc.vector.tensor_copy(c.vbf[:], p0[:, 0:1024]))
            b.dma(c.hv_d[ti], c.vbf[:])
            b.dma(c.hx_d[0][ti], p0[:, 1024:2048])
            b.dma(c.hx_d[1][ti], p0[:, 2048:3072])

        for ti_ in range(tps):
            tB(ti_)
        conv(0, c.hv_d, c.hx_d[0], c.hz_d, True)
        conv(1, c.hz_d, c.hx_d[1], c.hx_d[2], False)
        load_ln(b, c, w["ln1_g"], w["ln1_b"])
        bout_bc = c.acc[:, 1, :]
        b.dma(bout_bc, w["b_out"].partition_broadcast(128))

        def tC(ti):
            b.dma(c.xt[:], src_t[si][ti])
            b.dma(c.acc[:, 0, :], c.hx_d[2][ti])
            out_proj_ln(b, c, c.acc[:, 0, :], c.wd, bout_bc, dst_t[si][ti])

        b.loop(tps, tC, eng=nc.scalar)

    b.loop(nseq, seq, eng=nc.scalar)


def build_program(ntok, nlayers=DEPTH, wl=DEPTH, L=4096, mixers=(0, 1, 2, 0), do_moe=True):
    nc = bass.Bass("TRN2", target_bir_lowering=False)
    es = ExitStack()
    c = Ctx()

    def inp(name, shape, dt=F32):
        return nc.dram_tensor(name, list(shape), dt, kind="ExternalInput").ap()

    x = inp("x", [ntok, D])
    W = {}
    ident = inp("ident", [128, 128])
    tri = inp("tri", [128, 3, 128])
    nA = sum(1 for m in mixers[:nlayers] if m == 0)
    if nA:
        W.update({"gla_w_in": inp("gla_w_in", [nA, D, 3104]), "gla_gate_w2": inp("gla_gate_w2", [nA, 2, 16, 512]),
                  "gla_gate_b": inp("gla_gate_b", [nA, 2, 512]), "gla_norm_g": inp("gla_norm_g", [nA, 256]),
                  "gla_w_out": inp("gla_w_out", [nA, D, D])})
    nB = sum(1 for m in mixers[:nlayers] if m == 1)
    if nB:
        tps_ = L // 128
        nfc_ = tps_ + 1
        W.update({"hy_w_in": inp("hy_w_in", [nB, D, 3 * D]), "hy_b_in": inp("hy_b_in", [nB, 3 * D]),
                  "hy_conv_w": inp("hy_conv_w", [nB, 3, 3 * D]), "hy_conv_b": inp("hy_conv_b", [nB, 3 * D]),
                  "hy_ffn_w1": inp("hy_ffn_w1", [nB, 33, 64]), "hy_ffn_b1": inp("hy_ffn_b1", [nB, 64]),
                  "hy_sin_freq": inp("hy_sin_freq", [nB, 2, 64]), "hy_ffn_w2": inp("hy_ffn_w2", [nB, 64, 64]),
                  "hy_ffn_b2": inp("hy_ffn_b2", [nB, 64]), "hy_ffn_w3": inp("hy_ffn_w3", [nB, 64, 4 * D]),
                  "hy_filt_bias": inp("hy_filt_bias", [nB, 2, D]), "hy_w_out": inp("hy_w_out", [nB, D, D]),
                  "hy_b_out": inp("hy_b_out", [nB, D]),
                  "hy_peT": inp("hy_peT", [33, L]), "hy_win": inp("hy_win", [L, D]),
                  "hy_dft_c": inp("hy_dft_c", [nfc_, 128, tps_, 128], BF16), "hy_dft_s": inp("hy_dft_s", [nfc_, 128, tps_, 128], BF16),
                  "hy_idft_c": inp("hy_idft_c", [tps_, 128, nfc_, 128], BF16), "hy_idft_s": inp("hy_idft_s", [tps_, 128, nfc_, 128], BF16)})
        c.hk_d = nc.dram_tensor("hk_d", [2, 2, tps_, 128, D], BF16, kind="Internal").ap()
        c.K_d = nc.dram_tensor("K_d", [2, 2, nfc_, 128, D], F32, kind="Internal").ap()
        c.p_d = nc.dram_tensor("p_d", [L + 2, 3 * D], F32, kind="Internal").ap()
        c.hv_d = nc.dram_tensor("hv_d", [tps_, 128, D], BF16, kind="Internal").ap()
        c.hz_d = nc.dram_tensor("hz_d", [tps_, 128, D], BF16, kind="Internal").ap()
        c.hx_d = nc.dram_tensor("hx_d", [3, tps_, 128, D], F32, kind="Internal").ap()
    nC = sum(1 for m in mixers[:nlayers] if m == 2)
    if nC:
        W.update({"na_w_in": inp("na_w_in", [nC, D, 3 * D]), "na_b_in": inp("na_b_in", [nC, 3 * D]),
                  "na_bias2": inp("na_bias2", [nC, 32, 64, 15, 64]), "na_w_out": inp("na_w_out", [nC, D, D]),
                  "na_b_out": inp("na_b_out", [nC, D])})
        c.qT_d = nc.dram_tensor("qT_d", [8, 128, ntok], BF16, kind="Internal").ap()
        c.kT_d = nc.dram_tensor("kT_d", [8, 128, ntok], BF16, kind="Internal").ap()
        c.v_d = nc.dram_tensor("v_d", [ntok, D], BF16, kind="Internal").ap()
    W.update({
        "ln1_g": inp("ln1_g", [wl, D]), "ln1_b": inp("ln1_b", [wl, D]),
        "ln2_g": inp("ln2_g", [wl, D]), "ln2_b": inp("ln2_b", [wl, D]),
        "moe_router_w": inp("moe_router_w", [wl, D, NE]), "moe_router_b": inp("moe_router_b", [wl, NE]),
        "moe_w_gu": inp("moe_w_gu", [wl, NE, D, 2 * D]), "moe_b_gu": inp("moe_b_gu", [wl, NE, 2 * D]),
        "moe_w_down": inp("moe_w_down", [wl, NE, D, D]), "moe_b_down": inp("moe_b_down", [wl, NE, D]),
    })
    y = nc.dram_tensor("y", [ntok, D], F32, kind="ExternalOutput").ap()
    c.xa_d = nc.dram_tensor("xa_d", [ntok, D], F32, kind="Internal").ap()
    c.xb_d = nc.dram_tensor("xb_d", [ntok, D], F32, kind="Internal").ap()
    c.acc_d = nc.dram_tensor("acc_d", [ntok, D], F32, kind="Internal").ap()
    c.xT_d = nc.dram_tensor("xT_d", [ntok // 128, 128, 8, 128], BF16, kind="Internal").ap()
    c.comb_d = nc.dram_tensor("comb_d", [ntok // 128, 128, D], F32, kind="Internal").ap()

    with es:
        b = Builder(nc, es)
        alloc_common(b, c)
        alloc_moe(b, c)
        alloc_mixer(b, c)
        if nC:
            alloc_na(b, c)
        b.dma(c.ident[:], ident)
        b.dma(c.tri[:], tri)
        cur = x
        cnt = [0, 0, 0]
        for li in range(nlayers):
            m = mixers[li]
            j = cnt[m]
            cnt[m] += 1
            mdst = c.xb_d if do_moe else (y if li == nlayers - 1 else c.xa_d)
            if m == 0:
                gw = {"w_in": W["gla_w_in"][j], "gate_w2": W["gla_gate_w2"][j], "gate_b": W["gla_gate_b"][j],
                      "norm_g": W["gla_norm_g"][j], "w_out": W["gla_w_out"][j], "ln1_g": W["ln1_g"][li], "ln1_b": W["ln1_b"][li]}
                gla_layer(b, c, cur, mdst, gw, ntok, L)
            elif m == 1:
                hw = {"w_in": W["hy_w_in"][j], "b_in": W["hy_b_in"][j], "conv_w": W["hy_conv_w"][j], "conv_b": W["hy_conv_b"][j],
                      "ffn_w1": W["hy_ffn_w1"][j], "ffn_b1": W["hy_ffn_b1"][j], "sin_freq": W["hy_sin_freq"][j],
                      "ffn_w2": W["hy_ffn_w2"][j], "ffn_b2": W["hy_ffn_b2"][j], "ffn_w3": W["hy_ffn_w3"][j],
                      "filt_bias": W["hy_filt_bias"][j], "w_out": W["hy_w_out"][j], "b_out": W["hy_b_out"][j],
                      "peT": W["hy_peT"], "win": W["hy_win"], "dft_c": W["hy_dft_c"], "dft_s": W["hy_dft_s"],
                      "idft_c": W["hy_idft_c"], "idft_s": W["hy_idft_s"], "ln1_g": W["ln1_g"][li], "ln1_b": W["ln1_b"][li]}
                hyena_layer(b, c, cur, mdst, hw, ntok, L)
            elif m == 2:
                nw = {"w_in": W["na_w_in"][j], "b_in": W["na_b_in"][j], "bias2": W["na_bias2"][j], "w_out": W["na_w_out"][j],
                      "b_out": W["na_b_out"][j], "ln1_g": W["ln1_g"][li], "ln1_b": W["ln1_b"][li]}
                na_layer(b, c, cur, mdst, nw, ntok, L)
            elif m == -1:
                mdst = cur
            else:
                raise NotImplementedError("Hyena mixer (FFT long convolution) is not implemented in this version")
            if not do_moe:
                cur = c.xa_d
                continue
            mo_in = mdst
            w = {"router_w": W["moe_router_w"][li], "router_b": W["moe_router_b"][li],
                 "w_gu": W["moe_w_gu"][li], "b_gu": W["moe_b_gu"][li],
                 "w_down": W["moe_w_down"][li], "b_down": W["moe_b_down"][li],
                 "ln2_g": W["ln2_g"][li], "ln2_b": W["ln2_b"][li]}
            dst = y if li == nlayers - 1 else c.xa_d
            moe_layer(b, c, li, mo_in, dst, w, ntok)
            cur = c.xa_d
        b.finish()
    return nc


def na_bias_layout(rpb):
    n, H = rpb.shape[0], rpb.shape[1]
    cq = np.arange(64)[:, None]
    jk = np.arange(64)[None, :]
    cs = np.clip(cq - 8, 0, 48)
    inwin = (jk >= cs) & (jk < cs + 16)
    dc = np.clip(jk - cq + 15, 0, 30)
    g = rpb[:, :, :, dc]
    g = np.transpose(g, (0, 1, 3, 2, 4))
    out = np.where(inwin[None, None, :, None, :], g, np.float32(-30000.0)).astype(np.float32)
    return np.ascontiguousarray(out)


NCORES = 4
SEQ = 4096
BATCH = 16


def kernel(**inputs):
    n = NCORES
    ntok = BATCH * SEQ // n
    nc = build_program(ntok, nlayers=DEPTH, wl=DEPTH, L=SEQ, mixers=(0, 1, 2, 0), do_moe=True)
    s_ = np.arange(128)[:, None]
    t_ = np.arange(128)[None, :]
    tri = np.stack([(s_ <= t_), (s_ >= t_), (s_ > t_)], 1).astype(np.float32)
    shared = {k: np.ascontiguousarray(np.asarray(v, dtype=np.float32)) for k, v in inputs.items()
              if k.startswith(("ln", "moe_", "gla_", "hy_")) or k in ("na_w_in", "na_b_in", "na_w_out", "na_b_out")}
    shared.update(hyena_consts(SEQ))
    shared["na_bias2"] = na_bias_layout(np.asarray(inputs["na_rpb"], dtype=np.float32))
    shared["ident"] = np.eye(128, dtype=np.float32)
    shared["tri"] = tri
    x = np.asarray(inputs["x"], dtype=np.float32).reshape(n, ntok, D)
    in_maps = [dict(shared, x=np.ascontiguousarray(x[i])) for i in range(n)]
    res = run_bass_kernel_spmd(nc, in_maps, core_ids=list(range(n)))
    out = np.concatenate([res.results[i]["y"] for i in range(n)], axis=0)
    return out.reshape(BATCH, SEQ, D).astype(np.float32)
```

```python
import math
from contextlib import ExitStack
import numpy as np
import concourse.bass as bass
import concourse.mybir as mybir
from concourse.bass_utils import run_bass_kernel_spmd

F32 = mybir.dt.float32
BF16 = mybir.dt.bfloat16
AF = mybir.ActivationFunctionType
ALU = mybir.AluOpType
AX = mybir.AxisListType

D = 1024
DEPTH = 4
ALPHA = (2 * DEPTH) ** 0.25
LN_EPS = 1e-5
NE = 32
TOPK = 4


FORCE_SYNC = False
MOE_NEXP = 32


class Builder:
    def __init__(self, nc, es):
        self.nc = nc
        self.es = es
        self.levels = []
        self.depth = 0
        self._level(0)
        self.g = 0
        self.d = 0
        self.stack = []
        self.dummy = es.enter_context(nc.sbuf_tensor("sb_dummy", [128, 8], F32))
        self.engs = [nc.tensor, nc.vector, nc.scalar, nc.gpsimd, nc.sync]
        self.waited = {}
        self.a = 0
        self.a_need = 0
        self.dma_rr = 0
        self.nloops = 0
        self.loop_eng = nc.sync
        self.inloop = False

    def _level(self, k):
        while len(self.levels) <= k:
            n = len(self.levels)
            gs = self.es.enter_context(self.nc.semaphore(f"gsem{n}"))
            ds = self.es.enter_context(self.nc.semaphore(f"dsem{n}"))
            it = self.es.enter_context(self.nc.semaphore(f"isem{n}"))
            asm = self.es.enter_context(self.nc.semaphore(f"asem{n}"))
            self.levels.append((gs, ds, it, asm))
        return self.levels[k]

    def sb(self, name, shape, dt=F32):
        return self.es.enter_context(self.nc.sbuf_tensor("sb_" + name, list(shape), dt))

    def ps(self, name, shape, dt=F32):
        return self.es.enter_context(self.nc.psum_tensor("ps_" + name, list(shape), dt))

    def _wait(self, eng):
        gs, ds, _, asm = self.levels[self.depth]
        key = id(eng)
        lg, ld, la = self.waited.get(key, (-1, -1, 0))
        if self.g > lg:
            eng.wait_ge(gs, self.g)
        if self.d > ld:
            eng.wait_ge(ds, self.d)
        if self.a_need > la:
            eng.wait_ge(asm, self.a_need)
        self.waited[key] = (max(lg, self.g), max(ld, self.d), max(la, self.a_need))

    def dma_async(self, out, in_):
        eng = self.loop_eng if self.inloop else self.nc.sync
        self._wait(eng)
        eng.dma_start(out=out, in_=in_).then_inc(self.levels[self.depth][3], 16)
        self.a += 16
        return self.a

    def need(self, tok):
        self.a_need = max(self.a_need, tok)

    def op(self, eng, fn):
        self._wait(eng)
        fn().then_inc(self.levels[self.depth][0], 1)
        self.g += 1

    def V(self, fn):
        self.op(self.nc.vector, fn)

    def A(self, fn):
        self.op(self.nc.scalar, fn)

    def G(self, fn):
        self.op(self.nc.gpsimd, fn)

    def mm(self, fns):
        self._wait(self.nc.tensor)
        for f in fns[:-1]:
            f()
        fns[-1]().then_inc(self.levels[self.depth][0], 1)
        self.g += 1

    def dma(self, out, in_, slow=False):
        if not self.inloop:
            self.dma_rr = (self.dma_rr + 1) % 3
            eng = [self.nc.sync, self.nc.scalar, self.nc.sync][self.dma_rr]
        else:
            eng = self.loop_eng
        if FORCE_SYNC == "act":
            eng = self.nc.scalar
        elif FORCE_SYNC:
            eng = self.nc.sync
        self._wait(eng)
        if slow:
            eng.dma_start(out=out, in_=in_, allow_slow_non_contiguous=True).then_inc(self.levels[self.depth][1], 16)
        else:
            eng.dma_start(out=out, in_=in_).then_inc(self.levels[self.depth][1], 16)
        self.d += 16

    def loop(self, n, body, eng=None):
        prev_eng = self.loop_eng
        if eng is not None:
            self.loop_eng = eng
        try:
            self._loop(n, body)
        finally:
            self.loop_eng = prev_eng

    def _loop(self, n, body):
        if n == 1:
            old_depth_marker = self.inloop
            self.inloop = True
            body(0)
            self.inloop = old_depth_marker
            return
        nc = self.nc
        for e in self.engs:
            self._wait(e)
        og, od, odepth, owaited = self.g, self.d, self.depth, self.waited
        oa, oan = self.a, self.a_need
        self.depth += 1
        gs, ds, it, asm = self._level(self.depth)
        self.nloops += 1
        tag = self.nloops
        old_inloop = self.inloop
        self.inloop = True
        engines = mybir.ALL_ENGINES
        loop_start = f"L{tag}_loop"
        loop_end = f"L{tag}_end"
        registers = nc.alloc_registers(f"L{tag}_i", engines=engines)
        nc.regs_mov(registers, 0)
        nc.br(loop_start, engines=engines)
        with nc.body(loop_start, valid_engines=engines):
            i = nc.snap(registers, min_val=0, max_val=n - 1)
            for e in self.engs:
                e.wait_ge(it, i)
            self.g = 0
            self.d = 0
            self.a = 0
            self.a_need = 0
            self.waited = {}
            body(i)
            p = nc.gpsimd
            p.wait_ge(gs, self.g)
            p.wait_ge(ds, self.d)
            if self.a:
                p.wait_ge(asm, self.a)
                p.sem_clear(asm)
            p.sem_clear(gs)
            p.sem_clear(ds)
            p.memset(self.dummy[0:1, 0:1], 0.0).then_inc(it, 1)
            nc.regs_alu(registers, registers, 1, op=ALU.add)
            nc.br_lt(registers, n, on_true=loop_start, on_false=loop_end, engines=engines)
        nc.switch_bb(loop_end)
        for h in registers.handles:
            nc.free_register(h)
        for h in i.val.handles:
            nc.free_register(h)
        p = nc.gpsimd
        p.wait_ge(it, n)
        p.sem_clear(it)
        self.depth = odepth
        self.inloop = old_inloop
        self.g, self.d, self.waited = og, od, owaited
        self.a, self.a_need = oa, oan
        p.memset(self.dummy[0:1, 1:2], 0.0).then_inc(self.levels[self.depth][0], 1)
        self.g += 1

    def finish(self):
        for e in self.engs:
            self._wait(e)


def mm_group(nc, ps, pairs):
    fns = []
    n = len(pairs)
    for i, (l, r) in enumerate(pairs):
        fns.append(lambda l=l, r=r, i=i: nc.tensor.matmul(ps, l, r, start=(i == 0), stop=(i == n - 1)))
    return fns


class Ctx:
    pass


def alloc_common(b, c):
    c.ident = b.sb("ident", [128, 128], F32)
    c.xt = b.sb("xt", [128, D], F32)
    c.yt = b.sb("yt", [128, D], F32)
    c.gbc = b.sb("gbc", [128, D], F32)
    c.bbc = b.sb("bbc", [128, D], F32)
    c.stats = b.sb("stats", [128, 2, 6], F32)
    c.mv = b.sb("mv", [128, 2], F32)
    c.rstd = b.sb("rstd", [128, 1], F32)
    c.psA = b.ps("psA", [128, 512], F32)
    c.psB = b.ps("psB", [128, 512], F32)
    c.psC = b.ps("psC", [128, 512], F32)
    c.psD = b.ps("psD", [128, 512], F32)
    c.psT = b.ps("psT", [128, 512], F32)


def layer_norm(b, c, src, dst):
    nc = b.nc
    b.V(lambda: nc.vector.bn_stats(out=c.stats[:, 0, :], in_=src[:, 0:512]))
    b.V(lambda: nc.vector.bn_stats(out=c.stats[:, 1, :], in_=src[:, 512:1024]))
    b.V(lambda: nc.vector.bn_aggr(out=c.mv[:], in_=c.stats[:].rearrange("p a s -> p (a s)")))
    b.V(lambda: nc.vector.tensor_scalar(out=c.rstd[:], in0=c.mv[:, 1:2], scalar1=LN_EPS, scalar2=None, op0=ALU.add))
    b.A(lambda: nc.scalar.activation(out=c.rstd[:], in_=c.rstd[:], func=AF.Sqrt))
    b.V(lambda: nc.vector.reciprocal(out=c.rstd[:], in_=c.rstd[:]))
    b.V(lambda: nc.vector.tensor_scalar(out=dst, in0=src, scalar1=c.mv[:, 0:1], scalar2=c.rstd[:, 0:1],
                                        op0=ALU.subtract, op1=ALU.mult))
    b.V(lambda: nc.vector.tensor_tensor(out=dst, in0=dst, in1=c.gbc[:], op=ALU.mult))
    b.V(lambda: nc.vector.tensor_tensor(out=dst, in0=dst, in1=c.bbc[:], op=ALU.add))


def load_ln(b, c, g_ap, b_ap):
    b.dma(c.gbc[:], g_ap.partition_broadcast(128))
    b.dma(c.bbc[:], b_ap.partition_broadcast(128))


def alloc_moe(b, c):
    c.xT32 = b.sb("xT32", [128, 8, 128], F32)
    c.xTb = b.sb("xTb", [128, 8, 128], BF16)
    c.wr = b.sb("wr", [128, 8, NE], F32)
    c.rb = b.sb("rb", [128, NE], F32)
    c.bd = b.sb("bd", [NE, D], F32)
    c.lg = b.sb("lg", [128, NE], F32)
    c.ex = b.sb("ex", [128, NE], F32)
    c.msk = b.sb("msk", [128, NE], F32)
    c.top8 = b.sb("top8", [128, 8], F32)
    c.nm = b.sb("nm", [128, 1], F32)
    c.ssum = b.sb("ssum", [128, 1], F32)
    c.combT = b.sb("combT", [NE, 128], F32)
    c.xblk = b.sb("xblk", [128, 8, 8, 128], BF16)
    c.acc = b.sb("acc", [128, 8, D], F32)
    c.cmball = b.sb("cmball", [128, 8, NE], F32)
    c.stg = b.sb("stg", [128, 4, 2048], F32)
    c.wgu = b.sb("wgu", [128, 8, 2048], BF16)
    c.wd = b.sb("wd", [128, 8, D], BF16)
    c.bguall = b.sb("bguall", [128, NE, 16], F32)
    c.bl1all = b.sb("bl1all", [128, NE, 8], F32)
    c.hg = b.sb("hg", [128, 512], F32)
    c.hs = b.sb("hs", [128, 512], F32)
    c.hl = b.sb("hl", [128, 512], F32)
    c.hT = b.sb("hT", [128, 8, 512], BF16)


def moe_layer(b, c, li, src_d, dst_d, w, ntok):
    nc = b.nc
    ntiles = ntok // 128
    nblk = ntok // 1024
    src_t = src_d.rearrange("(n p) d -> n p d", p=128)
    dst_t = dst_d.rearrange("(n p) d -> n p d", p=128)
    acc_t = c.acc_d.rearrange("(n p) d -> n p d", p=128)
    xT_t = c.xT_d
    comb_t = c.comb_d

    b.dma(c.wr[:], w["router_w"].rearrange("(k p) e -> p k e", p=128))
    b.dma(c.rb[:], w["router_b"].partition_broadcast(128))
    b.dma(c.bd[:], w["b_down"])

    def p1(t):
        b.dma(c.xt[:], src_t[t])
        for k in range(8):
            b.mm([lambda k=k: nc.tensor.transpose(c.psT[:, 0:128], c.xt[:, k * 128:(k + 1) * 128], c.ident[:])])
            b.V(lambda k=k: nc.vector.tensor_copy(c.xT32[:, k, :], c.psT[:, 0:128]))
            b.G(lambda k=k: nc.gpsimd.tensor_copy(c.xTb[:, k, :], c.xT32[:, k, :]))
        b.mm(mm_group(nc, c.psA[:, 0:NE], [(c.xT32[:, k, :], c.wr[:, k, :]) for k in range(8)]))
        b.V(lambda: nc.vector.tensor_tensor(out=c.lg[:], in0=c.psA[:, 0:NE], in1=c.rb[:], op=ALU.add))
        b.V(lambda: nc.vector.max(out=c.top8[:], in_=c.lg[:]))
        b.V(lambda: nc.vector.tensor_scalar(out=c.msk[:], in0=c.lg[:], scalar1=c.top8[:, 3:4], scalar2=None, op0=ALU.is_ge))
        b.V(lambda: nc.vector.tensor_scalar(out=c.nm[:], in0=c.top8[:, 0:1], scalar1=-1.0, scalar2=None, op0=ALU.mult))
        b.A(lambda: nc.scalar.activation(out=c.ex[:], in_=c.lg[:], func=AF.Exp, bias=c.nm[:, 0:1], scale=1.0))
        b.V(lambda: nc.vector.tensor_tensor(out=c.ex[:], in0=c.ex[:], in1=c.msk[:], op=ALU.mult))
        b.V(lambda: nc.vector.reduce_sum(out=c.ssum[:], in_=c.ex[:], axis=AX.X))
        b.V(lambda: nc.vector.reciprocal(out=c.ssum[:], in_=c.ssum[:]))
        b.V(lambda: nc.vector.tensor_scalar(out=c.ex[:], in0=c.ex[:], scalar1=c.ssum[:, 0:1], scalar2=None, op0=ALU.mult))
        b.dma(comb_t[t][:, 0:NE], c.ex[:])
        b.mm([lambda: nc.tensor.transpose(c.psT[0:NE, 0:128], c.ex[:], c.ident[:])])
        b.V(lambda: nc.vector.tensor_copy(c.combT[:], c.psT[0:NE, 0:128]))
        for h in range(2):
            ps = c.psA if h == 0 else c.psB
            b.mm([lambda ps=ps, h=h: nc.tensor.matmul(ps[:], c.combT[:], c.bd[:, h * 512:(h + 1) * 512], start=True, stop=True)])
            b.V(lambda ps=ps, h=h: nc.vector.scalar_tensor_tensor(out=c.yt[:, h * 512:(h + 1) * 512], in0=c.xt[:, h * 512:(h + 1) * 512],
                                                                 scalar=ALPHA, in1=ps[:], op0=ALU.mult, op1=ALU.add))
        b.dma(acc_t[t], c.yt[:])
        b.dma(xT_t[t], c.xTb[:])

    b.loop(ntiles, p1, eng=nc.sync)

    load_ln(b, c, w["ln2_g"], w["ln2_b"])
    dst_bb = dst_d.rearrange("(n t p) d -> n p t d", t=8, p=128)
    xT_b = xT_t.rearrange("(n t) p k j -> n p t k j", t=8)
    acc_b = c.acc_d.rearrange("(n t p) d -> n p t d", t=8, p=128)
    comb_b = c.comb_d.rearrange("(n t) p e -> n p t e", t=8)[:, :, :, 0:NE]
    bgu_v = w["b_gu"].rearrange("e (c p) -> p e c", p=128)
    for e0 in range(0, NE, 4):
        b.dma(c.bguall[:, e0:e0 + 4, :], bgu_v[:, e0:e0 + 4, :], slow=True)
    b.V(lambda: nc.vector.tensor_scalar(out=c.bl1all[:], in0=c.bguall[:, :, 8:16], scalar1=1.0, scalar2=None, op0=ALU.add))
    wgu = w["w_gu"].rearrange("e (k p) f -> e p k f", p=128)
    wdn = w["w_down"].rearrange("e (k p) f -> e p k f", p=128)

    for e in range(NE):
        for hh in range(2):
            b.dma(c.stg[:], wgu[e][:, hh * 4:(hh + 1) * 4, :])
            b.V(lambda hh=hh: nc.vector.tensor_copy(c.wgu[:, hh * 4:hh * 4 + 2, :], c.stg[:, 0:2, :]))
            b.G(lambda hh=hh: nc.gpsimd.tensor_copy(c.wgu[:, hh * 4 + 2:hh * 4 + 4, :], c.stg[:, 2:4, :]))
        b.dma(c.stg[:].rearrange("p a (h f) -> p (a h) f", h=2), wdn[e])
        b.V(lambda: nc.vector.tensor_copy(c.wd[:, 0:4, :], c.stg[:, 0:2, :].rearrange("p a (h f) -> p (a h) f", h=2)))
        b.G(lambda: nc.gpsimd.tensor_copy(c.wd[:, 4:8, :], c.stg[:, 2:4, :].rearrange("p a (h f) -> p (a h) f", h=2)))
        b.dma(c.wgu_bf_d[e], c.wgu[:])
        b.dma(c.wd_bf_d[e], c.wd[:])

    def blk(bi):
        b.dma(c.xblk[:], xT_b[bi])
        b.dma(c.acc[:], acc_b[bi])
        b.dma(c.cmball[:], comb_b[bi])

        wbufs = [c.wgu[:], c.stg[:].rearrange("p a f -> p (a f)").bitcast(BF16).rearrange("p (k f) -> p k f", k=8)]
        toks = {}

        def expert(e):
            wg = wbufs[e % 2]
            if e == 0:
                b.dma(wg, c.wgu_bf_d[e])
            else:
                b.need(toks[e])
            if e + 1 < MOE_NEXP:
                toks[e + 1] = b.dma_async(wbufs[(e + 1) % 2], c.wgu_bf_d[e + 1])
            b.dma(c.wd[:], c.wd_bf_d[e])
            for tt in range(2):
                for j in range(8):
                    b.mm(mm_group(nc, c.psA[:], [(wg[:, k, j * 128:(j + 1) * 128],
                                                  c.xblk[:, tt * 4:(tt + 1) * 4, k, :]) for k in range(8)]))
                    b.mm(mm_group(nc, c.psB[:], [(wg[:, k, 1024 + j * 128:1024 + (j + 1) * 128],
                                                  c.xblk[:, tt * 4:(tt + 1) * 4, k, :]) for k in range(8)]))
                    b.V(lambda j=j, e=e: nc.vector.tensor_scalar(out=c.hg[:], in0=c.psA[:], scalar1=c.bguall[:, e, j:j + 1], scalar2=7.0,
                                                            op0=ALU.add, op1=ALU.min))
                    b.A(lambda: nc.scalar.activation(out=c.hs[:], in_=c.hg[:], func=AF.Sigmoid, scale=1.702))
                    b.V(lambda j=j, e=e: nc.vector.tensor_scalar(out=c.hl[:], in0=c.psB[:], scalar1=c.bl1all[:, e, j:j + 1], scalar2=8.0,
                                                            op0=ALU.add, op1=ALU.min))
                    b.G(lambda: nc.gpsimd.tensor_tensor(out=c.hg[:], in0=c.hg[:], in1=c.hs[:], op=ALU.mult))
                    b.V(lambda j=j: nc.vector.scalar_tensor_tensor(out=c.hT[:, j, :], in0=c.hl[:], scalar=-6.0, in1=c.hg[:],
                                                                   op0=ALU.max, op1=ALU.mult))
                for ts in range(4):
                    ti = tt * 4 + ts
                    for h in range(2):
                        ps = c.psC if h == 0 else c.psD
                        b.mm(mm_group(nc, ps[:], [(c.hT[:, j, ts * 128:(ts + 1) * 128], c.wd[:, j, h * 512:(h + 1) * 512])
                                                  for j in range(8)]))
                        b.V(lambda ps=ps, ti=ti, h=h, e=e: nc.vector.scalar_tensor_tensor(
                            out=c.acc[:, ti, h * 512:(h + 1) * 512], in0=ps[:], scalar=c.cmball[:, ti, e:e + 1],
                            in1=c.acc[:, ti, h * 512:(h + 1) * 512], op0=ALU.mult, op1=ALU.add))

        for e in range(MOE_NEXP):
            expert(e)
        for ti in range(8):
            layer_norm(b, c, c.acc[:, ti, :], c.acc[:, ti, :])
        b.dma(dst_bb[bi], c.acc[:])

    b.loop(nblk, blk, eng=nc.scalar)


def load_cast_rows(b, c, dst_bf, src_d, ncols):
    nc = b.nc
    src = src_d.rearrange("(k p) f -> p k f", p=128)
    st = c.stg[:].rearrange("p a f -> p (a f)")
    for k in range(8):
        for c0 in range(0, ncols, 2048):
            w = min(2048, ncols - c0)
            b.dma(st[:, 0:w], src[:, k, c0:c0 + w])
            b.V(lambda k=k, c0=c0, w=w: nc.vector.tensor_copy(dst_bf[:, k, c0:c0 + w], st[:, 0:w]))


def x_to_xT(b, c):
    nc = b.nc
    for k in range(8):
        b.mm([lambda k=k: nc.tensor.transpose(c.psT[:, 0:128], c.xt[:, k * 128:(k + 1) * 128], c.ident[:])])
        b.V(lambda k=k: nc.vector.tensor_copy(c.xTb[:, k, :], c.psT[:, 0:128]))


def out_proj_ln(b, c, src, wout_bf, bias_bc, dst_ap):
    nc = b.nc
    for k in range(8):
        b.mm([lambda k=k: nc.tensor.transpose(c.psT[:, 0:128], src[:, k * 128:(k + 1) * 128], c.ident[:])])
        b.V(lambda k=k: nc.vector.tensor_copy(c.xTb[:, k, :], c.psT[:, 0:128]))
    for h in range(2):
        ps = c.psA if h == 0 else c.psB
        b.mm(mm_group(nc, ps[:], [(c.xTb[:, k, :], wout_bf[:, k, h * 512:(h + 1) * 512]) for k in range(8)]))
        b.V(lambda ps=ps, h=h: nc.vector.scalar_tensor_tensor(out=c.yt[:, h * 512:(h + 1) * 512], in0=c.xt[:, h * 512:(h + 1) * 512],
                                                             scalar=ALPHA, in1=ps[:], op0=ALU.mult, op1=ALU.add))
    if bias_bc is not None:
        b.V(lambda: nc.vector.tensor_tensor(out=c.yt[:], in0=c.yt[:], in1=bias_bc, op=ALU.add))
    layer_norm(b, c, c.yt[:], c.yt[:])
    b.dma(dst_ap, c.yt[:])


def alloc_mixer(b, c):
    c.wmB = b.sb("wmB", [128, 8, 1056], BF16)
    c.tri = b.sb("tri", [128, 3, 128], F32)
    sflat = c.stg[:].rearrange("p a f -> p (a f)")
    c.gw2 = sflat[0:32, 0:1024].rearrange("p (n f) -> p n f", n=2)
    c.gbb = sflat[:, 2048:2560]
    c.ngb = b.sb("ngb", [128, 256], F32)
    c.qT = b.sb("qT", [128, 4, 128], BF16)
    c.kT = b.sb("kT", [128, 4, 128], BF16)
    c.ktm = b.sb("ktm", [128, 512], BF16)
    c.vbf = b.sb("vbf", [128, D], BF16)
    c.scT = b.sb("scT", [128, 128], BF16)
    c.Sbf = b.sb("Sbf", [128, 4, 256], BF16)
    c.gcol = b.sb("gcol", [128, 4], F32)
    c.glT = b.sb("glT", [32, 128], F32)
    c.gl = b.sb("gl", [128, 32], F32)
    c.r4 = b.sb("r4", [128, 4], F32)
    c.hv = b.sb("hv", [128, 16], F32)


def gla_layer(b, c, src_d, dst_d, w, ntok, L):
    nc = b.nc
    nseq = ntok // L
    tps = L // 128
    src_t = src_d.rearrange("(s n p) d -> s n p d", p=128, n=tps)
    dst_t = dst_d.rearrange("(s n p) d -> s n p d", p=128, n=tps)
    of_t = c.acc_d.rearrange("(s n p) d -> s n p d", p=128, n=tps)
    load_cast_rows(b, c, c.wgu, w["w_in"][:, 0:2048], 2048)
    load_cast_rows(b, c, c.wmB, w["w_in"][:, 2048:3104], 1056)
    load_cast_rows(b, c, c.wd, w["w_out"], 1024)
    b.G(lambda: nc.gpsimd.memset(c.gw2, 0.0))
    b.dma(c.gw2[0:16, 0, :], w["gate_w2"][0])
    b.dma(c.gw2[16:32, 1, :], w["gate_w2"][1])
    b.dma(c.ngb[:], w["norm_g"].partition_broadcast(128))
    load_ln(b, c, w["ln1_g"], w["ln1_b"])
    og = c.acc[:, 0, :]
    q = c.acc[:, 1, 0:512]
    k_ = c.acc[:, 1, 512:1024]
    oacc = c.acc[:, 2, :]
    S = c.acc[:, 3, :].rearrange("p (h e) -> p h e", h=4)
    oft = c.acc[:, 4, :]
    sq = c.acc[:, 5, :]
    B = c.hg
    EB = c.hs
    EnB = c.hl

    def one_pass(direction):
        n = direction
        b.dma(c.gbb, w["gate_b"][n].partition_broadcast(128))

        def seq(si):
            b.V(lambda: nc.vector.memset(S, 0.0))
            b.V(lambda: nc.vector.memset(c.Sbf[:], 0.0))

            def tile(ti):
                t = ti if n == 0 else (tps - 1) - ti
                b.dma(c.xt[:], src_t[si][t])
                x_to_xT(b, c)
                def proj(ps, wbf, c0, wid):
                    b.mm(mm_group(nc, ps[:, 0:wid], [(c.xTb[:, kk, :], wbf[:, kk, c0:c0 + wid]) for kk in range(8)]))
                proj(c.psA, c.wgu, 0, 512)
                b.V(lambda: nc.vector.tensor_copy(q, c.psA[:]))
                proj(c.psA, c.wgu, 512, 512)
                b.V(lambda: nc.vector.tensor_copy(k_, c.psA[:]))
                proj(c.psA, c.wgu, 1024, 512)
                b.V(lambda: nc.vector.tensor_copy(c.vbf[:, 0:512], c.psA[:]))
                proj(c.psA, c.wgu, 1536, 512)
                b.V(lambda: nc.vector.tensor_copy(c.vbf[:, 512:1024], c.psA[:]))
                proj(c.psA, c.wmB, 1024, 32)
                b.V(lambda: nc.vector.tensor_copy(c.gl[:], c.psA[:, 0:32]))
                b.mm([lambda: nc.tensor.transpose(c.psT[0:32, 0:128], c.gl[:], c.ident[:])])
                b.V(lambda: nc.vector.tensor_copy(c.glT[:], c.psT[0:32, 0:128]))
                b.mm([lambda: nc.tensor.matmul(c.psA[:], c.glT[:], c.gw2[:, n, :], start=True, stop=True)])
                b.V(lambda: nc.vector.tensor_tensor(out=B[:], in0=c.psA[:], in1=c.gbb, op=ALU.add))
                b.A(lambda: nc.scalar.activation(out=B[:], in_=B[:], func=AF.Exp, scale=-1.0))
                b.A(lambda: nc.scalar.activation(out=B[:], in_=B[:], func=AF.Ln, bias=1.0, scale=1.0))
                b.mm([lambda: nc.tensor.matmul(c.psA[:], c.tri[:, n, :], B[:], start=True, stop=True)])
                b.A(lambda: nc.scalar.activation(out=EB[:], in_=c.psA[:], func=AF.Exp, scale=-1.0 / 16.0))
                b.A(lambda: nc.scalar.activation(out=EnB[:], in_=c.psA[:], func=AF.Exp, scale=1.0 / 16.0))
                b.V(lambda: nc.vector.scalar_tensor_tensor(out=q, in0=q, scalar=128.0 ** -0.5, in1=EB[:], op0=ALU.mult, op1=ALU.mult))
                b.V(lambda: nc.vector.tensor_tensor(out=k_, in0=k_, in1=EnB[:], op=ALU.mult))
                b.V(lambda: nc.vector.tensor_copy(c.ktm[:], k_))
                for h in range(4):
                    hs = slice(h * 128, (h + 1) * 128)
                    b.mm([lambda hs=hs: nc.tensor.transpose(c.psT[:, 0:128], q[:, hs], c.ident[:])])
                    b.V(lambda h=h: nc.vector.tensor_copy(c.qT[:, h, :], c.psT[:, 0:128]))
                    b.mm([lambda hs=hs: nc.tensor.transpose(c.psT[:, 0:128], k_[:, hs], c.ident[:])])
                    b.V(lambda h=h: nc.vector.tensor_copy(c.kT[:, h, :], c.psT[:, 0:128]))
                    b.mm([lambda hs=hs: nc.tensor.transpose(c.psT[:, 0:128], EB[:, hs], c.ident[:])])
                    col = 127 if n == 0 else 0
                    b.V(lambda h=h, col=col: nc.vector.tensor_copy(c.gcol[:, h:h + 1], c.psT[:, col:col + 1]))
                for h in range(4):
                    vs = slice(h * 256, (h + 1) * 256)
                    b.mm([lambda h=h: nc.tensor.matmul(c.psB[:, 0:128], c.kT[:, h, :], c.qT[:, h, :], start=True, stop=True)])
                    mk = 0 if n == 0 else 2
                    b.V(lambda mk=mk: nc.vector.tensor_tensor(out=c.scT[:], in0=c.psB[:, 0:128], in1=c.tri[:, mk, :], op=ALU.mult))
                    b.mm(mm_group(nc, c.psC[:, 0:256], [(c.scT[:], c.vbf[:, vs]), (c.qT[:, h, :], c.Sbf[:, h, :])]))
                    b.V(lambda vs=vs: nc.vector.tensor_copy(oacc[:, vs], c.psC[:, 0:256]))
                    b.mm([lambda h=h, vs=vs: nc.tensor.matmul(c.psD[:, 0:256], c.ktm[:, h * 128:(h + 1) * 128], c.vbf[:, vs], start=True, stop=True)])
                    b.V(lambda h=h: nc.vector.tensor_tensor(out=S[:, h, :], in0=c.psD[:, 0:256], in1=S[:, h, :], op=ALU.add))
                    b.V(lambda h=h: nc.vector.tensor_scalar(out=S[:, h, :], in0=S[:, h, :], scalar1=c.gcol[:, h:h + 1], scalar2=None, op0=ALU.mult))
                    b.V(lambda h=h: nc.vector.tensor_copy(c.Sbf[:, h, :], S[:, h, :]))
                if n == 0:
                    b.dma(of_t[si][t], oacc)
                else:
                    b.dma(oft, of_t[si][t])
                    b.V(lambda: nc.vector.tensor_tensor(out=oacc, in0=oacc, in1=oft, op=ALU.add))
                    b.V(lambda: nc.vector.tensor_tensor(out=sq, in0=oacc, in1=oacc, op=ALU.mult))
                    b.V(lambda: nc.vector.reduce_sum(out=c.r4[:], in_=sq.rearrange("p (h e) -> p h e", h=4), axis=AX.X))
                    b.V(lambda: nc.vector.tensor_scalar(out=c.r4[:], in0=c.r4[:], scalar1=1.0 / 256.0, scalar2=1e-6, op0=ALU.mult, op1=ALU.add))
                    b.A(lambda: nc.scalar.activation(out=c.r4[:], in_=c.r4[:], func=AF.Sqrt))
                    b.V(lambda: nc.vector.reciprocal(out=c.r4[:], in_=c.r4[:]))
                    for h in range(4):
                        vs = slice(h * 256, (h + 1) * 256)
                        b.V(lambda h=h, vs=vs: nc.vector.scalar_tensor_tensor(out=oacc[:, vs], in0=oacc[:, vs], scalar=c.r4[:, h:h + 1],
                                                                           in1=c.ngb[:], op0=ALU.mult, op1=ALU.mult))
                    for hh in range(2):
                        proj(c.psA, c.wmB, hh * 512, 512)
                        b.V(lambda hh=hh: nc.vector.tensor_copy(og[:, hh * 512:(hh + 1) * 512], c.psA[:]))
                    b.A(lambda: nc.scalar.activation(out=sq, in_=og, func=AF.Sigmoid))
                    b.V(lambda: nc.vector.tensor_tensor(out=sq, in0=sq, in1=og, op=ALU.mult))
                    b.V(lambda: nc.vector.tensor_tensor(out=oacc, in0=oacc, in1=sq, op=ALU.mult))
                    out_proj_ln(b, c, oacc, c.wd, None, dst_t[si][t])

            b.loop(tps, tile)

        b.loop(nseq, seq, eng=nc.sync)

    one_pass(0)
    one_pass(1)


def alloc_na(b, c):
    xflat = c.xblk[:].rearrange("p a k t -> p (a k t)")
    sflat = c.stg[:].rearrange("p a f -> p (a f)")
    c.nqT = xflat[0:32, 0:4096]
    c.nkT = xflat[0:32, 4096:8192]
    c.nv = c.hT[:].rearrange("p a t -> p (a t)")[0:64, 0:2048].rearrange("p (r e) -> p r e", e=32)
    c.no = sflat[0:64, 0:2048].rearrange("p (r e) -> p r e", e=32)
    c.nbias = sflat[0:64, 2048:3008].rearrange("p (a j) -> p a j", j=64)
    c.nsc = sflat[0:64, 4096:4608]
    c.nP = b.sb("nP", [64, 512], BF16)
    c.nPT = b.sb("nPT", [64, 8, 64], BF16)
    c.nmx = b.sb("nmx", [64, 1], F32)
    c.nsm = b.sb("nsm", [64, 1], F32)
    c.identb = b.sb("identb", [128, 128], BF16)
    c.qkb = c.hl[:].bitcast(BF16)
    c.psTb = b.ps("psTb", [128, 1024], BF16)


def na_layer(b, c, src_d, dst_d, w, ntok, L):
    nc = b.nc
    nseq = ntok // L
    tps = L // 128
    R = L // 64
    src_t = src_d.rearrange("(n p) d -> n p d", p=128)
    dst_t = dst_d.rearrange("(n p) d -> n p d", p=128)
    o_t = c.acc_d.rearrange("(n p) d -> n p d", p=128)
    o_h = c.acc_d.rearrange("(s r p) (h e) -> s h p r e", r=R, p=64, e=32)
    qT_d = c.qT_d.rearrange("g (a e) t -> (g a) e t", e=32)
    kT_d = c.kT_d.rearrange("g (a e) t -> (g a) e t", e=32)
    qT_w = c.qT_d.rearrange("g p (n t) -> n g p t", t=128)
    kT_w = c.kT_d.rearrange("g p (n t) -> n g p t", t=128)
    v_w = c.v_d.rearrange("(n p) d -> n p d", p=128)
    v_h = c.v_d.rearrange("(s r p) (h e) -> s h p r e", r=R, p=64, e=32)
    load_cast_rows(b, c, c.wgu, w["w_in"][:, 0:2048], 2048)
    load_cast_rows(b, c, c.wmB, w["w_in"][:, 2048:3072], 1024)
    load_cast_rows(b, c, c.wd, w["w_out"], 1024)
    load_ln(b, c, w["ln1_g"], w["ln1_b"])
    bin_bc = c.acc[:, 0:3, :].rearrange("p a d -> p (a d)")
    bout_bc = c.acc[:, 3, :]
    pq = c.acc[:, 4, :]
    b.dma(bin_bc, w["b_in"].partition_broadcast(128))
    b.dma(bout_bc, w["b_out"].partition_broadcast(128))
    b.V(lambda: nc.vector.tensor_copy(c.identb[:], c.ident[:]))

    def p1(t):
        b.dma(c.xt[:], src_t[t])
        x_to_xT(b, c)
        for part in range(3):
            for hh in range(2):
                c0 = part * 1024 + hh * 512
                wbf, wc0 = (c.wgu, c0) if c0 < 2048 else (c.wmB, c0 - 2048)
                b.mm(mm_group(nc, c.psA[:], [(c.xTb[:, kk, :], wbf[:, kk, wc0:wc0 + 512]) for kk in range(8)]))
                b.V(lambda hh=hh, c0=c0: nc.vector.tensor_tensor(out=pq[:, hh * 512:(hh + 1) * 512], in0=c.psA[:],
                                                               in1=bin_bc[:, c0:c0 + 512], op=ALU.add))
            if part == 0:
                b.V(lambda: nc.vector.tensor_scalar(out=c.qkb, in0=pq, scalar1=32.0 ** -0.5, scalar2=None, op0=ALU.mult))
            else:
                b.V(lambda: nc.vector.tensor_copy(c.qkb, pq))
            if part == 2:
                b.dma(v_w[t], c.qkb)
            else:
                for g in range(8):
                    b.mm([lambda g=g: nc.tensor.transpose(c.psTb[:, g * 128:(g + 1) * 128], c.qkb[:, g * 128:(g + 1) * 128], c.identb[:])])
                b.V(lambda: nc.vector.tensor_copy(c.vbf[:], c.psTb[:]))
                dstw = qT_w if part == 0 else kT_w
                b.dma(dstw[t].rearrange("g p t -> p g t"), c.vbf[:].rearrange("p (g t) -> p g t", g=8))

    b.loop(ntok // 128, p1, eng=nc.sync)

    def seq(si):
        def head(h):
            b.dma(c.nqT[:, 0:L], qT_d[h].rearrange("e (s t) -> s e t", t=L)[si])
            b.dma(c.nkT[:, 0:L], kT_d[h].rearrange("e (s t) -> s e t", t=L)[si])
            b.dma(c.nv[:, 0:R, :], v_h[si][h])
            b.dma(c.nbias, w["bias2"][h])
            for r in range(R):
                rs = min(max(r - 4, 0), R - 8)
                dr0 = rs - r + 7
                b.mm([lambda r=r, rs=rs: nc.tensor.matmul(c.psA[0:64, :], c.nqT[:, r * 64:(r + 1) * 64], c.nkT[:, rs * 64:rs * 64 + 512],
                                                          start=True, stop=True)])
                b.V(lambda dr0=dr0: nc.vector.tensor_tensor(out=c.nsc, in0=c.psA[0:64, :],
                                                            in1=c.nbias[:, dr0:dr0 + 8, :].rearrange("p a j -> p (a j)"), op=ALU.add))
                b.V(lambda: nc.vector.reduce_max(out=c.nmx[:], in_=c.nsc, axis=AX.X))
                b.V(lambda: nc.vector.tensor_scalar(out=c.nmx[:], in0=c.nmx[:], scalar1=-1.0, scalar2=None, op0=ALU.mult))
                b.A(lambda: nc.scalar.activation(out=c.nsc, in_=c.nsc, func=AF.Exp, bias=c.nmx[:, 0:1], scale=1.0))
                b.V(lambda: nc.vector.reduce_sum(out=c.nsm[:], in_=c.nsc, axis=AX.X))
                b.V(lambda: nc.vector.reciprocal(out=c.nsm[:], in_=c.nsm[:]))
                b.V(lambda: nc.vector.tensor_copy(c.nP[:], c.nsc))
                for a in range(8):
                    b.mm([lambda a=a: nc.tensor.transpose(c.psTb[0:64, a * 64:(a + 1) * 64], c.nP[:, a * 64:(a + 1) * 64], c.identb[0:64, 0:64])])
                b.V(lambda: nc.vector.tensor_copy(c.nPT[:].rearrange("p a q -> p (a q)"), c.psTb[0:64, 0:512]))
                b.mm(mm_group(nc, c.psB[0:64, 0:32], [(c.nPT[:, a, :], c.nv[:, rs + a, :]) for a in range(8)]))
                b.V(lambda r=r: nc.vector.tensor_scalar(out=c.no[:, r, :], in0=c.psB[0:64, 0:32], scalar1=c.nsm[:, 0:1], scalar2=None, op0=ALU.mult))
            b.dma(o_h[si][h], c.no[:, 0:R, :])

        b.loop(32, head)

    b.loop(nseq, seq, eng=nc.scalar)

    def p3(t):
        b.dma(c.xt[:], src_t[t])
        b.dma(pq, o_t[t])
        out_proj_ln(b, c, pq, c.wd, bout_bc, dst_t[t])

    b.loop(ntok // 128, p3, eng=nc.sync)


def hyena_consts(L):
    import ml_dtypes
    N = 2 * L
    nfc = L // 128 + 1
    NF = nfc * 128
    t = np.arange(L, dtype=np.int64)
    f = np.arange(NF, dtype=np.int64)
    ang = 2.0 * np.pi * ((t[:, None] * f[None, :]) % N).astype(np.float64) / N
    cs, sn = np.cos(ang), np.sin(ang)
    wf = np.where((f == 0) | (f == L), 1.0, 2.0) / N
    wf[f > L] = 0.0
    bf = ml_dtypes.bfloat16
    bf = ml_dtypes.bfloat16
    tps = L // 128

    def fwd_blk(m):
        return np.ascontiguousarray(m.reshape(tps, 128, nfc, 128).transpose(2, 1, 0, 3)).astype(np.float32).astype(bf)

    def inv_blk(m):
        return np.ascontiguousarray(m.reshape(nfc, 128, tps, 128).transpose(2, 1, 0, 3)).astype(np.float32).astype(bf)

    out = {"hy_dft_c": fwd_blk(cs), "hy_dft_s": fwd_blk(sn),
           "hy_idft_c": inv_blk((cs * wf[None, :]).T), "hy_idft_s": inv_blk((sn * wf[None, :]).T)}
    tf = np.arange(L, dtype=np.float32)
    t_norm = tf / np.float32(max(L - 1, 1))
    freqs = np.linspace(1e-4, 15, 16, dtype=np.float32)
    a2 = (np.float32(2.0 * math.pi / L) * tf[:, None] * freqs[None, :]).astype(np.float32)
    pe = np.concatenate([t_norm[:, None], np.cos(a2), -np.sin(a2)], axis=-1).astype(np.float32)
    out["hy_peT"] = np.ascontiguousarray(pe.T)
    min_decay = math.log(1e-2) / 1.5
    max_decay = math.log(1e-2) / 0.3
    deltas = np.abs(np.linspace(min_decay, max_decay, D, dtype=np.float32))
    out["hy_win"] = (np.exp(-t_norm[:, None] * deltas[None, :]) + np.float32(0.05)).astype(np.float32)
    return out


def hyena_layer(b, c, src_d, dst_d, w, ntok, L):
    nc = b.nc
    nseq = ntok // L
    tps = L // 128
    nfc = tps + 1
    CW = 256
    nct = D // CW
    src_t = src_d.rearrange("(s n p) d -> s n p d", p=128, n=tps)
    dst_t = dst_d.rearrange("(s n p) d -> s n p d", p=128, n=tps)
    accf = c.acc[:].rearrange("p a d -> p (a d)")
    stgf = c.stg[:].rearrange("p a f -> p (a f)")
    accb = accf.bitcast(BF16)
    stgb = stgf.bitcast(BF16)
    zt = c.xblk[:].rearrange("p a k t -> p (a k t)")[:, 0:tps * CW].rearrange("p (t c) -> p t c", c=CW)
    hv = c.hv

    w1 = c.xt[0:33, 0:64]
    w2 = c.xt[0:64, 64:128]
    w3 = accf[0:64, 0:4096]
    peT = accf[0:33, 4096:4096 + L]
    b.dma(w1, w["ffn_w1"])
    b.dma(w2, w["ffn_w2"])
    b.dma(w3, w["ffn_w3"])
    b.dma(peT, w["peT"])
    b.dma(hv[0:64, 0:1], w["ffn_b1"].rearrange("(p o) -> p o", o=1))
    b.dma(hv[0:64, 1:2], w["ffn_b2"].rearrange("(p o) -> p o", o=1))
    b.dma(hv[0:64, 2:3], w["sin_freq"][0].rearrange("(p o) -> p o", o=1))
    b.dma(hv[0:64, 3:4], w["sin_freq"][1].rearrange("(p o) -> p o", o=1))
    for i in range(2):
        b.V(lambda i=i: nc.vector.tensor_scalar(out=hv[0:64, 4 + i:5 + i], in0=hv[0:64, 2 + i:3 + i], scalar1=0.125, scalar2=None, op0=ALU.mult))
        b.V(lambda i=i: nc.vector.tensor_tensor(out=hv[0:64, 6 + i:7 + i], in0=hv[0:64, 4 + i:5 + i], in1=hv[0:64, i:i + 1], op=ALU.mult))
        b.V(lambda i=i: nc.vector.tensor_scalar(out=hv[0:64, 8 + i:9 + i], in0=hv[0:64, 6 + i:7 + i], scalar1=math.pi / 2, scalar2=None, op0=ALU.add))
    m0 = hv[:, 10:11]
    b.V(lambda: nc.vector.tensor_scalar(out=m0, in0=c.ident[:, 0:1], scalar1=-1.0, scalar2=1.0, op0=ALU.mult, op1=ALU.add))

    def sin_layer(ps, i, out):
        S, C, T = out, c.hl[0:64, :], c.yt[0:64, 0:512]
        b.A(lambda: nc.scalar.activation(out=S, in_=ps, func=AF.Sin, bias=hv[0:64, 6 + i:7 + i], scale=hv[0:64, 4 + i:5 + i]))
        b.A(lambda: nc.scalar.activation(out=C, in_=ps, func=AF.Sin, bias=hv[0:64, 8 + i:9 + i], scale=hv[0:64, 4 + i:5 + i]))
        for _ in range(3):
            b.V(lambda: nc.vector.tensor_tensor(out=T, in0=S, in1=S, op=ALU.mult))
            b.V(lambda: nc.vector.scalar_tensor_tensor(out=S, in0=S, scalar=2.0, in1=C, op0=ALU.mult, op1=ALU.mult))
            b.V(lambda: nc.vector.tensor_scalar(out=C, in0=T, scalar1=-2.0, scalar2=1.0, op0=ALU.mult, op1=ALU.add))

    h3w = stgf[:, 0:4096]
    wint = stgf[:, 4096:5120]
    for q in range(L // 512):
        b.mm([lambda q=q: nc.tensor.matmul(c.psA[0:64, :], w1, peT[:, q * 512:(q + 1) * 512], start=True, stop=True)])
        sin_layer(c.psA[0:64, :], 0, c.hg[0:64, :])
        b.mm([lambda: nc.tensor.matmul(c.psB[0:64, :], w2, c.hg[0:64, :], start=True, stop=True)])
        sin_layer(c.psB[0:64, :], 1, c.hs[0:64, :])
        for tt in range(4):
            t = q * 4 + tt
            b.dma(wint, w["win"][t * 128:(t + 1) * 128, :])
            for cb in range(8):
                b.mm([lambda tt=tt, cb=cb: nc.tensor.matmul(c.psC[:], c.hs[0:64, tt * 128:(tt + 1) * 128], w3[:, cb * 512:(cb + 1) * 512],
                                                         start=True, stop=True)])
                b.V(lambda cb=cb: nc.vector.tensor_tensor(out=h3w[:, cb * 512:(cb + 1) * 512], in0=c.psC[:],
                                                         in1=wint[:, (cb % 2) * 512:(cb % 2 + 1) * 512], op=ALU.mult))
            for o in range(2):
                hf = h3w[:, o * 2048:o * 2048 + 1024]
                hb = h3w[:, o * 2048 + 1024:(o + 1) * 2048]
                if t == 0:
                    b.V(lambda hb=hb: nc.vector.tensor_scalar(out=hb, in0=hb, scalar1=m0, scalar2=None, op0=ALU.mult))
                b.V(lambda hf=hf, hb=hb: nc.vector.tensor_tensor(out=c.vbf[:], in0=hf, in1=hb, op=ALU.add))
                b.dma(c.hk_d[o][0][t], c.vbf[:])
                b.V(lambda hf=hf, hb=hb: nc.vector.tensor_tensor(out=c.vbf[:], in0=hf, in1=hb, op=ALU.subtract))
                b.dma(c.hk_d[o][1][t], c.vbf[:])

    dcv, dsv, icv, isv = w["dft_c"], w["dft_s"], w["idft_c"], w["idft_s"]
    dcb = accb[:, 8448:8448 + tps * 128].rearrange("p (t f) -> p t f", f=128)
    dsb = stgb[:, 8448:8448 + tps * 128].rearrange("p (t f) -> p t f", f=128)
    icb = accb[:, 8448:8448 + nfc * 128].rearrange("p (a t) -> p a t", t=128)
    isb = stgb[:, 8448:8448 + nfc * 128].rearrange("p (a t) -> p a t", t=128)
    Yr = accb[:, 0:nfc * CW].rearrange("p (a c) -> p a c", c=CW)
    Ys = stgb[:, 0:nfc * CW].rearrange("p (a c) -> p a c", c=CW)
    zt2 = accb[:, 0:tps * CW].rearrange("p (t c) -> p t c", c=CW)
    for o in range(2):
        for ct in range(nct):
            cs_ = slice(ct * CW, (ct + 1) * CW)
            b.dma(zt, c.hk_d[o][0].rearrange("t p c -> p t c")[:, :, cs_])
            b.dma(zt2, c.hk_d[o][1].rearrange("t p c -> p t c")[:, :, cs_])
            for fc in range(nfc):
                b.dma(dcb, dcv[fc])
                b.dma(dsb, dsv[fc])
                b.mm(mm_group(nc, c.psA[:, 0:CW], [(dcb[:, tc, :], zt[:, tc, :]) for tc in range(tps)]))
                b.mm(mm_group(nc, c.psB[:, 0:CW], [(dsb[:, tc, :], zt2[:, tc, :]) for tc in range(tps)]))
                b.V(lambda: nc.vector.tensor_copy(c.hg[:, 0:CW], c.psA[:, 0:CW]))
                b.V(lambda: nc.vector.tensor_copy(c.hg[:, CW:2 * CW], c.psB[:, 0:CW]))
                b.dma(c.K_d[o][0][fc][:, cs_], c.hg[:, 0:CW])
                b.dma(c.K_d[o][1][fc][:, cs_], c.hg[:, CW:2 * CW])

    load_cast_rows(b, c, c.wgu, w["w_in"][:, 0:2048], 2048)
    load_cast_rows(b, c, c.wmB, w["w_in"][:, 2048:3072], 1024)
    load_cast_rows(b, c, c.wd, w["w_out"], 1024)
    b.G(lambda: nc.gpsimd.memset(c.yt[:], 0.0))
    for j3 in range(3):
        b.dma(c.p_d[0:1, j3 * 1024:(j3 + 1) * 1024], c.yt[0:1, :])
        b.dma(c.p_d[L + 1:L + 2, j3 * 1024:(j3 + 1) * 1024], c.yt[0:1, :])
    p_rows = c.p_d[1:L + 1, :].rearrange("(n p) c -> n p c", p=128)
    p_m = c.p_d[0:L, :].rearrange("(n p) c -> n p c", p=128)
    p_p = c.p_d[2:L + 2, :].rearrange("(n p) c -> n p c", p=128)

    def conv(o, in_d, gate_d, out_d, out_bf):
        b.dma(c.gbc[:], w["filt_bias"][o].partition_broadcast(128))
        in_v = in_d.rearrange("t p c -> p t c")
        for ct in range(nct):
            cs_ = slice(ct * CW, (ct + 1) * CW)
            b.dma(zt, in_v[:, :, cs_])
            for fc in range(nfc):
                b.dma(dcb, dcv[fc])
                b.dma(dsb, dsv[fc])
                b.dma(c.hg[:, 0:CW], c.K_d[o][0][fc][:, cs_])
                b.dma(c.hg[:, CW:2 * CW], c.K_d[o][1][fc][:, cs_])
                b.mm(mm_group(nc, c.psA[:, 0:CW], [(dcb[:, tc, :], zt[:, tc, :]) for tc in range(tps)]))
                b.mm(mm_group(nc, c.psB[:, 0:CW], [(dsb[:, tc, :], zt[:, tc, :]) for tc in range(tps)]))
                b.V(lambda: nc.vector.tensor_tensor(out=c.hs[:, 0:CW], in0=c.psA[:, 0:CW], in1=c.hg[:, 0:CW], op=ALU.mult))
                b.V(lambda: nc.vector.tensor_tensor(out=c.hs[:, CW:2 * CW], in0=c.psB[:, 0:CW], in1=c.hg[:, CW:2 * CW], op=ALU.mult))
                b.V(lambda fc=fc: nc.vector.tensor_tensor(out=Yr[:, fc, :], in0=c.hs[:, 0:CW], in1=c.hs[:, CW:2 * CW], op=ALU.subtract))
                b.V(lambda: nc.vector.tensor_tensor(out=c.hs[:, 0:CW], in0=c.psA[:, 0:CW], in1=c.hg[:, CW:2 * CW], op=ALU.mult))
                b.V(lambda: nc.vector.tensor_tensor(out=c.hs[:, CW:2 * CW], in0=c.psB[:, 0:CW], in1=c.hg[:, 0:CW], op=ALU.mult))
                b.V(lambda fc=fc: nc.vector.tensor_tensor(out=Ys[:, fc, :], in0=c.hs[:, 0:CW], in1=c.hs[:, CW:2 * CW], op=ALU.add))
            for tc in range(tps):
                b.dma(icb, icv[tc])
                b.dma(isb, isv[tc])
                b.dma(c.hl[:, 0:CW], gate_d[tc][:, cs_])
                pairs = []
                for fc in range(nfc):
                    pairs.append((icb[:, fc, :], Yr[:, fc, :]))
                    pairs.append((isb[:, fc, :], Ys[:, fc, :]))
                b.mm(mm_group(nc, c.psC[:, 0:CW], pairs))
                b.V(lambda tc=tc, cs_=cs_: nc.vector.tensor_tensor(out=c.hl[:, CW:2 * CW], in0=zt[:, tc, :], in1=c.gbc[:, cs_], op=ALU.mult))
                b.V(lambda: nc.vector.tensor_tensor(out=c.hl[:, CW:2 * CW], in0=c.psC[:, 0:CW], in1=c.hl[:, CW:2 * CW], op=ALU.add))
                if out_bf:
                    b.V(lambda: nc.vector.tensor_tensor(out=c.vbf[:, 0:CW], in0=c.hl[:, CW:2 * CW], in1=c.hl[:, 0:CW], op=ALU.mult))
                    b.dma(out_d[tc][:, cs_], c.vbf[:, 0:CW])
                else:
                    b.V(lambda: nc.vector.tensor_tensor(out=c.hs[:, 0:CW], in0=c.hl[:, CW:2 * CW], in1=c.hl[:, 0:CW], op=ALU.mult))
                    b.dma(out_d[tc][:, cs_], c.hs[:, 0:CW])

    def seq(si):
        bin_bc = accf[:, 0:3072]
        pt = accf[:, 3072:6144]
        b.dma(bin_bc, w["b_in"].partition_broadcast(128))

        def tA(ti):
            b.dma(c.xt[:], src_t[si][ti])
            x_to_xT(b, c)
            for j in range(6):
                c0 = j * 512
                wbf, wc0 = (c.wgu, c0) if c0 < 2048 else (c.wmB, c0 - 2048)
                b.mm(mm_group(nc, c.psA[:], [(c.xTb[:, kk, :], wbf[:, kk, wc0:wc0 + 512]) for kk in range(8)]))
                b.V(lambda c0=c0: nc.vector.tensor_tensor(out=pt[:, c0:c0 + 512], in0=c.psA[:], in1=bin_bc[:, c0:c0 + 512], op=ALU.add))
            b.dma(p_rows[ti], pt)

        b.loop(tps, tA, eng=nc.scalar)

        def tB(ti):
            pm = accf[:, 0:3072]
            p0 = accf[:, 3072:6144]
            pp = stgf[:, 0:3072]
            cw = stgf[:, 3072:5120].rearrange("p (a f) -> p a f", a=4)
            tmp = stgf[:, 5120:5632]
            b.dma(pm, p_m[ti])
            b.dma(p0, p_rows[ti])
            b.dma(pp, p_p[ti])
            for j in range(6):
                cs_ = slice(j * 512, (j + 1) * 512)
                for a in range(3):
                    b.dma(cw[:, a, :], w["conv_w"][a][cs_].partition_broadcast(128))
                b.dma(cw[:, 3, :], w["conv_b"][cs_].partition_broadcast(128))
                b.V(lambda cs_=cs_: nc.vector.tensor_tensor(out=tmp, in0=pm[:, cs_], in1=cw[:, 0, :], op=ALU.mult))
                b.V(lambda cs_=cs_: nc.vector.tensor_tensor(out=p0[:, cs_], in0=p0[:, cs_], in1=cw[:, 1, :], op=ALU.mult))
                b.V(lambda cs_=cs_: nc.vector.tensor_tensor(out=p0[:, cs_], in0=p0[:, cs_], in1=tmp, op=ALU.add))
                b.V(lambda cs_=cs_: nc.vector.tensor_tensor(out=tmp, in0=pp[:, cs_], in1=cw[:, 2, :], op=ALU.mult))
                b.V(lambda cs_=cs_: nc.vector.tensor_tensor(out=p0[:, cs_], in0=p0[:, cs_], in1=tmp, op=ALU.add))
                b.V(lambda cs_=cs_: nc.vector.tensor_tensor(out=p0[:, cs_], in0=p0[:, cs_], in1=cw[:, 3, :], op=ALU.add))
            b.V(lambda: nc.vector.tensor_copy(c.vbf[:], p0[:, 0:1024]))
            b.dma(c.hv_d[ti], c.vbf[:])
            b.dma(c.hx_d[0][ti], p0[:, 1024:2048])
            b.dma(c.hx_d[1][ti], p0[:, 2048:3072])

        for ti_ in range(tps):
            tB(ti_)
        conv(0, c.hv_d, c.hx_d[0], c.hz_d, True)
        conv(1, c.hz_d, c.hx_d[1], c.hx_d[2], False)
        load_ln(b, c, w["ln1_g"], w["ln1_b"])
        bout_bc = c.acc[:, 1, :]
        b.dma(bout_bc, w["b_out"].partition_broadcast(128))

        def tC(ti):
            b.dma(c.xt[:], src_t[si][ti])
            b.dma(c.acc[:, 0, :], c.hx_d[2][ti])
            out_proj_ln(b, c, c.acc[:, 0, :], c.wd, bout_bc, dst_t[si][ti])

        b.loop(tps, tC, eng=nc.scalar)

    b.loop(nseq, seq, eng=nc.scalar)


def build_program(ntok, nlayers=DEPTH, wl=DEPTH, L=4096, mixers=(0, 1, 2, 0), do_moe=True):
    nc = bass.Bass("TRN2", target_bir_lowering=False)
    es = ExitStack()
    c = Ctx()

    def inp(name, shape, dt=F32):
        return nc.dram_tensor(name, list(shape), dt, kind="ExternalInput").ap()

    x = inp("x", [ntok, D])
    W = {}
    ident = inp("ident", [128, 128])
    tri = inp("tri", [128, 3, 128])
    nA = sum(1 for m in mixers[:nlayers] if m == 0)
    if nA:
        W.update({"gla_w_in": inp("gla_w_in", [nA, D, 3104]), "gla_gate_w2": inp("gla_gate_w2", [nA, 2, 16, 512]),
                  "gla_gate_b": inp("gla_gate_b", [nA, 2, 512]), "gla_norm_g": inp("gla_norm_g", [nA, 256]),
                  "gla_w_out": inp("gla_w_out", [nA, D, D])})
    nB = sum(1 for m in mixers[:nlayers] if m == 1)
    if nB:
        tps_ = L // 128
        nfc_ = tps_ + 1
        W.update({"hy_w_in": inp("hy_w_in", [nB, D, 3 * D]), "hy_b_in": inp("hy_b_in", [nB, 3 * D]),
                  "hy_conv_w": inp("hy_conv_w", [nB, 3, 3 * D]), "hy_conv_b": inp("hy_conv_b", [nB, 3 * D]),
                  "hy_ffn_w1": inp("hy_ffn_w1", [nB, 33, 64]), "hy_ffn_b1": inp("hy_ffn_b1", [nB, 64]),
                  "hy_sin_freq": inp("hy_sin_freq", [nB, 2, 64]), "hy_ffn_w2": inp("hy_ffn_w2", [nB, 64, 64]),
                  "hy_ffn_b2": inp("hy_ffn_b2", [nB, 64]), "hy_ffn_w3": inp("hy_ffn_w3", [nB, 64, 4 * D]),
                  "hy_filt_bias": inp("hy_filt_bias", [nB, 2, D]), "hy_w_out": inp("hy_w_out", [nB, D, D]),
                  "hy_b_out": inp("hy_b_out", [nB, D]),
                  "hy_peT": inp("hy_peT", [33, L]), "hy_win": inp("hy_win", [L, D]),
                  "hy_dft_c": inp("hy_dft_c", [nfc_, 128, tps_, 128], BF16), "hy_dft_s": inp("hy_dft_s", [nfc_, 128, tps_, 128], BF16),
                  "hy_idft_c": inp("hy_idft_c", [tps_, 128, nfc_, 128], BF16), "hy_idft_s": inp("hy_idft_s", [tps_, 128, nfc_, 128], BF16)})
        c.hk_d = nc.dram_tensor("hk_d", [2, 2, tps_, 128, D], BF16, kind="Internal").ap()
        c.K_d = nc.dram_tensor("K_d", [2, 2, nfc_, 128, D], F32, kind="Internal").ap()
        c.p_d = nc.dram_tensor("p_d", [L + 2, 3 * D], F32, kind="Internal").ap()
        c.hv_d = nc.dram_tensor("hv_d", [tps_, 128, D], BF16, kind="Internal").ap()
        c.hz_d = nc.dram_tensor("hz_d", [tps_, 128, D], BF16, kind="Internal").ap()
        c.hx_d = nc.dram_tensor("hx_d", [3, tps_, 128, D], F32, kind="Internal").ap()
    nC = sum(1 for m in mixers[:nlayers] if m == 2)
    if nC:
        W.update({"na_w_in": inp("na_w_in", [nC, D, 3 * D]), "na_b_in": inp("na_b_in", [nC, 3 * D]),
                  "na_bias2": inp("na_bias2", [nC, 32, 64, 15, 64]), "na_w_out": inp("na_w_out", [nC, D, D]),
                  "na_b_out": inp("na_b_out", [nC, D])})
        c.qT_d = nc.dram_tensor("qT_d", [8, 128, ntok], BF16, kind="Internal").ap()
        c.kT_d = nc.dram_tensor("kT_d", [8, 128, ntok], BF16, kind="Internal").ap()
        c.v_d = nc.dram_tensor("v_d", [ntok, D], BF16, kind="Internal").ap()
    W.update({
        "ln1_g": inp("ln1_g", [wl, D]), "ln1_b": inp("ln1_b", [wl, D]),
        "ln2_g": inp("ln2_g", [wl, D]), "ln2_b": inp("ln2_b", [wl, D]),
        "moe_router_w": inp("moe_router_w", [wl, D, NE]), "moe_router_b": inp("moe_router_b", [wl, NE]),
        "moe_w_gu": inp("moe_w_gu", [wl, NE, D, 2 * D]), "moe_b_gu": inp("moe_b_gu", [wl, NE, 2 * D]),
        "moe_w_down": inp("moe_w_down", [wl, NE, D, D]), "moe_b_down": inp("moe_b_down", [wl, NE, D]),
    })
    y = nc.dram_tensor("y", [ntok, D], F32, kind="ExternalOutput").ap()
    c.xa_d = nc.dram_tensor("xa_d", [ntok, D], F32, kind="Internal").ap()
    c.xb_d = nc.dram_tensor("xb_d", [ntok, D], F32, kind="Internal").ap()
    c.acc_d = nc.dram_tensor("acc_d", [ntok, D], F32, kind="Internal").ap()
    c.xT_d = nc.dram_tensor("xT_d", [ntok // 128, 128, 8, 128], BF16, kind="Internal").ap()
    c.wgu_bf_d = nc.dram_tensor("wgu_bf_d", [NE, 128, 8, 2 * D], BF16, kind="Internal").ap()
    c.wd_bf_d = nc.dram_tensor("wd_bf_d", [NE, 128, 8, D], BF16, kind="Internal").ap()
    c.comb_d = nc.dram_tensor("comb_d", [ntok // 128, 128, D], F32, kind="Internal").ap()

    with es:
        b = Builder(nc, es)
        alloc_common(b, c)
        alloc_moe(b, c)
        alloc_mixer(b, c)
        if nC:
            alloc_na(b, c)
        b.dma(c.ident[:], ident)
        b.dma(c.tri[:], tri)
        cur = x
        cnt = [0, 0, 0]
        for li in range(nlayers):
            m = mixers[li]
            j = cnt[m]
            cnt[m] += 1
            mdst = c.xb_d if do_moe else (y if li == nlayers - 1 else c.xa_d)
            if m == 0:
                gw = {"w_in": W["gla_w_in"][j], "gate_w2": W["gla_gate_w2"][j], "gate_b": W["gla_gate_b"][j],
                      "norm_g": W["gla_norm_g"][j], "w_out": W["gla_w_out"][j], "ln1_g": W["ln1_g"][li], "ln1_b": W["ln1_b"][li]}
                gla_layer(b, c, cur, mdst, gw, ntok, L)
            elif m == 1:
                hw = {"w_in": W["hy_w_in"][j], "b_in": W["hy_b_in"][j], "conv_w": W["hy_conv_w"][j], "conv_b": W["hy_conv_b"][j],
                      "ffn_w1": W["hy_ffn_w1"][j], "ffn_b1": W["hy_ffn_b1"][j], "sin_freq": W["hy_sin_freq"][j],
                      "ffn_w2": W["hy_ffn_w2"][j], "ffn_b2": W["hy_ffn_b2"][j], "ffn_w3": W["hy_ffn_w3"][j],
                      "filt_bias": W["hy_filt_bias"][j], "w_out": W["hy_w_out"][j], "b_out": W["hy_b_out"][j],
                      "peT": W["hy_peT"], "win": W["hy_win"], "dft_c": W["hy_dft_c"], "dft_s": W["hy_dft_s"],
                      "idft_c": W["hy_idft_c"], "idft_s": W["hy_idft_s"], "ln1_g": W["ln1_g"][li], "ln1_b": W["ln1_b"][li]}
                hyena_layer(b, c, cur, mdst, hw, ntok, L)
            elif m == 2:
                nw = {"w_in": W["na_w_in"][j], "b_in": W["na_b_in"][j], "bias2": W["na_bias2"][j], "w_out": W["na_w_out"][j],
                      "b_out": W["na_b_out"][j], "ln1_g": W["ln1_g"][li], "ln1_b": W["ln1_b"][li]}
                na_layer(b, c, cur, mdst, nw, ntok, L)
            elif m == -1:
                mdst = cur
            else:
                raise NotImplementedError("Hyena mixer (FFT long convolution) is not implemented in this version")
            if not do_moe:
                cur = c.xa_d
                continue
            mo_in = mdst
            w = {"router_w": W["moe_router_w"][li], "router_b": W["moe_router_b"][li],
                 "w_gu": W["moe_w_gu"][li], "b_gu": W["moe_b_gu"][li],
                 "w_down": W["moe_w_down"][li], "b_down": W["moe_b_down"][li],
                 "ln2_g": W["ln2_g"][li], "ln2_b": W["ln2_b"][li]}
            dst = y if li == nlayers - 1 else c.xa_d
            moe_layer(b, c, li, mo_in, dst, w, ntok)
            cur = c.xa_d
        b.finish()
    return nc


def na_bias_layout(rpb):
    n, H = rpb.shape[0], rpb.shape[1]
    cq = np.arange(64)[:, None]
    jk = np.arange(64)[None, :]
    cs = np.clip(cq - 8, 0, 48)
    inwin = (jk >= cs) & (jk < cs + 16)
    dc = np.clip(jk - cq + 15, 0, 30)
    g = rpb[:, :, :, dc]
    g = np.transpose(g, (0, 1, 3, 2, 4))
    out = np.where(inwin[None, None, :, None, :], g, np.float32(-30000.0)).astype(np.float32)
    return np.ascontiguousarray(out)


NCORES = 4
SEQ = 4096
BATCH = 16


def kernel(**inputs):
    n = NCORES
    ntok = BATCH * SEQ // n
    nc = build_program(ntok, nlayers=DEPTH, wl=DEPTH, L=SEQ, mixers=(0, 1, 2, 0), do_moe=True)
    s_ = np.arange(128)[:, None]
    t_ = np.arange(128)[None, :]
    tri = np.stack([(s_ <= t_), (s_ >= t_), (s_ > t_)], 1).astype(np.float32)
    shared = {k: np.ascontiguousarray(np.asarray(v, dtype=np.float32)) for k, v in inputs.items()
              if k.startswith(("ln", "moe_", "gla_", "hy_")) or k in ("na_w_in", "na_b_in", "na_w_out", "na_b_out")}
    shared.update(hyena_consts(SEQ))
    shared["na_bias2"] = na_bias_layout(np.asarray(inputs["na_rpb"], dtype=np.float32))
    shared["ident"] = np.eye(128, dtype=np.float32)
    shared["tri"] = tri
    x = np.asarray(inputs["x"], dtype=np.float32).reshape(n, ntok, D)
    in_maps = [dict(shared, x=np.ascontiguousarray(x[i])) for i in range(n)]
    res = run_bass_kernel_spmd(nc, in_maps, core_ids=list(range(n)))
    out = np.concatenate([res.results[i]["y"] for i in range(n)], axis=0)
    return out.reshape(BATCH, SEQ, D).astype(np.float32)
```

```python
import math
from contextlib import ExitStack
import numpy as np
import concourse.bass as bass
import concourse.mybir as mybir
from concourse.bass_utils import run_bass_kernel_spmd

F32 = mybir.dt.float32
BF16 = mybir.dt.bfloat16
AF = mybir.ActivationFunctionType
ALU = mybir.AluOpType
AX = mybir.AxisListType

D = 1024
DEPTH = 4
ALPHA = (2 * DEPTH) ** 0.25
LN_EPS = 1e-5
NE = 32
TOPK = 4


FORCE_SYNC = False
MOE_NEXP = 32


class Builder:
    def __init__(self, nc, es):
        self.nc = nc
        self.es = es
        self.levels = []
        self.depth = 0
        self._level(0)
        self.g = 0
        self.d = 0
        self.stack = []
        self.dummy = es.enter_context(nc.sbuf_tensor("sb_dummy", [128, 8], F32))
        self.engs = [nc.tensor, nc.vector, nc.scalar, nc.gpsimd, nc.sync]
        self.waited = {}
        self.a = 0
        self.a_need = 0
        self.dma_rr = 0
        self.nloops = 0
        self.loop_eng = nc.sync
        self.inloop = False

    def _level(self, k):
        while len(self.levels) <= k:
            n = len(self.levels)
            gs = self.es.enter_context(self.nc.semaphore(f"gsem{n}"))
            ds = self.es.enter_context(self.nc.semaphore(f"dsem{n}"))
            it = self.es.enter_context(self.nc.semaphore(f"isem{n}"))
            asm = self.es.enter_context(self.nc.semaphore(f"asem{n}"))
            self.levels.append((gs, ds, it, asm))
        return self.levels[k]

    def sb(self, name, shape, dt=F32):
        return self.es.enter_context(self.nc.sbuf_tensor("sb_" + name, list(shape), dt))

    def ps(self, name, shape, dt=F32):
        return self.es.enter_context(self.nc.psum_tensor("ps_" + name, list(shape), dt))

    def _wait(self, eng):
        gs, ds, _, asm = self.levels[self.depth]
        key = id(eng)
        lg, ld, la = self.waited.get(key, (-1, -1, 0))
        if self.g > lg:
            eng.wait_ge(gs, self.g)
        if self.d > ld:
            eng.wait_ge(ds, self.d)
        if self.a_need > la:
            eng.wait_ge(asm, self.a_need)
        self.waited[key] = (max(lg, self.g), max(ld, self.d), max(la, self.a_need))

    def dma_async(self, out, in_):
        eng = self.loop_eng if self.inloop else self.nc.sync
        self._wait(eng)
        eng.dma_start(out=out, in_=in_).then_inc(self.levels[self.depth][3], 16)
        self.a += 16
        return self.a

    def need(self, tok):
        self.a_need = max(self.a_need, tok)

    def op(self, eng, fn):
        self._wait(eng)
        fn().then_inc(self.levels[self.depth][0], 1)
        self.g += 1

    def V(self, fn):
        self.op(self.nc.vector, fn)

    def A(self, fn):
        self.op(self.nc.scalar, fn)

    def G(self, fn):
        self.op(self.nc.gpsimd, fn)

    def mm(self, fns):
        self._wait(self.nc.tensor)
        for f in fns[:-1]:
            f()
        fns[-1]().then_inc(self.levels[self.depth][0], 1)
        self.g += 1

    def dma(self, out, in_, slow=False):
        if not self.inloop:
            self.dma_rr = (self.dma_rr + 1) % 3
            eng = [self.nc.sync, self.nc.scalar, self.nc.sync][self.dma_rr]
        else:
            eng = self.loop_eng
        if FORCE_SYNC == "act":
            eng = self.nc.scalar
        elif FORCE_SYNC:
            eng = self.nc.sync
        self._wait(eng)
        if slow:
            eng.dma_start(out=out, in_=in_, allow_slow_non_contiguous=True).then_inc(self.levels[self.depth][1], 16)
        else:
            eng.dma_start(out=out, in_=in_).then_inc(self.levels[self.depth][1], 16)
        self.d += 16

    def loop(self, n, body, eng=None):
        prev_eng = self.loop_eng
        if eng is not None:
            self.loop_eng = eng
        try:
            self._loop(n, body)
        finally:
            self.loop_eng = prev_eng

    def _loop(self, n, body):
        if n == 1:
            old_depth_marker = self.inloop
            self.inloop = True
            body(0)
            self.inloop = old_depth_marker
            return
        nc = self.nc
        for e in self.engs:
            self._wait(e)
        og, od, odepth, owaited = self.g, self.d, self.depth, self.waited
        oa, oan = self.a, self.a_need
        self.depth += 1
        gs, ds, it, asm = self._level(self.depth)
        self.nloops += 1
        tag = self.nloops
        old_inloop = self.inloop
        self.inloop = True
        engines = mybir.ALL_ENGINES
        loop_start = f"L{tag}_loop"
        loop_end = f"L{tag}_end"
        registers = nc.alloc_registers(f"L{tag}_i", engines=engines)
        nc.regs_mov(registers, 0)
        nc.br(loop_start, engines=engines)
        with nc.body(loop_start, valid_engines=engines):
            i = nc.snap(registers, min_val=0, max_val=n - 1)
            for e in self.engs:
                e.wait_ge(it, i)
            self.g = 0
            self.d = 0
            self.a = 0
            self.a_need = 0
            self.waited = {}
            body(i)
            p = nc.gpsimd
            p.wait_ge(gs, self.g)
            p.wait_ge(ds, self.d)
            if self.a:
                p.wait_ge(asm, self.a)
                p.sem_clear(asm)
            p.sem_clear(gs)
            p.sem_clear(ds)
            p.memset(self.dummy[0:1, 0:1], 0.0).then_inc(it, 1)
            nc.regs_alu(registers, registers, 1, op=ALU.add)
            nc.br_lt(registers, n, on_true=loop_start, on_false=loop_end, engines=engines)
        nc.switch_bb(loop_end)
        for h in registers.handles:
            nc.free_register(h)
        for h in i.val.handles:
            nc.free_register(h)
        p = nc.gpsimd
        p.wait_ge(it, n)
        p.sem_clear(it)
        self.depth = odepth
        self.inloop = old_inloop
        self.g, self.d, self.waited = og, od, owaited
        self.a, self.a_need = oa, oan
        p.memset(self.dummy[0:1, 1:2], 0.0).then_inc(self.levels[self.depth][0], 1)
        self.g += 1

    def finish(self):
        for e in self.engs:
            self._wait(e)


def mm_group(nc, ps, pairs):
    fns = []
    n = len(pairs)
    for i, (l, r) in enumerate(pairs):
        fns.append(lambda l=l, r=r, i=i: nc.tensor.matmul(ps, l, r, start=(i == 0), stop=(i == n - 1)))
    return fns


class Ctx:
    pass


def alloc_common(b, c):
    c.ident = b.sb("ident", [128, 128], F32)
    c.xt = b.sb("xt", [128, D], F32)
    c.yt = b.sb("yt", [128, D], F32)
    c.gbc = b.sb("gbc", [128, D], F32)
    c.bbc = b.sb("bbc", [128, D], F32)
    c.stats = b.sb("stats", [128, 2, 6], F32)
    c.mv = b.sb("mv", [128, 2], F32)
    c.rstd = b.sb("rstd", [128, 1], F32)
    c.psA = b.ps("psA", [128, 512], F32)
    c.psB = b.ps("psB", [128, 512], F32)
    c.psC = b.ps("psC", [128, 512], F32)
    c.psD = b.ps("psD", [128, 512], F32)
    c.psT = b.ps("psT", [128, 512], F32)


def layer_norm(b, c, src, dst):
    nc = b.nc
    b.V(lambda: nc.vector.bn_stats(out=c.stats[:, 0, :], in_=src[:, 0:512]))
    b.V(lambda: nc.vector.bn_stats(out=c.stats[:, 1, :], in_=src[:, 512:1024]))
    b.V(lambda: nc.vector.bn_aggr(out=c.mv[:], in_=c.stats[:].rearrange("p a s -> p (a s)")))
    b.V(lambda: nc.vector.tensor_scalar(out=c.rstd[:], in0=c.mv[:, 1:2], scalar1=LN_EPS, scalar2=None, op0=ALU.add))
    b.A(lambda: nc.scalar.activation(out=c.rstd[:], in_=c.rstd[:], func=AF.Sqrt))
    b.V(lambda: nc.vector.reciprocal(out=c.rstd[:], in_=c.rstd[:]))
    b.V(lambda: nc.vector.tensor_scalar(out=dst, in0=src, scalar1=c.mv[:, 0:1], scalar2=c.rstd[:, 0:1],
                                        op0=ALU.subtract, op1=ALU.mult))
    b.V(lambda: nc.vector.tensor_tensor(out=dst, in0=dst, in1=c.gbc[:], op=ALU.mult))
    b.V(lambda: nc.vector.tensor_tensor(out=dst, in0=dst, in1=c.bbc[:], op=ALU.add))


def load_ln(b, c, g_ap, b_ap):
    b.dma(c.gbc[:], g_ap.partition_broadcast(128))
    b.dma(c.bbc[:], b_ap.partition_broadcast(128))


def alloc_moe(b, c):
    c.xT32 = b.sb("xT32", [128, 8, 128], F32)
    c.xTb = b.sb("xTb", [128, 8, 128], BF16)
    c.wr = b.sb("wr", [128, 8, NE], F32)
    c.rb = b.sb("rb", [128, NE], F32)
    c.bd = b.sb("bd", [NE, D], F32)
    c.lg = b.sb("lg", [128, NE], F32)
    c.ex = b.sb("ex", [128, NE], F32)
    c.msk = b.sb("msk", [128, NE], F32)
    c.top8 = b.sb("top8", [128, 8], F32)
    c.nm = b.sb("nm", [128, 1], F32)
    c.ssum = b.sb("ssum", [128, 1], F32)
    c.combT = b.sb("combT", [NE, 128], F32)
    c.xblk = b.sb("xblk", [128, 8, 8, 128], BF16)
    c.acc = b.sb("acc", [128, 8, D], F32)
    c.cmball = b.sb("cmball", [128, 8, NE], F32)
    c.stg = b.sb("stg", [128, 4, 2048], F32)
    c.wgu = b.sb("wgu", [128, 8, 2048], BF16)
    c.wd = b.sb("wd", [128, 8, D], BF16)
    c.bguall = b.sb("bguall", [128, NE, 16], F32)
    c.bl1all = b.sb("bl1all", [128, NE, 8], F32)
    c.hg = b.sb("hg", [128, 512], F32)
    c.hs = b.sb("hs", [128, 512], F32)
    c.hl = b.sb("hl", [128, 512], F32)
    c.hT = b.sb("hT", [128, 8, 512], BF16)


def moe_layer(b, c, li, src_d, dst_d, w, ntok):
    nc = b.nc
    ntiles = ntok // 128
    nblk = ntok // 1024
    src_t = src_d.rearrange("(n p) d -> n p d", p=128)
    dst_t = dst_d.rearrange("(n p) d -> n p d", p=128)
    acc_t = c.acc_d.rearrange("(n p) d -> n p d", p=128)
    xT_t = c.xT_d
    comb_t = c.comb_d

    b.dma(c.wr[:], w["router_w"].rearrange("(k p) e -> p k e", p=128))
    b.dma(c.rb[:], w["router_b"].partition_broadcast(128))
    b.dma(c.bd[:], w["b_down"])

    def p1(t):
        b.dma(c.xt[:], src_t[t])
        for k in range(8):
            b.mm([lambda k=k: nc.tensor.transpose(c.psT[:, 0:128], c.xt[:, k * 128:(k + 1) * 128], c.ident[:])])
            b.V(lambda k=k: nc.vector.tensor_copy(c.xT32[:, k, :], c.psT[:, 0:128]))
            b.G(lambda k=k: nc.gpsimd.tensor_copy(c.xTb[:, k, :], c.xT32[:, k, :]))
        b.mm(mm_group(nc, c.psA[:, 0:NE], [(c.xT32[:, k, :], c.wr[:, k, :]) for k in range(8)]))
        b.V(lambda: nc.vector.tensor_tensor(out=c.lg[:], in0=c.psA[:, 0:NE], in1=c.rb[:], op=ALU.add))
        b.V(lambda: nc.vector.max(out=c.top8[:], in_=c.lg[:]))
        b.V(lambda: nc.vector.tensor_scalar(out=c.msk[:], in0=c.lg[:], scalar1=c.top8[:, 3:4], scalar2=None, op0=ALU.is_ge))
        b.V(lambda: nc.vector.tensor_scalar(out=c.nm[:], in0=c.top8[:, 0:1], scalar1=-1.0, scalar2=None, op0=ALU.mult))
        b.A(lambda: nc.scalar.activation(out=c.ex[:], in_=c.lg[:], func=AF.Exp, bias=c.nm[:, 0:1], scale=1.0))
        b.V(lambda: nc.vector.tensor_tensor(out=c.ex[:], in0=c.ex[:], in1=c.msk[:], op=ALU.mult))
        b.V(lambda: nc.vector.reduce_sum(out=c.ssum[:], in_=c.ex[:], axis=AX.X))
        b.V(lambda: nc.vector.reciprocal(out=c.ssum[:], in_=c.ssum[:]))
        b.V(lambda: nc.vector.tensor_scalar(out=c.ex[:], in0=c.ex[:], scalar1=c.ssum[:, 0:1], scalar2=None, op0=ALU.mult))
        b.dma(comb_t[t][:, 0:NE], c.ex[:])
        b.mm([lambda: nc.tensor.transpose(c.psT[0:NE, 0:128], c.ex[:], c.ident[:])])
        b.V(lambda: nc.vector.tensor_copy(c.combT[:], c.psT[0:NE, 0:128]))
        for h in range(2):
            ps = c.psA if h == 0 else c.psB
            b.mm([lambda ps=ps, h=h: nc.tensor.matmul(ps[:], c.combT[:], c.bd[:, h * 512:(h + 1) * 512], start=True, stop=True)])
            b.V(lambda ps=ps, h=h: nc.vector.scalar_tensor_tensor(out=c.yt[:, h * 512:(h + 1) * 512], in0=c.xt[:, h * 512:(h + 1) * 512],
                                                                 scalar=ALPHA, in1=ps[:], op0=ALU.mult, op1=ALU.add))
        b.dma(acc_t[t], c.yt[:])
        b.dma(xT_t[t], c.xTb[:])

    b.loop(ntiles, p1, eng=nc.sync)

    load_ln(b, c, w["ln2_g"], w["ln2_b"])
    dst_bb = dst_d.rearrange("(n t p) d -> n p t d", t=8, p=128)
    xT_b = xT_t.rearrange("(n t) p k j -> n p t k j", t=8)
    acc_b = c.acc_d.rearrange("(n t p) d -> n p t d", t=8, p=128)
    comb_b = c.comb_d.rearrange("(n t) p e -> n p t e", t=8)[:, :, :, 0:NE]
    bgu_v = w["b_gu"].rearrange("e (c p) -> p e c", p=128)
    for e0 in range(0, NE, 4):
        b.dma(c.bguall[:, e0:e0 + 4, :], bgu_v[:, e0:e0 + 4, :], slow=True)
    b.V(lambda: nc.vector.tensor_scalar(out=c.bl1all[:], in0=c.bguall[:, :, 8:16], scalar1=1.0, scalar2=None, op0=ALU.add))
    wgu = w["w_gu"].rearrange("e (k p) f -> e p k f", p=128)
    wdn = w["w_down"].rearrange("e (k p) f -> e p k f", p=128)

    for e in range(NE):
        for hh in range(2):
            b.dma(c.stg[:], wgu[e][:, hh * 4:(hh + 1) * 4, :])
            b.V(lambda hh=hh: nc.vector.tensor_copy(c.wgu[:, hh * 4:hh * 4 + 2, :], c.stg[:, 0:2, :]))
            b.G(lambda hh=hh: nc.gpsimd.tensor_copy(c.wgu[:, hh * 4 + 2:hh * 4 + 4, :], c.stg[:, 2:4, :]))
        b.dma(c.stg[:].rearrange("p a (h f) -> p (a h) f", h=2), wdn[e])
        b.V(lambda: nc.vector.tensor_copy(c.wd[:, 0:4, :], c.stg[:, 0:2, :].rearrange("p a (h f) -> p (a h) f", h=2)))
        b.G(lambda: nc.gpsimd.tensor_copy(c.wd[:, 4:8, :], c.stg[:, 2:4, :].rearrange("p a (h f) -> p (a h) f", h=2)))
        b.dma(c.wgu_bf_d[e], c.wgu[:])
        b.dma(c.wd_bf_d[e], c.wd[:])

    def blk(bi):
        b.dma(c.xblk[:], xT_b[bi])
        b.dma(c.acc[:], acc_b[bi])
        b.dma(c.cmball[:], comb_b[bi])
        b.V(lambda: nc.vector.tensor_scalar(out=c.cmball[:], in0=c.cmball[:], scalar1=1.0 / 1.702, scalar2=None, op0=ALU.mult))

        wbufs = [c.wgu[:], c.stg[:].rearrange("p a f -> p (a f)").bitcast(BF16).rearrange("p (k f) -> p k f", k=8)]
        toks = {}

        def expert(e):
            wg = wbufs[e % 2]
            if e == 0:
                b.dma(wg, c.wgu_bf_d[e])
            else:
                b.need(toks[e])
            if e + 1 < MOE_NEXP:
                toks[e + 1] = b.dma_async(wbufs[(e + 1) % 2], c.wgu_bf_d[e + 1])
            b.dma(c.wd[:], c.wd_bf_d[e])
            for tt in range(2):
                for j in range(8):
                    b.mm(mm_group(nc, c.psA[:], [(wg[:, k, j * 128:(j + 1) * 128],
                                                  c.xblk[:, tt * 4:(tt + 1) * 4, k, :]) for k in range(8)]))
                    b.mm(mm_group(nc, c.psB[:], [(wg[:, k, 1024 + j * 128:1024 + (j + 1) * 128],
                                                  c.xblk[:, tt * 4:(tt + 1) * 4, k, :]) for k in range(8)]))
                    b.V(lambda j=j, e=e: nc.vector.tensor_scalar(out=c.hg[:], in0=c.psA[:], scalar1=c.bguall[:, e, j:j + 1], scalar2=7.0,
                                                            op0=ALU.add, op1=ALU.min))
                    b.A(lambda: nc.scalar.activation(out=c.hs[:], in_=c.hg[:], func=AF.Silu, scale=1.702))
                    b.V(lambda j=j, e=e: nc.vector.tensor_scalar(out=c.hl[:], in0=c.psB[:], scalar1=c.bl1all[:, e, j:j + 1], scalar2=8.0,
                                                            op0=ALU.add, op1=ALU.min))
                    b.V(lambda j=j: nc.vector.scalar_tensor_tensor(out=c.hT[:, j, :], in0=c.hl[:], scalar=-6.0, in1=c.hs[:],
                                                                   op0=ALU.max, op1=ALU.mult))
                for ts in range(4):
                    ti = tt * 4 + ts
                    for h in range(2):
                        ps = c.psC if h == 0 else c.psD
                        b.mm(mm_group(nc, ps[:], [(c.hT[:, j, ts * 128:(ts + 1) * 128], c.wd[:, j, h * 512:(h + 1) * 512])
                                                  for j in range(8)]))
                        b.V(lambda ps=ps, ti=ti, h=h, e=e: nc.vector.scalar_tensor_tensor(
                            out=c.acc[:, ti, h * 512:(h + 1) * 512], in0=ps[:], scalar=c.cmball[:, ti, e:e + 1],
                            in1=c.acc[:, ti, h * 512:(h + 1) * 512], op0=ALU.mult, op1=ALU.add))

        for e in range(MOE_NEXP):
            expert(e)
        for ti in range(8):
            layer_norm(b, c, c.acc[:, ti, :], c.acc[:, ti, :])
        b.dma(dst_bb[bi], c.acc[:])

    b.loop(nblk, blk, eng=nc.scalar)


def load_cast_rows(b, c, dst_bf, src_d, ncols):
    nc = b.nc
    src = src_d.rearrange("(k p) f -> p k f", p=128)
    st = c.stg[:].rearrange("p a f -> p (a f)")
    for k in range(8):
        for c0 in range(0, ncols, 2048):
            w = min(2048, ncols - c0)
            b.dma(st[:, 0:w], src[:, k, c0:c0 + w])
            b.V(lambda k=k, c0=c0, w=w: nc.vector.tensor_copy(dst_bf[:, k, c0:c0 + w], st[:, 0:w]))


def x_to_xT(b, c):
    nc = b.nc
    for k in range(8):
        b.mm([lambda k=k: nc.tensor.transpose(c.psT[:, 0:128], c.xt[:, k * 128:(k + 1) * 128], c.ident[:])])
        b.V(lambda k=k: nc.vector.tensor_copy(c.xTb[:, k, :], c.psT[:, 0:128]))


def out_proj_ln(b, c, src, wout_bf, bias_bc, dst_ap):
    nc = b.nc
    for k in range(8):
        b.mm([lambda k=k: nc.tensor.transpose(c.psT[:, 0:128], src[:, k * 128:(k + 1) * 128], c.ident[:])])
        b.V(lambda k=k: nc.vector.tensor_copy(c.xTb[:, k, :], c.psT[:, 0:128]))
    for h in range(2):
        ps = c.psA if h == 0 else c.psB
        b.mm(mm_group(nc, ps[:], [(c.xTb[:, k, :], wout_bf[:, k, h * 512:(h + 1) * 512]) for k in range(8)]))
        b.V(lambda ps=ps, h=h: nc.vector.scalar_tensor_tensor(out=c.yt[:, h * 512:(h + 1) * 512], in0=c.xt[:, h * 512:(h + 1) * 512],
                                                             scalar=ALPHA, in1=ps[:], op0=ALU.mult, op1=ALU.add))
    if bias_bc is not None:
        b.V(lambda: nc.vector.tensor_tensor(out=c.yt[:], in0=c.yt[:], in1=bias_bc, op=ALU.add))
    layer_norm(b, c, c.yt[:], c.yt[:])
    b.dma(dst_ap, c.yt[:])


def alloc_mixer(b, c):
    c.wmB = b.sb("wmB", [128, 8, 1056], BF16)
    c.tri = b.sb("tri", [128, 3, 128], F32)
    sflat = c.stg[:].rearrange("p a f -> p (a f)")
    c.gw2 = sflat[0:32, 0:1024].rearrange("p (n f) -> p n f", n=2)
    c.gbb = sflat[:, 2048:2560]
    c.ngb = b.sb("ngb", [128, 256], F32)
    c.qT = b.sb("qT", [128, 4, 128], BF16)
    c.kT = b.sb("kT", [128, 4, 128], BF16)
    c.ktm = b.sb("ktm", [128, 512], BF16)
    c.vbf = b.sb("vbf", [128, D], BF16)
    c.scT = b.sb("scT", [128, 128], BF16)
    c.Sbf = b.sb("Sbf", [128, 4, 256], BF16)
    c.gcol = b.sb("gcol", [128, 4], F32)
    c.glT = b.sb("glT", [32, 128], F32)
    c.gl = b.sb("gl", [128, 32], F32)
    c.r4 = b.sb("r4", [128, 4], F32)
    c.hv = b.sb("hv", [128, 16], F32)


def gla_layer(b, c, src_d, dst_d, w, ntok, L):
    nc = b.nc
    nseq = ntok // L
    tps = L // 128
    src_t = src_d.rearrange("(s n p) d -> s n p d", p=128, n=tps)
    dst_t = dst_d.rearrange("(s n p) d -> s n p d", p=128, n=tps)
    of_t = c.acc_d.rearrange("(s n p) d -> s n p d", p=128, n=tps)
    load_cast_rows(b, c, c.wgu, w["w_in"][:, 0:2048], 2048)
    load_cast_rows(b, c, c.wmB, w["w_in"][:, 2048:3104], 1056)
    load_cast_rows(b, c, c.wd, w["w_out"], 1024)
    b.G(lambda: nc.gpsimd.memset(c.gw2, 0.0))
    b.dma(c.gw2[0:16, 0, :], w["gate_w2"][0])
    b.dma(c.gw2[16:32, 1, :], w["gate_w2"][1])
    b.dma(c.ngb[:], w["norm_g"].partition_broadcast(128))
    load_ln(b, c, w["ln1_g"], w["ln1_b"])
    og = c.acc[:, 0, :]
    q = c.acc[:, 1, 0:512]
    k_ = c.acc[:, 1, 512:1024]
    oacc = c.acc[:, 2, :]
    S = c.acc[:, 3, :].rearrange("p (h e) -> p h e", h=4)
    oft = c.acc[:, 4, :]
    sq = c.acc[:, 5, :]
    B = c.hg
    EB = c.hs
    EnB = c.hl

    def one_pass(direction):
        n = direction
        b.dma(c.gbb, w["gate_b"][n].partition_broadcast(128))

        def seq(si):
            b.V(lambda: nc.vector.memset(S, 0.0))
            b.V(lambda: nc.vector.memset(c.Sbf[:], 0.0))

            def tile(ti):
                t = ti if n == 0 else (tps - 1) - ti
                b.dma(c.xt[:], src_t[si][t])
                x_to_xT(b, c)
                def proj(ps, wbf, c0, wid):
                    b.mm(mm_group(nc, ps[:, 0:wid], [(c.xTb[:, kk, :], wbf[:, kk, c0:c0 + wid]) for kk in range(8)]))
                proj(c.psA, c.wgu, 0, 512)
                b.V(lambda: nc.vector.tensor_copy(q, c.psA[:]))
                proj(c.psA, c.wgu, 512, 512)
                b.V(lambda: nc.vector.tensor_copy(k_, c.psA[:]))
                proj(c.psA, c.wgu, 1024, 512)
                b.V(lambda: nc.vector.tensor_copy(c.vbf[:, 0:512], c.psA[:]))
                proj(c.psA, c.wgu, 1536, 512)
                b.V(lambda: nc.vector.tensor_copy(c.vbf[:, 512:1024], c.psA[:]))
                proj(c.psA, c.wmB, 1024, 32)
                b.V(lambda: nc.vector.tensor_copy(c.gl[:], c.psA[:, 0:32]))
                b.mm([lambda: nc.tensor.transpose(c.psT[0:32, 0:128], c.gl[:], c.ident[:])])
                b.V(lambda: nc.vector.tensor_copy(c.glT[:], c.psT[0:32, 0:128]))
                b.mm([lambda: nc.tensor.matmul(c.psA[:], c.glT[:], c.gw2[:, n, :], start=True, stop=True)])
                b.V(lambda: nc.vector.tensor_tensor(out=B[:], in0=c.psA[:], in1=c.gbb, op=ALU.add))
                b.A(lambda: nc.scalar.activation(out=B[:], in_=B[:], func=AF.Exp, scale=-1.0))
                b.A(lambda: nc.scalar.activation(out=B[:], in_=B[:], func=AF.Ln, bias=1.0, scale=1.0))
                b.mm([lambda: nc.tensor.matmul(c.psA[:], c.tri[:, n, :], B[:], start=True, stop=True)])
                b.A(lambda: nc.scalar.activation(out=EB[:], in_=c.psA[:], func=AF.Exp, scale=-1.0 / 16.0))
                b.A(lambda: nc.scalar.activation(out=EnB[:], in_=c.psA[:], func=AF.Exp, scale=1.0 / 16.0))
                b.V(lambda: nc.vector.scalar_tensor_tensor(out=q, in0=q, scalar=128.0 ** -0.5, in1=EB[:], op0=ALU.mult, op1=ALU.mult))
                b.V(lambda: nc.vector.tensor_tensor(out=k_, in0=k_, in1=EnB[:], op=ALU.mult))
                b.V(lambda: nc.vector.tensor_copy(c.ktm[:], k_))
                for h in range(4):
                    hs = slice(h * 128, (h + 1) * 128)
                    b.mm([lambda hs=hs: nc.tensor.transpose(c.psT[:, 0:128], q[:, hs], c.ident[:])])
                    b.V(lambda h=h: nc.vector.tensor_copy(c.qT[:, h, :], c.psT[:, 0:128]))
                    b.mm([lambda hs=hs: nc.tensor.transpose(c.psT[:, 0:128], k_[:, hs], c.ident[:])])
                    b.V(lambda h=h: nc.vector.tensor_copy(c.kT[:, h, :], c.psT[:, 0:128]))
                    b.mm([lambda hs=hs: nc.tensor.transpose(c.psT[:, 0:128], EB[:, hs], c.ident[:])])
                    col = 127 if n == 0 else 0
                    b.V(lambda h=h, col=col: nc.vector.tensor_copy(c.gcol[:, h:h + 1], c.psT[:, col:col + 1]))
                for h in range(4):
                    vs = slice(h * 256, (h + 1) * 256)
                    b.mm([lambda h=h: nc.tensor.matmul(c.psB[:, 0:128], c.kT[:, h, :], c.qT[:, h, :], start=True, stop=True)])
                    mk = 0 if n == 0 else 2
                    b.V(lambda mk=mk: nc.vector.tensor_tensor(out=c.scT[:], in0=c.psB[:, 0:128], in1=c.tri[:, mk, :], op=ALU.mult))
                    b.mm(mm_group(nc, c.psC[:, 0:256], [(c.scT[:], c.vbf[:, vs]), (c.qT[:, h, :], c.Sbf[:, h, :])]))
                    b.V(lambda vs=vs: nc.vector.tensor_copy(oacc[:, vs], c.psC[:, 0:256]))
                    b.mm([lambda h=h, vs=vs: nc.tensor.matmul(c.psD[:, 0:256], c.ktm[:, h * 128:(h + 1) * 128], c.vbf[:, vs], start=True, stop=True)])
                    b.V(lambda h=h: nc.vector.tensor_tensor(out=S[:, h, :], in0=c.psD[:, 0:256], in1=S[:, h, :], op=ALU.add))
                    b.V(lambda h=h: nc.vector.tensor_scalar(out=S[:, h, :], in0=S[:, h, :], scalar1=c.gcol[:, h:h + 1], scalar2=None, op0=ALU.mult))
                    b.V(lambda h=h: nc.vector.tensor_copy(c.Sbf[:, h, :], S[:, h, :]))
                if n == 0:
                    b.dma(of_t[si][t], oacc)
                else:
                    b.dma(oft, of_t[si][t])
                    b.V(lambda: nc.vector.tensor_tensor(out=oacc, in0=oacc, in1=oft, op=ALU.add))
                    b.V(lambda: nc.vector.tensor_tensor(out=sq, in0=oacc, in1=oacc, op=ALU.mult))
                    b.V(lambda: nc.vector.reduce_sum(out=c.r4[:], in_=sq.rearrange("p (h e) -> p h e", h=4), axis=AX.X))
                    b.V(lambda: nc.vector.tensor_scalar(out=c.r4[:], in0=c.r4[:], scalar1=1.0 / 256.0, scalar2=1e-6, op0=ALU.mult, op1=ALU.add))
                    b.A(lambda: nc.scalar.activation(out=c.r4[:], in_=c.r4[:], func=AF.Sqrt))
                    b.V(lambda: nc.vector.reciprocal(out=c.r4[:], in_=c.r4[:]))
                    for h in range(4):
                        vs = slice(h * 256, (h + 1) * 256)
                        b.V(lambda h=h, vs=vs: nc.vector.scalar_tensor_tensor(out=oacc[:, vs], in0=oacc[:, vs], scalar=c.r4[:, h:h + 1],
                                                                           in1=c.ngb[:], op0=ALU.mult, op1=ALU.mult))
                    for hh in range(2):
                        proj(c.psA, c.wmB, hh * 512, 512)
                        b.V(lambda hh=hh: nc.vector.tensor_copy(og[:, hh * 512:(hh + 1) * 512], c.psA[:]))
                    b.A(lambda: nc.scalar.activation(out=sq, in_=og, func=AF.Sigmoid))
                    b.V(lambda: nc.vector.tensor_tensor(out=sq, in0=sq, in1=og, op=ALU.mult))
                    b.V(lambda: nc.vector.tensor_tensor(out=oacc, in0=oacc, in1=sq, op=ALU.mult))
                    out_proj_ln(b, c, oacc, c.wd, None, dst_t[si][t])

            b.loop(tps, tile)

        b.loop(nseq, seq, eng=nc.sync)

    one_pass(0)
    one_pass(1)


def alloc_na(b, c):
    xflat = c.xblk[:].rearrange("p a k t -> p (a k t)")
    sflat = c.stg[:].rearrange("p a f -> p (a f)")
    c.nqT = xflat[0:32, 0:4096]
    c.nkT = xflat[0:32, 4096:8192]
    c.nv = c.hT[:].rearrange("p a t -> p (a t)")[0:64, 0:2048].rearrange("p (r e) -> p r e", e=32)
    c.no = sflat[0:64, 0:2048].rearrange("p (r e) -> p r e", e=32)
    c.nbias = sflat[0:64, 2048:3008].rearrange("p (a j) -> p a j", j=64)
    c.nsc = sflat[0:64, 4096:4608]
    c.nP = b.sb("nP", [64, 512], BF16)
    c.nPT = b.sb("nPT", [64, 8, 64], BF16)
    c.nmx = b.sb("nmx", [64, 1], F32)
    c.nsm = b.sb("nsm", [64, 1], F32)
    c.identb = b.sb("identb", [128, 128], BF16)
    c.qkb = c.hl[:].bitcast(BF16)
    c.psTb = b.ps("psTb", [128, 1024], BF16)


def na_layer(b, c, src_d, dst_d, w, ntok, L):
    nc = b.nc
    nseq = ntok // L
    tps = L // 128
    R = L // 64
    src_t = src_d.rearrange("(n p) d -> n p d", p=128)
    dst_t = dst_d.rearrange("(n p) d -> n p d", p=128)
    o_t = c.acc_d.rearrange("(n p) d -> n p d", p=128)
    o_h = c.acc_d.rearrange("(s r p) (h e) -> s h p r e", r=R, p=64, e=32)
    qT_d = c.qT_d.rearrange("g (a e) t -> (g a) e t", e=32)
    kT_d = c.kT_d.rearrange("g (a e) t -> (g a) e t", e=32)
    qT_w = c.qT_d.rearrange("g p (n t) -> n g p t", t=128)
    kT_w = c.kT_d.rearrange("g p (n t) -> n g p t", t=128)
    v_w = c.v_d.rearrange("(n p) d -> n p d", p=128)
    v_h = c.v_d.rearrange("(s r p) (h e) -> s h p r e", r=R, p=64, e=32)
    load_cast_rows(b, c, c.wgu, w["w_in"][:, 0:2048], 2048)
    load_cast_rows(b, c, c.wmB, w["w_in"][:, 2048:3072], 1024)
    load_cast_rows(b, c, c.wd, w["w_out"], 1024)
    load_ln(b, c, w["ln1_g"], w["ln1_b"])
    bin_bc = c.acc[:, 0:3, :].rearrange("p a d -> p (a d)")
    bout_bc = c.acc[:, 3, :]
    pq = c.acc[:, 4, :]
    b.dma(bin_bc, w["b_in"].partition_broadcast(128))
    b.dma(bout_bc, w["b_out"].partition_broadcast(128))
    b.V(lambda: nc.vector.tensor_copy(c.identb[:], c.ident[:]))

    def p1(t):
        b.dma(c.xt[:], src_t[t])
        x_to_xT(b, c)
        for part in range(3):
            for hh in range(2):
                c0 = part * 1024 + hh * 512
                wbf, wc0 = (c.wgu, c0) if c0 < 2048 else (c.wmB, c0 - 2048)
                b.mm(mm_group(nc, c.psA[:], [(c.xTb[:, kk, :], wbf[:, kk, wc0:wc0 + 512]) for kk in range(8)]))
                b.V(lambda hh=hh, c0=c0: nc.vector.tensor_tensor(out=pq[:, hh * 512:(hh + 1) * 512], in0=c.psA[:],
                                                               in1=bin_bc[:, c0:c0 + 512], op=ALU.add))
            if part == 0:
                b.V(lambda: nc.vector.tensor_scalar(out=c.qkb, in0=pq, scalar1=32.0 ** -0.5, scalar2=None, op0=ALU.mult))
            else:
                b.V(lambda: nc.vector.tensor_copy(c.qkb, pq))
            if part == 2:
                b.dma(v_w[t], c.qkb)
            else:
                for g in range(8):
                    b.mm([lambda g=g: nc.tensor.transpose(c.psTb[:, g * 128:(g + 1) * 128], c.qkb[:, g * 128:(g + 1) * 128], c.identb[:])])
                b.V(lambda: nc.vector.tensor_copy(c.vbf[:], c.psTb[:]))
                dstw = qT_w if part == 0 else kT_w
                b.dma(dstw[t].rearrange("g p t -> p g t"), c.vbf[:].rearrange("p (g t) -> p g t", g=8))

    b.loop(ntok // 128, p1, eng=nc.sync)

    def seq(si):
        def head(h):
            b.dma(c.nqT[:, 0:L], qT_d[h].rearrange("e (s t) -> s e t", t=L)[si])
            b.dma(c.nkT[:, 0:L], kT_d[h].rearrange("e (s t) -> s e t", t=L)[si])
            b.dma(c.nv[:, 0:R, :], v_h[si][h])
            b.dma(c.nbias, w["bias2"][h])
            for r in range(R):
                rs = min(max(r - 4, 0), R - 8)
                dr0 = rs - r + 7
                b.mm([lambda r=r, rs=rs: nc.tensor.matmul(c.psA[0:64, :], c.nqT[:, r * 64:(r + 1) * 64], c.nkT[:, rs * 64:rs * 64 + 512],
                                                          start=True, stop=True)])
                b.V(lambda dr0=dr0: nc.vector.tensor_tensor(out=c.nsc, in0=c.psA[0:64, :],
                                                            in1=c.nbias[:, dr0:dr0 + 8, :].rearrange("p a j -> p (a j)"), op=ALU.add))
                b.V(lambda: nc.vector.reduce_max(out=c.nmx[:], in_=c.nsc, axis=AX.X))
                b.V(lambda: nc.vector.tensor_scalar(out=c.nmx[:], in0=c.nmx[:], scalar1=-1.0, scalar2=None, op0=ALU.mult))
                b.A(lambda: nc.scalar.activation(out=c.nsc, in_=c.nsc, func=AF.Exp, bias=c.nmx[:, 0:1], scale=1.0))
                b.V(lambda: nc.vector.reduce_sum(out=c.nsm[:], in_=c.nsc, axis=AX.X))
                b.V(lambda: nc.vector.reciprocal(out=c.nsm[:], in_=c.nsm[:]))
                b.V(lambda: nc.vector.tensor_copy(c.nP[:], c.nsc))
                for a in range(8):
                    b.mm([lambda a=a: nc.tensor.transpose(c.psTb[0:64, a * 64:(a + 1) * 64], c.nP[:, a * 64:(a + 1) * 64], c.identb[0:64, 0:64])])
                b.V(lambda: nc.vector.tensor_copy(c.nPT[:].rearrange("p a q -> p (a q)"), c.psTb[0:64, 0:512]))
                b.mm(mm_group(nc, c.psB[0:64, 0:32], [(c.nPT[:, a, :], c.nv[:, rs + a, :]) for a in range(8)]))
                b.V(lambda r=r: nc.vector.tensor_scalar(out=c.no[:, r, :], in0=c.psB[0:64, 0:32], scalar1=c.nsm[:, 0:1], scalar2=None, op0=ALU.mult))
            b.dma(o_h[si][h], c.no[:, 0:R, :])

        b.loop(32, head)

    b.loop(nseq, seq, eng=nc.scalar)

    def p3(t):
        b.dma(c.xt[:], src_t[t])
        b.dma(pq, o_t[t])
        out_proj_ln(b, c, pq, c.wd, bout_bc, dst_t[t])

    b.loop(ntok // 128, p3, eng=nc.sync)


def hyena_consts(L):
    import ml_dtypes
    N = 2 * L
    nfc = L // 128 + 1
    NF = nfc * 128
    t = np.arange(L, dtype=np.int64)
    f = np.arange(NF, dtype=np.int64)
    ang = 2.0 * np.pi * ((t[:, None] * f[None, :]) % N).astype(np.float64) / N
    cs, sn = np.cos(ang), np.sin(ang)
    wf = np.where((f == 0) | (f == L), 1.0, 2.0) / N
    wf[f > L] = 0.0
    bf = ml_dtypes.bfloat16
    bf = ml_dtypes.bfloat16
    tps = L // 128

    def fwd_blk(m):
        return np.ascontiguousarray(m.reshape(tps, 128, nfc, 128).transpose(2, 1, 0, 3)).astype(np.float32).astype(bf)

    def inv_blk(m):
        return np.ascontiguousarray(m.reshape(nfc, 128, tps, 128).transpose(2, 1, 0, 3)).astype(np.float32).astype(bf)

    out = {"hy_dft_c": fwd_blk(cs), "hy_dft_s": fwd_blk(sn),
           "hy_idft_c": inv_blk((cs * wf[None, :]).T), "hy_idft_s": inv_blk((sn * wf[None, :]).T)}
    tf = np.arange(L, dtype=np.float32)
    t_norm = tf / np.float32(max(L - 1, 1))
    freqs = np.linspace(1e-4, 15, 16, dtype=np.float32)
    a2 = (np.float32(2.0 * math.pi / L) * tf[:, None] * freqs[None, :]).astype(np.float32)
    pe = np.concatenate([t_norm[:, None], np.cos(a2), -np.sin(a2)], axis=-1).astype(np.float32)
    out["hy_peT"] = np.ascontiguousarray(pe.T)
    min_decay = math.log(1e-2) / 1.5
    max_decay = math.log(1e-2) / 0.3
    deltas = np.abs(np.linspace(min_decay, max_decay, D, dtype=np.float32))
    out["hy_win"] = (np.exp(-t_norm[:, None] * deltas[None, :]) + np.float32(0.05)).astype(np.float32)
    return out


def hyena_layer(b, c, src_d, dst_d, w, ntok, L):
    nc = b.nc
    nseq = ntok // L
    tps = L // 128
    nfc = tps + 1
    CW = 256
    nct = D // CW
    src_t = src_d.rearrange("(s n p) d -> s n p d", p=128, n=tps)
    dst_t = dst_d.rearrange("(s n p) d -> s n p d", p=128, n=tps)
    accf = c.acc[:].rearrange("p a d -> p (a d)")
    stgf = c.stg[:].rearrange("p a f -> p (a f)")
    accb = accf.bitcast(BF16)
    stgb = stgf.bitcast(BF16)
    zt = c.xblk[:].rearrange("p a k t -> p (a k t)")[:, 0:tps * CW].rearrange("p (t c) -> p t c", c=CW)
    hv = c.hv

    w1 = c.xt[0:33, 0:64]
    w2 = c.xt[0:64, 64:128]
    w3 = accf[0:64, 0:4096]
    peT = accf[0:33, 4096:4096 + L]
    b.dma(w1, w["ffn_w1"])
    b.dma(w2, w["ffn_w2"])
    b.dma(w3, w["ffn_w3"])
    b.dma(peT, w["peT"])
    b.dma(hv[0:64, 0:1], w["ffn_b1"].rearrange("(p o) -> p o", o=1))
    b.dma(hv[0:64, 1:2], w["ffn_b2"].rearrange("(p o) -> p o", o=1))
    b.dma(hv[0:64, 2:3], w["sin_freq"][0].rearrange("(p o) -> p o", o=1))
    b.dma(hv[0:64, 3:4], w["sin_freq"][1].rearrange("(p o) -> p o", o=1))
    for i in range(2):
        b.V(lambda i=i: nc.vector.tensor_scalar(out=hv[0:64, 4 + i:5 + i], in0=hv[0:64, 2 + i:3 + i], scalar1=0.125, scalar2=None, op0=ALU.mult))
        b.V(lambda i=i: nc.vector.tensor_tensor(out=hv[0:64, 6 + i:7 + i], in0=hv[0:64, 4 + i:5 + i], in1=hv[0:64, i:i + 1], op=ALU.mult))
        b.V(lambda i=i: nc.vector.tensor_scalar(out=hv[0:64, 8 + i:9 + i], in0=hv[0:64, 6 + i:7 + i], scalar1=math.pi / 2, scalar2=None, op0=ALU.add))
    m0 = hv[:, 10:11]
    b.V(lambda: nc.vector.tensor_scalar(out=m0, in0=c.ident[:, 0:1], scalar1=-1.0, scalar2=1.0, op0=ALU.mult, op1=ALU.add))

    def sin_layer(ps, i, out):
        S, C, T = out, c.hl[0:64, :], c.yt[0:64, 0:512]
        b.A(lambda: nc.scalar.activation(out=S, in_=ps, func=AF.Sin, bias=hv[0:64, 6 + i:7 + i], scale=hv[0:64, 4 + i:5 + i]))
        b.A(lambda: nc.scalar.activation(out=C, in_=ps, func=AF.Sin, bias=hv[0:64, 8 + i:9 + i], scale=hv[0:64, 4 + i:5 + i]))
        for _ in range(3):
            b.V(lambda: nc.vector.tensor_tensor(out=T, in0=S, in1=S, op=ALU.mult))
            b.V(lambda: nc.vector.scalar_tensor_tensor(out=S, in0=S, scalar=2.0, in1=C, op0=ALU.mult, op1=ALU.mult))
            b.V(lambda: nc.vector.tensor_scalar(out=C, in0=T, scalar1=-2.0, scalar2=1.0, op0=ALU.mult, op1=ALU.add))

    h3w = stgf[:, 0:4096]
    wint = stgf[:, 4096:5120]
    for q in range(L // 512):
        b.mm([lambda q=q: nc.tensor.matmul(c.psA[0:64, :], w1, peT[:, q * 512:(q + 1) * 512], start=True, stop=True)])
        sin_layer(c.psA[0:64, :], 0, c.hg[0:64, :])
        b.mm([lambda: nc.tensor.matmul(c.psB[0:64, :], w2, c.hg[0:64, :], start=True, stop=True)])
        sin_layer(c.psB[0:64, :], 1, c.hs[0:64, :])
        for tt in range(4):
            t = q * 4 + tt
            b.dma(wint, w["win"][t * 128:(t + 1) * 128, :])
            for cb in range(8):
                b.mm([lambda tt=tt, cb=cb: nc.tensor.matmul(c.psC[:], c.hs[0:64, tt * 128:(tt + 1) * 128], w3[:, cb * 512:(cb + 1) * 512],
                                                         start=True, stop=True)])
                b.V(lambda cb=cb: nc.vector.tensor_tensor(out=h3w[:, cb * 512:(cb + 1) * 512], in0=c.psC[:],
                                                         in1=wint[:, (cb % 2) * 512:(cb % 2 + 1) * 512], op=ALU.mult))
            for o in range(2):
                hf = h3w[:, o * 2048:o * 2048 + 1024]
                hb = h3w[:, o * 2048 + 1024:(o + 1) * 2048]
                if t == 0:
                    b.V(lambda hb=hb: nc.vector.tensor_scalar(out=hb, in0=hb, scalar1=m0, scalar2=None, op0=ALU.mult))
                b.V(lambda hf=hf, hb=hb: nc.vector.tensor_tensor(out=c.vbf[:], in0=hf, in1=hb, op=ALU.add))
                b.dma(c.hk_d[o][0][t], c.vbf[:])
                b.V(lambda hf=hf, hb=hb: nc.vector.tensor_tensor(out=c.vbf[:], in0=hf, in1=hb, op=ALU.subtract))
                b.dma(c.hk_d[o][1][t], c.vbf[:])

    dcv, dsv, icv, isv = w["dft_c"], w["dft_s"], w["idft_c"], w["idft_s"]
    dcb = accb[:, 8448:8448 + tps * 128].rearrange("p (t f) -> p t f", f=128)
    dsb = stgb[:, 8448:8448 + tps * 128].rearrange("p (t f) -> p t f", f=128)
    icb = accb[:, 8448:8448 + nfc * 128].rearrange("p (a t) -> p a t", t=128)
    isb = stgb[:, 8448:8448 + nfc * 128].rearrange("p (a t) -> p a t", t=128)
    Yr = accb[:, 0:nfc * CW].rearrange("p (a c) -> p a c", c=CW)
    Ys = stgb[:, 0:nfc * CW].rearrange("p (a c) -> p a c", c=CW)
    zt2 = accb[:, 0:tps * CW].rearrange("p (t c) -> p t c", c=CW)
    for o in range(2):
        for ct in range(nct):
            cs_ = slice(ct * CW, (ct + 1) * CW)
            b.dma(zt, c.hk_d[o][0].rearrange("t p c -> p t c")[:, :, cs_])
            b.dma(zt2, c.hk_d[o][1].rearrange("t p c -> p t c")[:, :, cs_])
            for fc in range(nfc):
                b.dma(dcb, dcv[fc])
                b.dma(dsb, dsv[fc])
                b.mm(mm_group(nc, c.psA[:, 0:CW], [(dcb[:, tc, :], zt[:, tc, :]) for tc in range(tps)]))
                b.mm(mm_group(nc, c.psB[:, 0:CW], [(dsb[:, tc, :], zt2[:, tc, :]) for tc in range(tps)]))
                b.V(lambda: nc.vector.tensor_copy(c.hg[:, 0:CW], c.psA[:, 0:CW]))
                b.V(lambda: nc.vector.tensor_copy(c.hg[:, CW:2 * CW], c.psB[:, 0:CW]))
                b.dma(c.K_d[o][0][fc][:, cs_], c.hg[:, 0:CW])
                b.dma(c.K_d[o][1][fc][:, cs_], c.hg[:, CW:2 * CW])

    load_cast_rows(b, c, c.wgu, w["w_in"][:, 0:2048], 2048)
    load_cast_rows(b, c, c.wmB, w["w_in"][:, 2048:3072], 1024)
    load_cast_rows(b, c, c.wd, w["w_out"], 1024)
    b.G(lambda: nc.gpsimd.memset(c.yt[:], 0.0))
    for j3 in range(3):
        b.dma(c.p_d[0:1, j3 * 1024:(j3 + 1) * 1024], c.yt[0:1, :])
        b.dma(c.p_d[L + 1:L + 2, j3 * 1024:(j3 + 1) * 1024], c.yt[0:1, :])
    p_rows = c.p_d[1:L + 1, :].rearrange("(n p) c -> n p c", p=128)
    p_m = c.p_d[0:L, :].rearrange("(n p) c -> n p c", p=128)
    p_p = c.p_d[2:L + 2, :].rearrange("(n p) c -> n p c", p=128)

    def conv(o, in_d, gate_d, out_d, out_bf):
        b.dma(c.gbc[:], w["filt_bias"][o].partition_broadcast(128))
        in_v = in_d.rearrange("t p c -> p t c")
        for ct in range(nct):
            cs_ = slice(ct * CW, (ct + 1) * CW)
            b.dma(zt, in_v[:, :, cs_])
            for fc in range(nfc):
                b.dma(dcb, dcv[fc])
                b.dma(dsb, dsv[fc])
                b.dma(c.hg[:, 0:CW], c.K_d[o][0][fc][:, cs_])
                b.dma(c.hg[:, CW:2 * CW], c.K_d[o][1][fc][:, cs_])
                b.mm(mm_group(nc, c.psA[:, 0:CW], [(dcb[:, tc, :], zt[:, tc, :]) for tc in range(tps)]))
                b.mm(mm_group(nc, c.psB[:, 0:CW], [(dsb[:, tc, :], zt[:, tc, :]) for tc in range(tps)]))
                b.V(lambda: nc.vector.tensor_tensor(out=c.hs[:, 0:CW], in0=c.psA[:, 0:CW], in1=c.hg[:, 0:CW], op=ALU.mult))
                b.V(lambda: nc.vector.tensor_tensor(out=c.hs[:, CW:2 * CW], in0=c.psB[:, 0:CW], in1=c.hg[:, CW:2 * CW], op=ALU.mult))
                b.V(lambda fc=fc: nc.vector.tensor_tensor(out=Yr[:, fc, :], in0=c.hs[:, 0:CW], in1=c.hs[:, CW:2 * CW], op=ALU.subtract))
                b.V(lambda: nc.vector.tensor_tensor(out=c.hs[:, 0:CW], in0=c.psA[:, 0:CW], in1=c.hg[:, CW:2 * CW], op=ALU.mult))
                b.V(lambda: nc.vector.tensor_tensor(out=c.hs[:, CW:2 * CW], in0=c.psB[:, 0:CW], in1=c.hg[:, 0:CW], op=ALU.mult))
                b.V(lambda fc=fc: nc.vector.tensor_tensor(out=Ys[:, fc, :], in0=c.hs[:, 0:CW], in1=c.hs[:, CW:2 * CW], op=ALU.add))
            for tc in range(tps):
                b.dma(icb, icv[tc])
                b.dma(isb, isv[tc])
                b.dma(c.hl[:, 0:CW], gate_d[tc][:, cs_])
                pairs = []
                for fc in range(nfc):
                    pairs.append((icb[:, fc, :], Yr[:, fc, :]))
                    pairs.append((isb[:, fc, :], Ys[:, fc, :]))
                b.mm(mm_group(nc, c.psC[:, 0:CW], pairs))
                b.V(lambda tc=tc, cs_=cs_: nc.vector.tensor_tensor(out=c.hl[:, CW:2 * CW], in0=zt[:, tc, :], in1=c.gbc[:, cs_], op=ALU.mult))
                b.V(lambda: nc.vector.tensor_tensor(out=c.hl[:, CW:2 * CW], in0=c.psC[:, 0:CW], in1=c.hl[:, CW:2 * CW], op=ALU.add))
                if out_bf:
                    b.V(lambda: nc.vector.tensor_tensor(out=c.vbf[:, 0:CW], in0=c.hl[:, CW:2 * CW], in1=c.hl[:, 0:CW], op=ALU.mult))
                    b.dma(out_d[tc][:, cs_], c.vbf[:, 0:CW])
                else:
                    b.V(lambda: nc.vector.tensor_tensor(out=c.hs[:, 0:CW], in0=c.hl[:, CW:2 * CW], in1=c.hl[:, 0:CW], op=ALU.mult))
                    b.dma(out_d[tc][:, cs_], c.hs[:, 0:CW])

    def seq(si):
        bin_bc = accf[:, 0:3072]
        pt = accf[:, 3072:6144]
        b.dma(bin_bc, w["b_in"].partition_broadcast(128))

        def tA(ti):
            b.dma(c.xt[:], src_t[si][ti])
            x_to_xT(b, c)
            for j in range(6):
                c0 = j * 512
                wbf, wc0 = (c.wgu, c0) if c0 < 2048 else (c.wmB, c0 - 2048)
                b.mm(mm_group(nc, c.psA[:], [(c.xTb[:, kk, :], wbf[:, kk, wc0:wc0 + 512]) for kk in range(8)]))
                b.V(lambda c0=c0: nc.vector.tensor_tensor(out=pt[:, c0:c0 + 512], in0=c.psA[:], in1=bin_bc[:, c0:c0 + 512], op=ALU.add))
            b.dma(p_rows[ti], pt)

        b.loop(tps, tA, eng=nc.scalar)

        def tB(ti):
            pm = accf[:, 0:3072]
            p0 = accf[:, 3072:6144]
            pp = stgf[:, 0:3072]
            cw = stgf[:, 3072:5120].rearrange("p (a f) -> p a f", a=4)
            tmp = stgf[:, 5120:5632]
            b.dma(pm, p_m[ti])
            b.dma(p0, p_rows[ti])
            b.dma(pp, p_p[ti])
            for j in range(6):
                cs_ = slice(j * 512, (j + 1) * 512)
                for a in range(3):
                    b.dma(cw[:, a, :], w["conv_w"][a][cs_].partition_broadcast(128))
                b.dma(cw[:, 3, :], w["conv_b"][cs_].partition_broadcast(128))
                b.V(lambda cs_=cs_: nc.vector.tensor_tensor(out=tmp, in0=pm[:, cs_], in1=cw[:, 0, :], op=ALU.mult))
                b.V(lambda cs_=cs_: nc.vector.tensor_tensor(out=p0[:, cs_], in0=p0[:, cs_], in1=cw[:, 1, :], op=ALU.mult))
                b.V(lambda cs_=cs_: nc.vector.tensor_tensor(out=p0[:, cs_], in0=p0[:, cs_], in1=tmp, op=ALU.add))
                b.V(lambda cs_=cs_: nc.vector.tensor_tensor(out=tmp, in0=pp[:, cs_], in1=cw[:, 2, :], op=ALU.mult))
                b.V(lambda cs_=cs_: nc.vector.tensor_tensor(out=p0[:, cs_], in0=p0[:, cs_], in1=tmp, op=ALU.add))
                b.V(lambda cs_=cs_: nc.vector.tensor_tensor(out=p0[:, cs_], in0=p0[:, cs_], in1=cw[:, 3, :], op=ALU.add))
            b.V(lambda: nc.vector.tensor_copy(c.vbf[:], p0[:, 0:1024]))
            b.dma(c.hv_d[ti], c.vbf[:])
            b.dma(c.hx_d[0][ti], p0[:, 1024:2048])
            b.dma(c.hx_d[1][ti], p0[:, 2048:3072])

        for ti_ in range(tps):
            tB(ti_)
        conv(0, c.hv_d, c.hx_d[0], c.hz_d, True)
        conv(1, c.hz_d, c.hx_d[1], c.hx_d[2], False)
        load_ln(b, c, w["ln1_g"], w["ln1_b"])
        bout_bc = c.acc[:, 1, :]
        b.dma(bout_bc, w["b_out"].partition_broadcast(128))

        def tC(ti):
            b.dma(c.xt[:], src_t[si][ti])
            b.dma(c.acc[:, 0, :], c.hx_d[2][ti])
            out_proj_ln(b, c, c.acc[:, 0, :], c.wd, bout_bc, dst_t[si][ti])

        b.loop(tps, tC, eng=nc.scalar)

    b.loop(nseq, seq, eng=nc.scalar)


def build_program(ntok, nlayers=DEPTH, wl=DEPTH, L=4096, mixers=(0, 1, 2, 0), do_moe=True):
    nc = bass.Bass("TRN2", target_bir_lowering=False)
    es = ExitStack()
    c = Ctx()

    def inp(name, shape, dt=F32):
        return nc.dram_tensor(name, list(shape), dt, kind="ExternalInput").ap()

    x = inp("x", [ntok, D])
    W = {}
    ident = inp("ident", [128, 128])
    tri = inp("tri", [128, 3, 128])
    nA = sum(1 for m in mixers[:nlayers] if m == 0)
    if nA:
        W.update({"gla_w_in": inp("gla_w_in", [nA, D, 3104]), "gla_gate_w2": inp("gla_gate_w2", [nA, 2, 16, 512]),
                  "gla_gate_b": inp("gla_gate_b", [nA, 2, 512]), "gla_norm_g": inp("gla_norm_g", [nA, 256]),
                  "gla_w_out": inp("gla_w_out", [nA, D, D])})
    nB = sum(1 for m in mixers[:nlayers] if m == 1)
    if nB:
        tps_ = L // 128
        nfc_ = tps_ + 1
        W.update({"hy_w_in": inp("hy_w_in", [nB, D, 3 * D]), "hy_b_in": inp("hy_b_in", [nB, 3 * D]),
                  "hy_conv_w": inp("hy_conv_w", [nB, 3, 3 * D]), "hy_conv_b": inp("hy_conv_b", [nB, 3 * D]),
                  "hy_ffn_w1": inp("hy_ffn_w1", [nB, 33, 64]), "hy_ffn_b1": inp("hy_ffn_b1", [nB, 64]),
                  "hy_sin_freq": inp("hy_sin_freq", [nB, 2, 64]), "hy_ffn_w2": inp("hy_ffn_w2", [nB, 64, 64]),
                  "hy_ffn_b2": inp("hy_ffn_b2", [nB, 64]), "hy_ffn_w3": inp("hy_ffn_w3", [nB, 64, 4 * D]),
                  "hy_filt_bias": inp("hy_filt_bias", [nB, 2, D]), "hy_w_out": inp("hy_w_out", [nB, D, D]),
                  "hy_b_out": inp("hy_b_out", [nB, D]),
                  "hy_peT": inp("hy_peT", [33, L]), "hy_win": inp("hy_win", [L, D]),
                  "hy_dft_c": inp("hy_dft_c", [nfc_, 128, tps_, 128], BF16), "hy_dft_s": inp("hy_dft_s", [nfc_, 128, tps_, 128], BF16),
                  "hy_idft_c": inp("hy_idft_c", [tps_, 128, nfc_, 128], BF16), "hy_idft_s": inp("hy_idft_s", [tps_, 128, nfc_, 128], BF16)})
        c.hk_d = nc.dram_tensor("hk_d", [2, 2, tps_, 128, D], BF16, kind="Internal").ap()
        c.K_d = nc.dram_tensor("K_d", [2, 2, nfc_, 128, D], F32, kind="Internal").ap()
        c.p_d = nc.dram_tensor("p_d", [L + 2, 3 * D], F32, kind="Internal").ap()
        c.hv_d = nc.dram_tensor("hv_d", [tps_, 128, D], BF16, kind="Internal").ap()
        c.hz_d = nc.dram_tensor("hz_d", [tps_, 128, D], BF16, kind="Internal").ap()
        c.hx_d = nc.dram_tensor("hx_d", [3, tps_, 128, D], F32, kind="Internal").ap()
    nC = sum(1 for m in mixers[:nlayers] if m == 2)
    if nC:
        W.update({"na_w_in": inp("na_w_in", [nC, D, 3 * D]), "na_b_in": inp("na_b_in", [nC, 3 * D]),
                  "na_bias2": inp("na_bias2", [nC, 32, 64, 15, 64]), "na_w_out": inp("na_w_out", [nC, D, D]),
                  "na_b_out": inp("na_b_out", [nC, D])})
        c.qT_d = nc.dram_tensor("qT_d", [8, 128, ntok], BF16, kind="Internal").ap()
        c.kT_d = nc.dram_tensor("kT_d", [8, 128, ntok], BF16, kind="Internal").ap()
        c.v_d = nc.dram_tensor("v_d", [ntok, D], BF16, kind="Internal").ap()
    W.update({
        "ln1_g": inp("ln1_g", [wl, D]), "ln1_b": inp("ln1_b", [wl, D]),
        "ln2_g": inp("ln2_g", [wl, D]), "ln2_b": inp("ln2_b", [wl, D]),
        "moe_router_w": inp("moe_router_w", [wl, D, NE]), "moe_router_b": inp("moe_router_b", [wl, NE]),
        "moe_w_gu": inp("moe_w_gu", [wl, NE, D, 2 * D]), "moe_b_gu": inp("moe_b_gu", [wl, NE, 2 * D]),
        "moe_w_down": inp("moe_w_down", [wl, NE, D, D]), "moe_b_down": inp("moe_b_down", [wl, NE, D]),
    })
    y = nc.dram_tensor("y", [ntok, D], F32, kind="ExternalOutput").ap()
    c.xa_d = nc.dram_tensor("xa_d", [ntok, D], F32, kind="Internal").ap()
    c.xb_d = nc.dram_tensor("xb_d", [ntok, D], F32, kind="Internal").ap()
    c.acc_d = nc.dram_tensor("acc_d", [ntok, D], F32, kind="Internal").ap()
    c.xT_d = nc.dram_tensor("xT_d", [ntok // 128, 128, 8, 128], BF16, kind="Internal").ap()
    c.wgu_bf_d = nc.dram_tensor("wgu_bf_d", [NE, 128, 8, 2 * D], BF16, kind="Internal").ap()
    c.wd_bf_d = nc.dram_tensor("wd_bf_d", [NE, 128, 8, D], BF16, kind="Internal").ap()
    c.comb_d = nc.dram_tensor("comb_d", [ntok // 128, 128, D], F32, kind="Internal").ap()

    with es:
        b = Builder(nc, es)
        alloc_common(b, c)
        alloc_moe(b, c)
        alloc_mixer(b, c)
        if nC:
            alloc_na(b, c)
        b.dma(c.ident[:], ident)
        b.dma(c.tri[:], tri)
        cur = x
        cnt = [0, 0, 0]
        for li in range(nlayers):
            m = mixers[li]
            j = cnt[m]
            cnt[m] += 1
            mdst = c.xb_d if do_moe else (y if li == nlayers - 1 else c.xa_d)
            if m == 0:
                gw = {"w_in": W["gla_w_in"][j], "gate_w2": W["gla_gate_w2"][j], "gate_b": W["gla_gate_b"][j],
                      "norm_g": W["gla_norm_g"][j], "w_out": W["gla_w_out"][j], "ln1_g": W["ln1_g"][li], "ln1_b": W["ln1_b"][li]}
                gla_layer(b, c, cur, mdst, gw, ntok, L)
            elif m == 1:
                hw = {"w_in": W["hy_w_in"][j], "b_in": W["hy_b_in"][j], "conv_w": W["hy_conv_w"][j], "conv_b": W["hy_conv_b"][j],
                      "ffn_w1": W["hy_ffn_w1"][j], "ffn_b1": W["hy_ffn_b1"][j], "sin_freq": W["hy_sin_freq"][j],
                      "ffn_w2": W["hy_ffn_w2"][j], "ffn_b2": W["hy_ffn_b2"][j], "ffn_w3": W["hy_ffn_w3"][j],
                      "filt_bias": W["hy_filt_bias"][j], "w_out": W["hy_w_out"][j], "b_out": W["hy_b_out"][j],
                      "peT": W["hy_peT"], "win": W["hy_win"], "dft_c": W["hy_dft_c"], "dft_s": W["hy_dft_s"],
                      "idft_c": W["hy_idft_c"], "idft_s": W["hy_idft_s"], "ln1_g": W["ln1_g"][li], "ln1_b": W["ln1_b"][li]}
                hyena_layer(b, c, cur, mdst, hw, ntok, L)
            elif m == 2:
                nw = {"w_in": W["na_w_in"][j], "b_in": W["na_b_in"][j], "bias2": W["na_bias2"][j], "w_out": W["na_w_out"][j],
                      "b_out": W["na_b_out"][j], "ln1_g": W["ln1_g"][li], "ln1_b": W["ln1_b"][li]}
                na_layer(b, c, cur, mdst, nw, ntok, L)
            elif m == -1:
                mdst = cur
            else:
                raise NotImplementedError("Hyena mixer (FFT long convolution) is not implemented in this version")
            if not do_moe:
                cur = c.xa_d
                continue
            mo_in = mdst
            w = {"router_w": W["moe_router_w"][li], "router_b": W["moe_router_b"][li],
                 "w_gu": W["moe_w_gu"][li], "b_gu": W["moe_b_gu"][li],
                 "w_down": W["moe_w_down"][li], "b_down": W["moe_b_down"][li],
                 "ln2_g": W["ln2_g"][li], "ln2_b": W["ln2_b"][li]}
            dst = y if li == nlayers - 1 else c.xa_d
            moe_layer(b, c, li, mo_in, dst, w, ntok)
            cur = c.xa_d
        b.finish()
    return nc


def na_bias_layout(rpb):
    n, H = rpb.shape[0], rpb.shape[1]
    cq = np.arange(64)[:, None]
    jk = np.arange(64)[None, :]
    cs = np.clip(cq - 8, 0, 48)
    inwin = (jk >= cs) & (jk < cs + 16)
    dc = np.clip(jk - cq + 15, 0, 30)
    g = rpb[:, :, :, dc]
    g = np.transpose(g, (0, 1, 3, 2, 4))
    out = np.where(inwin[None, None, :, None, :], g, np.float32(-30000.0)).astype(np.float32)
    return np.ascontiguousarray(out)


NCORES = 4
SEQ = 4096
BATCH = 16


def kernel(**inputs):
    n = NCORES
    ntok = BATCH * SEQ // n
    nc = build_program(ntok, nlayers=DEPTH, wl=DEPTH, L=SEQ, mixers=(0, 1, 2, 0), do_moe=True)
    s_ = np.arange(128)[:, None]
    t_ = np.arange(128)[None, :]
    tri = np.stack([(s_ <= t_), (s_ >= t_), (s_ > t_)], 1).astype(np.float32)
    shared = {k: np.ascontiguousarray(np.asarray(v, dtype=np.float32)) for k, v in inputs.items()
              if k.startswith(("ln", "moe_", "gla_", "hy_")) or k in ("na_w_in", "na_b_in", "na_w_out", "na_b_out")}
    shared.update(hyena_consts(SEQ))
    shared["na_bias2"] = na_bias_layout(np.asarray(inputs["na_rpb"], dtype=np.float32))
    shared["ident"] = np.eye(128, dtype=np.float32)
    shared["tri"] = tri
    x = np.asarray(inputs["x"], dtype=np.float32).reshape(n, ntok, D)
    in_maps = [dict(shared, x=np.ascontiguousarray(x[i])) for i in range(n)]
    res = run_bass_kernel_spmd(nc, in_maps, core_ids=list(range(n)))
    out = np.concatenate([res.results[i]["y"] for i in range(n)], axis=0)
    return out.reshape(BATCH, SEQ, D).astype(np.float32)
```
